# Optimizing a Trainium2 kernel written in Bass

```python
import math
import jax, jax.numpy as jnp
from jax import lax
import numpy as np

D_MODEL = 1024
BATCH = 8
SEQ = 2048
DEPTH = 1
DEC_BATCH = 128
DEC_SEQ = 8
PAST_LEN = 8192
PAGE_SIZE = 128

N_HEADS = 16
HEAD_DIM = 64
KV_HEADS = 2
GROUP = N_HEADS // KV_HEADS
CMP_BLOCK = 32
CMP_STRIDE = 16
CMP_HIDDEN = 2 * HEAD_DIM
SEL_BLOCK = 64
N_SELECT = 16
N_LOCAL = 2
WINDOW = 512
Q_BLOCK = 128
KV_SLOTS = 4
D_INNER = D_MODEL
SSM_HEAD_DIM = 64
SSM_HEADS = D_INNER // SSM_HEAD_DIM
SSM_GROUPS = 4
HEADS_PER_GROUP = SSM_HEADS // SSM_GROUPS
D_STATE = 128
CONV_W = 4
CONV_DIM = D_INNER + 2 * SSM_GROUPS * D_STATE
SSD_CHUNK = 128
N_EXPERT_GROUPS = 4
EXPERTS_PER_GROUP = 8
N_EXPERTS = N_EXPERT_GROUPS * EXPERTS_PER_GROUP
TOP_K_IN_GROUP = 2
D_EXPERT = 512
EPS = 1e-6
IN_SPLITS = (N_HEADS * HEAD_DIM, 6 * KV_HEADS * HEAD_DIM, 3 * N_HEADS, D_INNER, CONV_DIM, SSM_HEADS, 2 * D_MODEL)
D_IN_PROJ = N_HEADS * HEAD_DIM + 6 * KV_HEADS * HEAD_DIM + 3 * N_HEADS + D_INNER + CONV_DIM + SSM_HEADS + 2 * D_MODEL

kernel_name = 'hybrid_nsa_mamba2_hmoe_step'


def rmsnorm(x, g):
    xf = x.astype(jnp.float32)
    xf = xf * lax.rsqrt(jnp.mean(xf * xf, axis=-1, keepdims=True) + EPS)
    return (xf * g.astype(jnp.float32)).astype(x.dtype)


def masked_softmax(logits, mask):
    logits = jnp.where(mask, logits.astype(jnp.float32), -jnp.inf)
    m = jnp.max(logits, axis=-1, keepdims=True)
    p = jnp.exp(logits - jnp.where(jnp.isfinite(m), m, 0.0))
    return p / jnp.maximum(jnp.sum(p, axis=-1, keepdims=True), 1e-30)


def alibi_slopes():
    h = jnp.arange(1, N_HEADS + 1, dtype=jnp.float32)
    return jnp.exp2(-8.0 * h / N_HEADS).reshape(KV_HEADS, GROUP)


def mixer_inputs(x, norm1, w_in, q_norm, k_norm):
    b, t = x.shape[:2]
    xn = rmsnorm(x, norm1)
    points = [int(v) for v in np.cumsum(IN_SPLITS)[:-1]]
    q, kv, ag, z, xbc, dt_raw, mg = jnp.split(xn @ w_in, points, axis=-1)
    q = rmsnorm(q.reshape(b, t, KV_HEADS, GROUP, HEAD_DIM), q_norm) * (HEAD_DIM ** -0.5)
    kv = kv.reshape(b, t, 6, KV_HEADS, HEAD_DIM)
    sk = rmsnorm(kv[:, :, 2], k_norm[1])
    wk = rmsnorm(kv[:, :, 4], k_norm[2])
    rows = (kv[:, :, 0], kv[:, :, 1], sk, kv[:, :, 3])
    win = (wk, kv[:, :, 5])
    gate = jax.nn.sigmoid(ag.astype(jnp.float32)).astype(x.dtype).reshape(b, t, KV_HEADS, GROUP, 3)
    return q, gate, rows, win, z, xbc, dt_raw, mg


def compress(rows, pe, w1, w2):
    b, t = rows.shape[:2]
    nch = t // CMP_STRIDE
    ch = rows[:, :nch * CMP_STRIDE].reshape(b, nch, CMP_STRIDE, KV_HEADS, HEAD_DIM)
    ch = jnp.swapaxes(ch, 2, 3).reshape(b, nch, KV_HEADS, CMP_STRIDE * HEAD_DIM)
    half = CMP_STRIDE * HEAD_DIM
    lead = ch @ w1[:half]
    trail = ch @ w1[half:]
    h = lead[:, :-1] + trail[:, 1:] + pe.reshape(-1) @ w1
    return jax.nn.gelu(h) @ w2


def to_blocks(rows):
    b, t = rows.shape[:2]
    nb = -(-t // SEL_BLOCK)
    rows = jnp.pad(rows, ((0, 0), (0, nb * SEL_BLOCK - t), (0, 0), (0, 0)))
    return rows.reshape(b, nb, SEL_BLOCK, KV_HEADS, HEAD_DIM)


def nsa_context(ck, cv, sk, sv, k_norm, cmp_pe, cmp_w1, cmp_w2):
    kc = rmsnorm(compress(ck, cmp_pe[0], cmp_w1[0], cmp_w2[0]), k_norm[0])
    vc = compress(cv, cmp_pe[1], cmp_w1[1], cmp_w2[1])
    cend = jnp.arange(kc.shape[1], dtype=jnp.int32) * CMP_STRIDE + (CMP_BLOCK - 1)
    return kc, vc, cend, to_blocks(sk), to_blocks(sv)


def nsa_core(q, qpos, kc, vc, cend, kblk, vblk, kw, vw, wpos, gate, slopes):
    f32 = jnp.float32
    b, tq = q.shape[:2]
    nb = kblk.shape[1]
    slope5 = slopes[None, None, :, :, None]
    dist_c = qpos[:, None] - cend[None, :]
    s_c = jnp.einsum('btgrd,bngd->btgrn', q, kc).astype(f32) - slope5 * dist_c.astype(f32)[None, :, None, None, :]
    p_c = masked_softmax(s_c, (dist_c >= 0)[None, :, None, None, :])
    o_c = jnp.einsum('btgrn,bngd->btgrd', p_c.astype(vc.dtype), vc)
    jb = jnp.arange(nb, dtype=jnp.int32)
    cstart = cend - (CMP_BLOCK - 1)
    cover = ((cstart[:, None] < (jb[None, :] + 1) * SEL_BLOCK) & (cend[:, None] >= jb[None, :] * SEL_BLOCK)).astype(f32)
    imp = jnp.einsum('btgrn,nj->btgj', p_c, cover)
    back = (qpos // SEL_BLOCK)[:, None] - jb[None, :]
    forced = (jb[None, :] == 0) | ((back >= 0) & (back < N_LOCAL))
    imp = jnp.where(forced[None, :, None, :], jnp.inf, jnp.where((back < 0)[None, :, None, :], -jnp.inf, imp))
    n_top = min(N_SELECT, nb)
    _, idx = lax.top_k(imp, n_top)
    take = jax.vmap(jax.vmap(lambda blocks, ids: blocks[ids]))
    ix = jnp.moveaxis(idx, 2, 1).reshape(b, KV_HEADS, tq * n_top)
    ks = take(jnp.moveaxis(kblk, 3, 1), ix).reshape(b, KV_HEADS, tq, n_top, SEL_BLOCK, HEAD_DIM)
    vs = take(jnp.moveaxis(vblk, 3, 1), ix).reshape(b, KV_HEADS, tq, n_top, SEL_BLOCK, HEAD_DIM)
    spos = idx[..., None] * SEL_BLOCK + jnp.arange(SEL_BLOCK, dtype=jnp.int32)
    dist_s = (qpos[None, :, None, None, None] - spos)[:, :, :, None]
    s_s = jnp.einsum('btgrd,bgtksd->btgrks', q, ks).astype(f32) - slopes[None, None, :, :, None, None] * dist_s.astype(f32)
    p_s = masked_softmax(s_s.reshape(b, tq, KV_HEADS, GROUP, n_top * SEL_BLOCK),
                         (dist_s >= 0).reshape(b, tq, KV_HEADS, 1, n_top * SEL_BLOCK))
    o_s = jnp.einsum('btgrks,bgtksd->btgrd', p_s.reshape(b, tq, KV_HEADS, GROUP, n_top, SEL_BLOCK).astype(vs.dtype), vs)
    dist_w = qpos[:, None] - wpos[None, :]
    valid_w = (dist_w >= 0) & (dist_w < WINDOW) & (wpos[None, :] >= 0)
    s_w = jnp.einsum('btgrd,bsgd->btgrs', q, kw).astype(f32) - slope5 * dist_w.astype(f32)[None, :, None, None, :]
    p_w = masked_softmax(s_w, valid_w[None, :, None, None, :])
    o_w = jnp.einsum('btgrs,bsgd->btgrd', p_w.astype(vw.dtype), vw)
    return gate[..., 0:1] * o_c + gate[..., 1:2] * o_s + gate[..., 2:3] * o_w


def nsa_prompt(q, gate, rows, win, slopes, k_norm, cmp_pe, cmp_w1, cmp_w2):
    b, t = q.shape[:2]
    kc, vc, cend, kblk, vblk = nsa_context(rows[0], rows[1], rows[2], rows[3], k_norm, cmp_pe, cmp_w1, cmp_w2)
    pad = ((0, 0), (WINDOW, 0), (0, 0), (0, 0))
    wk = jnp.pad(win[0], pad)
    wv = jnp.pad(win[1], pad)
    nq = t // Q_BLOCK

    def one_block(args):
        qb, gb, start = args
        qpos = start + jnp.arange(Q_BLOCK, dtype=jnp.int32)
        wpos = start - WINDOW + jnp.arange(Q_BLOCK + WINDOW, dtype=jnp.int32)
        kwb = lax.dynamic_slice_in_dim(wk, start, Q_BLOCK + WINDOW, axis=1)
        vwb = lax.dynamic_slice_in_dim(wv, start, Q_BLOCK + WINDOW, axis=1)
        return nsa_core(qb, qpos, kc, vc, cend, kblk, vblk, kwb, vwb, wpos, gb, slopes)

    def blocks(a):
        return jnp.moveaxis(a.reshape((b, nq, Q_BLOCK) + a.shape[2:]), 1, 0)

    out = lax.map(one_block, (blocks(q), blocks(gate), jnp.arange(nq, dtype=jnp.int32) * Q_BLOCK))
    return jnp.moveaxis(out, 0, 1).reshape(b, t, N_HEADS * HEAD_DIM)


def nsa_sample(q, gate, rows_new, win_new, past_rows, win_buf, slopes, k_norm, cmp_pe, cmp_w1, cmp_w2):
    b, s = q.shape[:2]
    past_len = past_rows[0].shape[1]
    full = [jnp.concatenate([p, r], axis=1) for p, r in zip(past_rows, rows_new)]
    kc, vc, cend, kblk, vblk = nsa_context(full[0], full[1], full[2], full[3], k_norm, cmp_pe, cmp_w1, cmp_w2)
    w0 = win_buf.shape[1]
    wk = jnp.concatenate([win_buf[:, :, 0].astype(win_new[0].dtype), win_new[0]], axis=1)
    wv = jnp.concatenate([win_buf[:, :, 1].astype(win_new[1].dtype), win_new[1]], axis=1)
    wpos = past_len - w0 + jnp.arange(w0 + s, dtype=jnp.int32)
    qpos = past_len + jnp.arange(s, dtype=jnp.int32)
    out = nsa_core(q, qpos, kc, vc, cend, kblk, vblk, wk, wv, wpos, gate, slopes)
    keep = min(WINDOW, w0 + s)
    new_win = jnp.stack([wk[:, -keep:], wv[:, -keep:]], axis=2)
    return out.reshape(b, s, N_HEADS * HEAD_DIM), new_win


def ssd_scan(x, dt, a, bm, cm, h0):
    bsz, t = x.shape[:2]
    L = min(SSD_CHUNK, t)
    nc = -(-t // L)
    pad = nc * L - t

    def padt(u):
        return jnp.pad(u, [(0, 0), (0, pad)] + [(0, 0)] * (u.ndim - 2))

    x, dt, bm, cm = padt(x), padt(dt), padt(bm), padt(cm)
    xg = (x * dt[..., None]).reshape(bsz, nc, L, SSM_GROUPS, HEADS_PER_GROUP, SSM_HEAD_DIM)
    ad = jnp.moveaxis((dt * a).reshape(bsz, nc, L, SSM_GROUPS, HEADS_PER_GROUP), 2, -1)
    acum = jnp.cumsum(ad, axis=-1)
    bc = bm.reshape(bsz, nc, L, SSM_GROUPS, D_STATE)
    cc = cm.reshape(bsz, nc, L, SSM_GROUPS, D_STATE)
    causal = jnp.tril(jnp.ones((L, L), dtype=bool))
    seg = acum[..., :, None] - acum[..., None, :]
    lmat = jnp.where(causal, jnp.exp(jnp.where(causal, seg, 0.0)), 0.0)
    cb = jnp.einsum('bclgn,bcsgn->bcgls', cc, bc)
    y_diag = jnp.einsum('bcgrls,bcsgrp->bclgrp', cb[:, :, :, None] * lmat, xg)
    decay = jnp.exp(acum[..., -1:] - acum)
    states = jnp.einsum('bclgn,bcgrl,bclgrp->bcgrpn', bc, decay, xg)
    chunk_decay = jnp.exp(acum[..., -1])

    def step(h, inp):
        st, d = inp
        return h * d[..., None, None] + st, h

    h_init = h0.reshape(bsz, SSM_GROUPS, HEADS_PER_GROUP, SSM_HEAD_DIM, D_STATE)
    h_last, h_prev = lax.scan(step, h_init, (jnp.moveaxis(states, 1, 0), jnp.moveaxis(chunk_decay, 1, 0)))
    h_prev = jnp.moveaxis(h_prev, 0, 1)
    y_off = jnp.einsum('bclgn,bcgrpn,bcgrl->bclgrp', cc, h_prev, jnp.exp(acum))
    y = (y_diag + y_off).reshape(bsz, nc * L, SSM_HEADS, SSM_HEAD_DIM)[:, :t]
    return y, h_last.reshape(bsz, SSM_HEADS, SSM_HEAD_DIM, D_STATE)


def mamba_branch(z, xbc, dt_raw, conv_buf, h0, conv_w, conv_b, dt_bias, a_log, d_skip, ssm_norm):
    f32 = jnp.float32
    b, t = xbc.shape[:2]
    full = jnp.concatenate([conv_buf.astype(xbc.dtype), xbc], axis=1)
    conv = lax.conv_general_dilated(full, conv_w[:, None, :].astype(xbc.dtype), window_strides=(1,), padding='VALID',
                                    dimension_numbers=('NWC', 'WIO', 'NWC'), feature_group_count=CONV_DIM)
    u = jax.nn.silu(conv + conv_b)
    new_conv = full[:, -(CONV_W - 1):]
    xs, bm, cm = jnp.split(u, [D_INNER, D_INNER + SSM_GROUPS * D_STATE], axis=-1)
    xs = xs.reshape(b, t, SSM_HEADS, SSM_HEAD_DIM).astype(f32)
    bm = bm.reshape(b, t, SSM_GROUPS, D_STATE).astype(f32)
    cm = cm.reshape(b, t, SSM_GROUPS, D_STATE).astype(f32)
    dt = jax.nn.softplus(dt_raw.astype(f32) + dt_bias.astype(f32))
    a = -jnp.exp(a_log.astype(f32))
    y, h_last = ssd_scan(xs, dt, a, bm, cm, h0.astype(f32))
    y = y + d_skip.astype(f32)[:, None] * xs
    y = y.reshape(b, t, D_INNER) * jax.nn.silu(z.astype(f32))
    y = rmsnorm(y.reshape(b, t, SSM_GROUPS, D_INNER // SSM_GROUPS), ssm_norm.reshape(SSM_GROUPS, -1)).reshape(b, t, D_INNER)
    return y.astype(z.dtype), new_conv, h_last


def hier_moe(x, w_rg, b_rg, w_re, b_re, w_eg, w_eu, w_ed):
    f32 = jnp.float32
    n = x.shape[0]
    rows = jnp.arange(n)
    g_logits = (x @ w_rg).astype(f32) + b_rg.astype(f32)
    g_idx = jnp.argmax(g_logits, axis=-1)
    g_w = jax.nn.softmax(g_logits, axis=-1)[rows, g_idx][:, None]
    e_logits = ((x @ w_re).astype(f32) + b_re.astype(f32)).reshape(n, N_EXPERT_GROUPS, EXPERTS_PER_GROUP)[rows, g_idx]
    top_v, top_i = lax.top_k(e_logits, TOP_K_IN_GROUP)
    top_w = jax.nn.softmax(top_v, axis=-1) * g_w
    eid = g_idx[:, None] * EXPERTS_PER_GROUP + top_i
    comb = jnp.einsum('nk,nke->ne', top_w, jax.nn.one_hot(eid, N_EXPERTS, dtype=f32)).astype(x.dtype)
    y = jnp.zeros_like(x)
    for gi in range(N_EXPERT_GROUPS):
        sl = slice(gi * EXPERTS_PER_GROUP, (gi + 1) * EXPERTS_PER_GROUP)
        hg = jax.nn.silu(jnp.einsum('nd,edf->nef', x, w_eg[sl])) * jnp.einsum('nd,edf->nef', x, w_eu[sl])
        y = y + jnp.einsum('nef,efd->nd', hg * comb[:, sl, None], w_ed[sl])
    return y


def finish(x, attn, ssm, merge_logits, w_branch_attn, w_branch_ssm, w_out, norm2, w_rg, b_rg, w_re, b_re, w_eg, w_eu, w_ed):
    ga, gm = jnp.split(jax.nn.sigmoid(merge_logits.astype(jnp.float32)).astype(x.dtype), 2, axis=-1)
    mixed = ga * (attn @ w_branch_attn) + gm * (ssm @ w_branch_ssm)
    h = x + mixed @ w_out
    b, t, d = h.shape
    hn = rmsnorm(h, norm2).reshape(b * t, d)
    return h + hier_moe(hn, w_rg, b_rg, w_re, b_re, w_eg, w_eu, w_ed).reshape(b, t, d)


def setup_inputs(seed: int = 0) -> dict:
    key = jax.random.key(seed)
    ks = jax.random.split(key, 40)
    f32 = jnp.float32
    n_pages = PAST_LEN // PAGE_SIZE
    n_used = DEC_BATCH * n_pages
    n_pool = n_used + (n_used + 3) // 4
    page_table = jax.random.permutation(ks[0], n_pool)[:n_used].reshape(DEC_BATCH, n_pages).astype(jnp.int32)
    win_len = min(WINDOW, PAST_LEN)

    def normal(k, shape, scale):
        return jax.random.normal(k, shape, f32) * scale

    def gain(k, shape):
        return 1.0 + 0.02 * jax.random.normal(k, shape, f32)

    dt0 = jnp.exp(jax.random.uniform(ks[16], (DEPTH, SSM_HEADS), f32, math.log(1e-3), math.log(1e-1)))
    return {
        'x_prompt': normal(ks[1], (BATCH, SEQ, D_MODEL), 1.0),
        'x_sample': normal(ks[2], (DEC_BATCH, DEC_SEQ, D_MODEL), 1.0),
        'cache_kv': normal(ks[3], (DEPTH, n_pool, PAGE_SIZE, KV_SLOTS, KV_HEADS, HEAD_DIM), 1.0),
        'cache_win_kv': normal(ks[4], (DEPTH, DEC_BATCH, win_len, 2, KV_HEADS, HEAD_DIM), 1.0),
        'state_conv': normal(ks[5], (DEPTH, DEC_BATCH, CONV_W - 1, CONV_DIM), 1.0),
        'state_ssm': normal(ks[6], (DEPTH, DEC_BATCH, SSM_HEADS, SSM_HEAD_DIM, D_STATE), 0.1),
        'page_table': page_table,
        'norm1': gain(ks[7], (DEPTH, D_MODEL)),
        'w_in': normal(ks[8], (DEPTH, D_MODEL, D_IN_PROJ), D_MODEL ** -0.5),
        'q_norm': gain(ks[9], (DEPTH, HEAD_DIM)),
        'k_norm': gain(ks[10], (DEPTH, 3, HEAD_DIM)),
        'cmp_pe': normal(ks[11], (DEPTH, 2, CMP_BLOCK, HEAD_DIM), 0.1),
        'cmp_w1': normal(ks[12], (DEPTH, 2, CMP_BLOCK * HEAD_DIM, CMP_HIDDEN), (CMP_BLOCK * HEAD_DIM) ** -0.5),
        'cmp_w2': normal(ks[13], (DEPTH, 2, CMP_HIDDEN, HEAD_DIM), CMP_HIDDEN ** -0.5),
        'conv_w': normal(ks[14], (DEPTH, CONV_W, CONV_DIM), CONV_W ** -0.5),
        'conv_b': normal(ks[15], (DEPTH, CONV_DIM), 0.02),
        'dt_bias': dt0 + jnp.log(-jnp.expm1(-dt0)),
        'a_log': jnp.log(jax.random.uniform(ks[17], (DEPTH, SSM_HEADS), f32, 1.0, 16.0)),
        'd_skip': gain(ks[18], (DEPTH, SSM_HEADS)),
        'ssm_norm': gain(ks[19], (DEPTH, D_INNER)),
        'w_branch_attn': normal(ks[20], (DEPTH, N_HEADS * HEAD_DIM, D_MODEL), (N_HEADS * HEAD_DIM) ** -0.5),
        'w_branch_ssm': normal(ks[21], (DEPTH, D_INNER, D_MODEL), D_INNER ** -0.5),
        'w_out': normal(ks[22], (DEPTH, D_MODEL, D_MODEL), D_MODEL ** -0.5),
        'norm2': gain(ks[23], (DEPTH, D_MODEL)),
        'w_router_group': normal(ks[24], (DEPTH, D_MODEL, N_EXPERT_GROUPS), D_MODEL ** -0.5),
        'b_router_group': normal(ks[25], (DEPTH, N_EXPERT_GROUPS), 0.01),
        'w_router_expert': normal(ks[26], (DEPTH, D_MODEL, N_EXPERTS), D_MODEL ** -0.5),
        'b_router_expert': normal(ks[27], (DEPTH, N_EXPERTS), 0.01),
        'w_exp_gate': normal(ks[28], (DEPTH, N_EXPERTS, D_MODEL, D_EXPERT), D_MODEL ** -0.5),
        'w_exp_up': normal(ks[29], (DEPTH, N_EXPERTS, D_MODEL, D_EXPERT), D_MODEL ** -0.5),
        'w_exp_down': normal(ks[30], (DEPTH, N_EXPERTS, D_EXPERT, D_MODEL), D_EXPERT ** -0.5),
    }


def reference(x_prompt, x_sample, cache_kv, cache_win_kv, state_conv, state_ssm, page_table, norm1, w_in, q_norm, k_norm,
              cmp_pe, cmp_w1, cmp_w2, conv_w, conv_b, dt_bias, a_log, d_skip, ssm_norm, w_branch_attn, w_branch_ssm, w_out,
              norm2, w_router_group, b_router_group, w_router_expert, b_router_expert, w_exp_gate, w_exp_up, w_exp_down):
    slopes = alibi_slopes()
    bp, sp = x_prompt.shape[:2]
    bs = x_sample.shape[0]
    hp, hs = x_prompt, x_sample
    kv_p, win_p, conv_p, ssm_p = [], [], [], []
    kv_s, win_s, conv_s, ssm_s = [], [], [], []
    for l in range(DEPTH):
        q, gate, rows, win, z, xbc, dtr, mg = mixer_inputs(hp, norm1[l], w_in[l], q_norm[l], k_norm[l])
        attn = nsa_prompt(q, gate, rows, win, slopes, k_norm[l], cmp_pe[l], cmp_w1[l], cmp_w2[l])
        ssm, cbuf, hstate = mamba_branch(z, xbc, dtr, jnp.zeros((bp, CONV_W - 1, CONV_DIM), hp.dtype),
                                         jnp.zeros((bp, SSM_HEADS, SSM_HEAD_DIM, D_STATE), jnp.float32),
                                         conv_w[l], conv_b[l], dt_bias[l], a_log[l], d_skip[l], ssm_norm[l])
        kv_p.append(jnp.stack(rows, axis=2))
        win_p.append(jnp.stack(win, axis=2)[:, -min(WINDOW, sp):])
        conv_p.append(cbuf)
        ssm_p.append(hstate)
        hp = finish(hp, attn, ssm, mg, w_branch_attn[l], w_branch_ssm[l], w_out[l], norm2[l], w_router_group[l],
                    b_router_group[l], w_router_expert[l], b_router_expert[l], w_exp_gate[l], w_exp_up[l], w_exp_down[l])
        q, gate, rows, win, z, xbc, dtr, mg = mixer_inputs(hs, norm1[l], w_in[l], q_norm[l], k_norm[l])
        past = [cache_kv[l, page_table, :, si].reshape(bs, -1, KV_HEADS, HEAD_DIM).astype(hs.dtype) for si in range(KV_SLOTS)]
        attn, new_win = nsa_sample(q, gate, rows, win, past, cache_win_kv[l], slopes, k_norm[l], cmp_pe[l], cmp_w1[l], cmp_w2[l])
        ssm, cbuf, hstate = mamba_branch(z, xbc, dtr, state_conv[l], state_ssm[l], conv_w[l], conv_b[l], dt_bias[l],
                                         a_log[l], d_skip[l], ssm_norm[l])
        kv_s.append(jnp.stack(rows, axis=2))
        win_s.append(new_win)
        conv_s.append(cbuf)
        ssm_s.append(hstate)
        hs = finish(hs, attn, ssm, mg, w_branch_attn[l], w_branch_ssm[l], w_out[l], norm2[l], w_router_group[l],
                    b_router_group[l], w_router_expert[l], b_router_expert[l], w_exp_gate[l], w_exp_up[l], w_exp_down[l])
    return (hp, hs, jnp.stack(kv_p), jnp.stack(win_p), jnp.stack(conv_p), jnp.stack(ssm_p),
            jnp.stack(kv_s), jnp.stack(win_s), jnp.stack(conv_s), jnp.stack(ssm_s))
```

```python
import numpy as np
import ml_dtypes
from contextlib import ExitStack
import concourse.bass as bass
import concourse.mybir as mybir
from concourse.bass_utils import run_bass_kernel_spmd

F32 = mybir.dt.float32
BF16 = mybir.dt.bfloat16
I32 = mybir.dt.int32
U32 = mybir.dt.uint32
ALU = mybir.AluOpType
AF = mybir.ActivationFunctionType
AX = mybir.AxisListType

NCORES = 8
D = 1024
TP = 2048
TS = 128
TT = TP + TS
NT = TT // 128
DIN = 6976
EPS = 1e-6
C_Q, C_KV, C_AG, C_Z, C_XBC, C_DT, C_MG = 0, 1024, 1792, 1840, 2864, 4912, 4928

MT_W = 912 + 2048
AT_W = 1024 + 128 + 32 + 2048 + 128 + 3 * 2048 + 65 * 16 + 62 + 62 + 64
ST_W = 4568
EPOCH = 2048
SAME_ENGINE_SYNC = True


class Sched:
    def __init__(self, n_dma_sems=24):
        self.streams = {e: [] for e in ("pe", "act", "dve", "pool", "sp")}
        self.cnt = {e: 0 for e in self.streams}
        self.lastw = {}
        self.readers = {}
        self.waited = {e: {} for e in self.streams}
        self.n_dma = n_dma_sems
        self.n_sw = 20
        self.sw_rr = 0
        self.dma_uses = [0] * (n_dma_sems + self.n_sw)
        self.dma_rr = 0
        self.sem_keys = set()

    def _need(self, eng, tok, waits):
        if tok is None:
            return
        s, v = tok
        if s[0] == eng and (eng == "pe" or not SAME_ENGINE_SYNC) and s[0] != "dma":
            return
        if self.waited[eng].get(s, 0) >= v:
            return
        if waits.get(s, 0) < v:
            waits[s] = v

    def _deps(self, eng, reads, writes):
        waits = {}
        ps_r = [k for k in reads if isinstance(k, str) and k.startswith("ps")]
        if ps_r:
            writes = list(writes) + ps_r
        for k in reads:
            self._need(eng, self.lastw.get(k), waits)
        for k in writes:
            self._need(eng, self.lastw.get(k), waits)
            for s, v in self.readers.get(k, {}).items():
                self._need(eng, (s, v), waits)
        for s, v in waits.items():
            self.waited[eng][s] = v
        return waits

    def _commit(self, tok, reads, writes):
        s, v = tok
        ps_r = [k for k in reads if isinstance(k, str) and k.startswith("ps")]
        if ps_r:
            writes = list(writes) + ps_r
        for k in reads:
            r = self.readers.setdefault(k, {})
            if r.get(s, 0) < v:
                r[s] = v
        for k in writes:
            self.lastw[k] = tok
            self.readers[k] = {}

    def op(self, eng, fn, reads=(), writes=()):
        waits = self._deps(eng, reads, writes)
        c = self.cnt[eng]
        self.cnt[eng] = c + 1
        tok = ((eng, c // EPOCH), (c % EPOCH) + 1)
        self.sem_keys.add(tok[0])
        self.streams[eng].append((list(waits.items()), fn, tok, 1))
        self._commit(tok, reads, writes)
        return tok

    def dma(self, q, fn, reads=(), writes=()):
        if q == "pool":
            i = self.n_dma + self.sw_rr
            self.sw_rr = (self.sw_rr + 1) % self.n_sw
        else:
            i = self.dma_rr
            self.dma_rr = (i + 1) % self.n_dma
        n_prev = self.dma_uses[i]
        self.dma_uses[i] = n_prev + 1
        key = ("dma", i)
        self.sem_keys.add(key)
        waits = self._deps(q, reads, writes)
        if n_prev > 0 and self.waited[q].get(key, 0) < 16 * n_prev:
            waits[key] = 16 * n_prev
            self.waited[q][key] = 16 * n_prev
        tok = (key, 16 * (n_prev + 1))
        self.streams[q].append((list(waits.items()), fn, tok, 16))
        self._commit(tok, reads, writes)
        return tok

    def barrier(self):
        waits = []
        for i in range(len(self.dma_uses)):
            if self.dma_uses[i]:
                waits.append((("dma", i), 16 * self.dma_uses[i]))
        for e, c in self.cnt.items():
            if c:
                waits.append(((e, (c - 1) // EPOCH), ((c - 1) % EPOCH) + 1))
        for q in self.streams:
            w = [(s_, v) for s_, v in waits if not (s_[0] == q and q in ('pe', 'sp')) and self.waited[q].get(s_, 0) < v]
            for s_, v in w:
                self.waited[q][s_] = v
            if w:
                self.streams[q].append((w, None, None, 0))
        self.lastw = {}
        self.readers = {}

    def finish(self, q="sp"):
        waits = []
        for i in range(len(self.dma_uses)):
            if self.dma_uses[i]:
                waits.append((("dma", i), 16 * self.dma_uses[i]))
        for e, c in self.cnt.items():
            if c and e != q:
                waits.append(((e, (c - 1) // EPOCH), ((c - 1) % EPOCH) + 1))
        self.streams[q].append((waits, None, None, 0))

    def emit(self, nc, es):
        sems = {}
        for k in sorted(self.sem_keys, key=str):
            sems[k] = es.enter_context(nc.semaphore("s_" + "_".join(str(x) for x in k)))
        block = es.enter_context(nc.Block())

        def run(stream):
            def body(eng):
                for waits, fn, tok, inc in stream:
                    for s, v in waits:
                        eng.wait_ge(sems[s], v)
                    if fn is not None:
                        fn(eng).then_inc(sems[tok[0]], inc)
            return body

        block.tensor(run(self.streams["pe"]))
        block.scalar(run(self.streams["act"]))
        block.vector(run(self.streams["dve"]))
        block.gpsimd(run(self.streams["pool"]))
        block.sync(run(self.streams["sp"]))


class Builder:
    ARENA = 51200

    def __init__(self):
        self.nc = bass.Bass("TRN2", target_bir_lowering=False)
        self.es = ExitStack()
        self.S = Sched()
        self.alt = 0
        self.arena = self.es.enter_context(self.nc.sbuf_tensor("arena", [128, self.ARENA], F32))
        self.top = 0

    def inp(self, name, shape, dt=F32):
        return self.nc.dram_tensor(name, list(shape), dt, kind="ExternalInput").ap()

    def outp(self, name, shape, dt=F32):
        return self.nc.dram_tensor(name, list(shape), dt, kind="ExternalOutput").ap()

    def scratch(self, name, shape, dt=F32):
        return self.nc.dram_tensor(name, list(shape), dt, kind="Internal").ap()

    def sb(self, name, shape, dt=F32):
        n = int(np.prod(shape[1:]))
        nf = n if dt in (F32, I32, U32) else (n + 1) // 2
        nf = (nf + 7) // 8 * 8
        assert self.top + nf <= self.ARENA, f"SBUF arena overflow at {name}: {self.top}+{nf}"
        ap = self.arena[:, self.top:self.top + nf]
        self.top += nf
        if dt != F32:
            ap = ap.bitcast(dt)
        ap = ap[:, 0:n]
        if len(shape) == 3:
            ap = ap.rearrange("p (a b) -> p a b", a=shape[1])
        elif len(shape) == 4:
            ap = ap.rearrange("p (a b c) -> p a b c", a=shape[1], b=shape[2])
        if shape[0] != 128:
            ap = ap[0:shape[0]]
        return ap

    def mark(self):
        return self.top

    def release(self, m):
        self.S.barrier()
        self.top = m

    def ps(self, name, shape, dt=F32):
        return self.es.enter_context(self.nc.psum_tensor(name, list(shape), dt))

    def evac_eng(self):
        self.alt ^= 1
        return "act" if self.alt else "dve"


DEV_SKIP = set()
DEV_NSEQ = 16
POOL_PAGES = 10240


def build_program():
    B = Builder()
    nc, S = B.nc, B.S
    x = B.inp("x", [TT, D])
    w_in = B.inp("w_in", [D, DIN])
    g1 = B.inp("g1_bc", [128, D])
    gk1 = B.inp("gk1_bc", [128, 128])
    gk2 = B.inp("gk2_bc", [128, 128])
    ident_in = B.inp("ident_bf", [128, 128], BF16)
    cwin = B.inp("cache_win", [16, 512, 256])

    kv_out = B.outp("kv_out", [TT, 512])
    winp_out = B.outp("winp_out", [512, 256])
    wins_out = B.outp("wins_out", [16, 512, 256])
    convp_out = B.outp("convp_out", [3, 2048])
    convs_out = B.outp("convs_out", [16, 3, 2048])

    conv_wb = B.inp("conv_wb", [128, 5, 2048])
    mvec = B.inp("mvec", [128, 48])
    gssm = B.inp("gssm_bc", [128, 1024])
    mtab_in = B.inp("mtab", [128, MT_W])
    st_conv = B.inp("state_conv", [16, 3, 2048])
    st_ssm = B.inp("state_ssm", [16, 1024, 128])
    ssmp_out = B.outp("ssmp_out", [1024, 128])
    ssms_out = B.outp("ssms_out", [16, 1024, 128])

    atab_in = B.inp("atab", [128, AT_W])
    ex_in = B.inp("ex_bf", [32, 16, 128], BF16)
    exw_in = B.inp("exw_bf", [128, 32, 128], BF16)
    w1_in = B.inp("w1h", [128, 2, 32, 128])
    w2_in = B.inp("w2h", [128, 2, 64])

    wa_in = B.inp("w_branch_attn", [D, D])
    wm_in = B.inp("w_branch_ssm", [D, D])
    wo_in = B.inp("w_out", [D, D])
    wr_in = B.inp("wr", [128, 8, 36])
    rb_in = B.inp("rb_bc", [128, 36])
    g2_in = B.inp("g2_bc", [128, D])
    NEX = 1 if "moe" in DEV_SKIP else 32
    weg_in = B.inp("w_exp_gate", [NEX, D, 512])
    weu_in = B.inp("w_exp_up", [NEX, D, 512])
    wed_in = B.inp("w_exp_down", [NEX, 512, D])
    y_out = B.outp("y_out", [TT, D])

    stab_in = B.inp("stab", [128, ST_W])
    pt_in = B.inp("pt_bc", [128, 1024], I32)
    ckv_in = B.inp("cache_kv", [POOL_PAGES * 128, 512])

    proj = B.scratch("proj", [TT, DIN])
    Mscr = B.scratch("Mscr", [TT, 1024], BF16)
    fullx = B.scratch("fullx", [16, 11, 2048])
    kvn = B.scratch("kvn", [TT, 768])
    Ascr = B.outp("Ascr", [TT, 1024], BF16)

    ident = B.sb("ident", [128, 128], BF16)
    gk1t = B.sb("gk1t", [128, 128])
    gk2t = B.sb("gk2t", [128, 128])
    m0 = B.mark()
    g1t = B.sb("g1t", [128, D])
    xT = B.sb("xT", [128, 8, TT], BF16)
    rstd = B.sb("rstd", [128, NT])
    ssq = B.sb("ssq", [128, NT])
    xt = [B.sb(f"xt{i}", [128, D]) for i in range(2)]
    xg = [B.sb(f"xg{i}", [128, D], BF16) for i in range(2)]
    junk = B.sb("junk", [128, D])
    wbuf = [B.sb(f"wbuf{i}", [128, 8, 512], BF16) for i in range(2)]
    obuf = [B.sb(f"obuf{i}", [128, 512]) for i in range(3)]
    kvt = [B.sb(f"kvt{i}", [128, 768]) for i in range(2)]
    nsq0 = B.sb("nsq", [128, 128])
    nss0 = B.sb("nss", [128, 4])
    ntmp0 = B.sb("ntmp", [128, 128])
    psall = B.ps("psall", [128, 4096])
    psb = [psall[:, i * 512:(i + 1) * 512] for i in range(8)]

    S.dma("sp", lambda e: e.dma_start(out=ident[:], in_=ident_in), writes=["ident"])
    S.dma("sp", lambda e: e.dma_start(out=g1t[:], in_=g1), writes=["g1t"])
    S.dma("sp", lambda e: e.dma_start(out=gk1t[:], in_=gk1), writes=["gk1t"])
    S.dma("sp", lambda e: e.dma_start(out=gk2t[:], in_=gk2), writes=["gk2t"])
    S.dma("sp", lambda e: e.dma_start(out=wins_out[:, 0:504, :], in_=cwin[:, 8:512, :]),
          writes=["wins_old"])

    for t in range(NT):
        b = t % 2
        S.dma("sp", lambda e, t=t, b=b: e.dma_start(out=xt[b][:], in_=x[t * 128:(t + 1) * 128, :]),
              writes=[f"xt{b}"])
        S.op("act", lambda e, t=t, b=b: e.activation(out=junk[:], in_=xt[b][:], func=AF.Square,
                                                     accum_out=ssq[:, t:t + 1]),
             reads=[f"xt{b}"], writes=["junk", ("ssq", t)])
        S.op("dve", lambda e, b=b: e.tensor_tensor(out=xg[b][:], in0=xt[b][:], in1=g1t[:], op=ALU.mult),
             reads=[f"xt{b}", "g1t"], writes=[f"xg{b}"])
        pb = t % 2
        pst = psb[pb][:].bitcast(BF16)
        for kc in range(8):
            S.op("pe", lambda e, kc=kc, b=b, pst=pst: e.transpose(
                out=pst[:, kc * 128:(kc + 1) * 128], in_=xg[b][:, kc * 128:(kc + 1) * 128], identity=ident[:]),
                reads=[f"xg{b}", "ident"], writes=[f"ps{pb}"])
        ev = B.evac_eng()
        if ev == "act":
            S.op("act", lambda e, t=t, pst=pst: e.activation(
                out=xT[:, :, t * 128:(t + 1) * 128], in_=pst.rearrange("p (k c) -> p k c", k=8), func=AF.Copy),
                reads=[f"ps{pb}"], writes=[("xT", t)])
        else:
            S.op("dve", lambda e, t=t, pst=pst: e.tensor_copy(
                out=xT[:, :, t * 128:(t + 1) * 128], in_=pst.rearrange("p (k c) -> p k c", k=8)),
                reads=[f"ps{pb}"], writes=[("xT", t)])
    S.op("dve", lambda e: e.tensor_scalar(out=rstd[:], in0=ssq[:], scalar1=1.0 / D, scalar2=EPS,
                                          op0=ALU.mult, op1=ALU.add),
         reads=[("ssq", t) for t in range(NT)], writes=["rstd"])
    S.op("act", lambda e: e.activation(out=rstd[:], in_=rstd[:], func=AF.Sqrt), reads=["rstd"], writes=["rstd"])
    S.op("dve", lambda e: e.reciprocal(out=rstd[:], in_=rstd[:]), reads=["rstd"], writes=["rstd"])

    w_v = w_in.rearrange("(kc p) n -> p kc n", p=128)
    nchunks = (DIN + 511) // 512
    oi = 0
    for c in range(nchunks):
        c0 = c * 512
        ncol = min(512, DIN - c0)
        wb = c % 2
        S.dma("pool", lambda e, wb=wb, c0=c0, ncol=ncol: e.dma_start(out=wbuf[wb][:, :, 0:ncol],
                                                                      in_=w_v[:, :, c0:c0 + ncol]),
              writes=[f"wbuf{wb}"])
        for t in range(NT):
            pb = 2 + (oi % 4)
            for kc in range(8):
                S.op("pe", lambda e, kc=kc, t=t, wb=wb, pb=pb, ncol=ncol: e.matmul(
                    psb[pb][:, 0:ncol], lhsT=xT[:, kc, t * 128:(t + 1) * 128], rhs=wbuf[wb][:, kc, 0:ncol],
                    start=(kc == 0), stop=(kc == 7)),
                    reads=[("xT", t), f"wbuf{wb}"], writes=[f"ps{pb}"])
            ob = oi % 3
            ev = B.evac_eng()
            if ev == "act":
                S.op("act", lambda e, t=t, pb=pb, ob=ob, ncol=ncol: e.activation(
                    out=obuf[ob][:, 0:ncol], in_=psb[pb][:, 0:ncol], func=AF.Copy, scale=rstd[:, t:t + 1]),
                    reads=[f"ps{pb}", "rstd"], writes=[f"obuf{ob}"])
            else:
                S.op("dve", lambda e, t=t, pb=pb, ob=ob, ncol=ncol: e.tensor_scalar(
                    out=obuf[ob][:, 0:ncol], in0=psb[pb][:, 0:ncol], scalar1=rstd[:, t:t + 1], scalar2=None,
                    op0=ALU.mult),
                    reads=[f"ps{pb}", "rstd"], writes=[f"obuf{ob}"])
            S.dma("sp", lambda e, t=t, ob=ob, c0=c0, ncol=ncol: e.dma_start(
                out=proj[t * 128:(t + 1) * 128, c0:c0 + ncol], in_=obuf[ob][:, 0:ncol]),
                reads=[f"obuf{ob}"], writes=[("proj", t, c)])
            oi += 1

    def proj_keys(t, lo, hi):
        return [("proj", t, c) for c in range(lo // 512, (hi - 1) // 512 + 1)]

    def rms_heads(src, gain, nh, scale, rkeys, wkey, nsq=None, nss=None, ntmp=None):
        nsq = nsq if nsq is not None else nsq0
        nss = nss if nss is not None else nss0
        ntmp = ntmp if ntmp is not None else ntmp0
        w = nh * 64
        S.op("dve", lambda e: e.tensor_tensor(out=nsq[:, 0:w], in0=src, in1=src, op=ALU.mult),
             reads=rkeys, writes=["nsq"])
        S.op("dve", lambda e: e.tensor_reduce(out=nss[:, 0:nh], in_=nsq[:, 0:w].rearrange("p (h d) -> p h d", d=64),
                                              axis=AX.X, op=ALU.add), reads=["nsq"], writes=["nss"])
        S.op("dve", lambda e: e.tensor_scalar(out=nss[:, 0:nh], in0=nss[:, 0:nh], scalar1=1.0 / 64, scalar2=EPS,
                                              op0=ALU.mult, op1=ALU.add), reads=["nss"], writes=["nss"])
        S.op("act", lambda e: e.activation(out=nss[:, 0:nh], in_=nss[:, 0:nh], func=AF.Sqrt),
             reads=["nss"], writes=["nss"])
        S.op("dve", lambda e: e.reciprocal(out=nss[:, 0:nh], in_=nss[:, 0:nh]), reads=["nss"], writes=["nss"])
        S.op("dve", lambda e: e.tensor_tensor(
            out=ntmp[:, 0:w].rearrange("p (h d) -> p h d", d=64), in0=src.rearrange("p (h d) -> p h d", d=64),
            in1=nss[:, 0:nh].unsqueeze(2).to_broadcast([128, nh, 64]), op=ALU.mult),
            reads=rkeys + ["nss"], writes=["ntmp"])
        if scale == 1.0:
            S.op("dve", lambda e: e.tensor_tensor(out=src, in0=ntmp[:, 0:w], in1=gain, op=ALU.mult),
                 reads=["ntmp"], writes=[wkey])
        else:
            S.op("dve", lambda e: e.scalar_tensor_tensor(out=src, in0=ntmp[:, 0:w], scalar=scale, in1=gain,
                                                         op0=ALU.mult, op1=ALU.mult),
                 reads=["ntmp"], writes=[wkey])

    for t in range(NT):
        b = t % 2
        S.dma("sp", lambda e, t=t, b=b: e.dma_start(out=kvt[b][:], in_=proj[t * 128:(t + 1) * 128, C_KV:C_KV + 768]),
              reads=proj_keys(t, C_KV, C_KV + 768), writes=[f"kvt{b}"])
        rms_heads(kvt[b][:, 256:384], gk1t[:], 2, 1.0, [f"kvt{b}"], f"kvt{b}")
        rms_heads(kvt[b][:, 512:640], gk2t[:], 2, 1.0, [f"kvt{b}"], f"kvt{b}")
        S.dma("sp", lambda e, t=t, b=b: e.dma_start(out=kv_out[t * 128:(t + 1) * 128, :], in_=kvt[b][:, 0:512]),
              reads=[f"kvt{b}"], writes=[("kv_out", t)])
        S.dma("sp", lambda e, t=t, b=b: e.dma_start(out=kvn[t * 128:(t + 1) * 128, :], in_=kvt[b][:]),
              reads=[f"kvt{b}"], writes=[("kvn", t)])
        if 12 <= t < 16:
            S.dma("sp", lambda e, t=t, b=b: e.dma_start(out=winp_out[(t - 12) * 128:(t - 11) * 128, :],
                                                         in_=kvt[b][:, 512:768]),
                  reads=[f"kvt{b}"], writes=[("winp_out", t)])
        if t == 16:
            for sq in range(16):
                S.dma("sp", lambda e, b=b, sq=sq: e.dma_start(
                    out=wins_out[sq, 504:512, :], in_=kvt[b][sq * 8:(sq + 1) * 8, 512:768]),
                    reads=[f"kvt{b}"], writes=[("wins_new", sq)])
    S.dma("sp", lambda e: e.dma_start(out=convp_out, in_=proj[TP - 3:TP, C_XBC:C_XBC + 2048]),
          reads=proj_keys(15, C_XBC, C_XBC + 2048), writes=["convp"])
    S.dma("sp", lambda e: e.dma_start(
        out=convs_out, in_=proj[TP:TT, C_XBC:C_XBC + 2048].rearrange("(b t) n -> b t n", t=8)[:, 5:8, :]),
        reads=proj_keys(16, C_XBC, C_XBC + 2048), writes=["convs"])


    B.release(m0)
    def dve(fn, r=(), w=()):
        return S.op("dve", fn, list(r), list(w))

    def act(fn, r=(), w=()):
        return S.op("act", fn, list(r), list(w))

    def pool(fn, r=(), w=()):
        return S.op("pool", fn, list(r), list(w))

    def pe(fn, r=(), w=()):
        return S.op("pe", fn, list(r), list(w))

    def sp(fn, r=(), w=()):
        return S.dma("sp", fn, list(r), list(w))

    cw = B.sb("cw", [128, 5, 2048])
    mv = B.sb("mv", [128, 48])
    gs = B.sb("gs", [128, 1024])
    mtab = B.sb("mtab", [128, MT_W])
    T_UTp, T_UTs, T_ONp, T_ONs, T_NEGp, T_NEGs, T_IDF = [mtab[:, i * 128:(i + 1) * 128] for i in range(7)]
    T_ROWM = mtab[:, 896:912]
    T_ROWSEL = mtab[:, 912:912 + 2048].rearrange("p (b c) -> p b c", b=16)
    Aneg = B.sb("Aneg", [128, 16])
    hT = B.sb("hT", [128, 1024])
    hTb = B.sb("hTb", [128, 1024], BF16)
    xsh = [B.sb(f"xsh{k}", [128, 1024]) for k in range(4)]
    u = B.sb("u", [128, 2048])
    ub = B.sb("ub", [128, 1024], BF16)
    zt = B.sb("zt", [128, 1024])
    dtt = B.sb("dtt", [128, 16])
    a_t = B.sb("a_t", [128, 16])
    xdt = B.sb("xdt", [128, 16, 64])
    xdtb = B.sb("xdtb", [128, 16, 64], BF16)
    xdd = B.sb("xdd", [128, 16, 64], BF16)
    xddm = B.sb("xddm", [128, 16, 64], BF16)
    a_rep = B.sb("a_rep", [128, 16, 128])
    acs = B.sb("acs", [128, 32])
    ea = B.sb("ea", [128, 16])
    dcy = B.sb("dcy", [128, 16])
    cd = B.sb("cd", [128, 16])
    cds = B.sb("cds", [128, 16, 16])
    tmpE = B.sb("tmpE", [128, 16, 128])
    Wt = B.sb("Wt", [128, 16, 128], BF16)
    BT = B.sb("BT", [128, 4, 128], BF16)
    CT = B.sb("CT", [128, 4, 128], BF16)
    CTpad = B.sb("CTpad", [128, 4, 2176], BF16)
    yb = B.sb("yb", [128, 16, 64])
    y2 = B.sb("y2", [128, 16, 64])
    msq = B.sb("msq", [128, 1024])
    mss = B.sb("mss", [128, 4])
    Mb = B.sb("Mb", [128, 1024], BF16)
    sst = B.sb("sst", [128, 8, 128])
    hTs = B.sb("hTs", [128, 1024])
    hTsb = B.sb("hTsb", [128, 1024], BF16)
    sso = B.sb("sso", [128, 8, 128])

    sp(lambda e: e.dma_start(out=cw[:], in_=conv_wb), w=["cw"])
    sp(lambda e: e.dma_start(out=mv[:], in_=mvec), w=["mv"])
    sp(lambda e: e.dma_start(out=gs[:], in_=gssm), w=["gs"])
    sp(lambda e: e.dma_start(out=mtab[:], in_=mtab_in), w=["mtab"])
    sp(lambda e: e.dma_start(out=fullx[:, 0:3, :], in_=st_conv), w=["fullx_a"])
    sp(lambda e: e.dma_start(out=fullx[:, 3:11, :],
                             in_=proj[TP:TT, C_XBC:C_XBC + 2048].rearrange("(b t) n -> b t n", t=8)),
       w=["fullx_b"])
    act(lambda e: e.activation(out=Aneg[:], in_=mv[:, 16:32], func=AF.Exp), r=["mv"], w=["Aneg"])
    dve(lambda e: e.tensor_scalar(out=Aneg[:], in0=Aneg[:], scalar1=-1.0, scalar2=None, op0=ALU.mult),
        r=["Aneg"], w=["Aneg"])
    dve(lambda e: e.memset(hT[:], 0.0), w=["hT"])
    dve(lambda e: e.memset(hTb[:], 0.0), w=["hTb"])
    dve(lambda e: e.memset(CTpad[:], 0.0), w=["CTpad"])

    hv = lambda ap: ap.rearrange("p (h d) -> p h d", d=64)
    bank_bf = lambda i: psb[i][:].bitcast(BF16)

    def mm1(out, lhsT, rhs, r, w):
        pe(lambda e: e.matmul(out, lhsT=lhsT, rhs=rhs, start=True, stop=True), r=r, w=w)

    for t in ([] if "mamba" in DEV_SKIP else range(NT)):
        smp = (t == NT - 1)
        UT, ON, NEG = (T_UTs, T_ONs, T_NEGs) if smp else (T_UTp, T_ONp, T_NEGp)
        r0 = t * 128
        for half in range(2):
            c0 = half * 1024
            for k in range(4):
                sh = 3 - k
                if smp:
                    for b in range(16):
                        sp(lambda e, k=k, b=b, c0=c0: e.dma_start(
                            out=xsh[k][b * 8:(b + 1) * 8, :], in_=fullx[b, k:k + 8, c0:c0 + 1024]),
                            r=["fullx_a", "fullx_b"], w=[f"xsh{k}"])
                elif t == 0 and sh > 0:
                    dve(lambda e, k=k: e.memset(xsh[k][:], 0.0), w=[f"xsh{k}"])
                    sp(lambda e, k=k, sh=sh, c0=c0: e.dma_start(
                        out=xsh[k][sh:128, :], in_=proj[0:128 - sh, C_XBC + c0:C_XBC + c0 + 1024]),
                        w=[f"xsh{k}"])
                else:
                    sp(lambda e, k=k, sh=sh, c0=c0, r0=r0: e.dma_start(
                        out=xsh[k][:], in_=proj[r0 - sh:r0 - sh + 128, C_XBC + c0:C_XBC + c0 + 1024]),
                        w=[f"xsh{k}"])
            uk = f"u{half}"
            for k in range(3):
                pool(lambda e, k=k, c0=c0: e.tensor_tensor(out=xsh[k][:], in0=xsh[k][:], in1=cw[:, k, c0:c0 + 1024],
                                                           op=ALU.mult), r=[f"xsh{k}", "cw"], w=[f"xsh{k}"])
            dve(lambda e, c0=c0: e.tensor_tensor(out=u[:, c0:c0 + 1024], in0=xsh[3][:], in1=cw[:, 3, c0:c0 + 1024],
                                                 op=ALU.mult), r=["xsh3", "cw"], w=[uk])
            for k in range(3):
                dve(lambda e, k=k, c0=c0: e.tensor_tensor(out=u[:, c0:c0 + 1024], in0=u[:, c0:c0 + 1024],
                                                          in1=xsh[k][:], op=ALU.add), r=[uk, f"xsh{k}"], w=[uk])
            dve(lambda e, c0=c0: e.tensor_tensor(out=u[:, c0:c0 + 1024], in0=u[:, c0:c0 + 1024],
                                                 in1=cw[:, 4, c0:c0 + 1024], op=ALU.add), r=[uk, "cw"], w=[uk])
            act(lambda e, c0=c0: e.activation(out=u[:, c0:c0 + 1024], in_=u[:, c0:c0 + 1024], func=AF.Silu),
                r=[uk], w=[uk])
        sp(lambda e, r0=r0: e.dma_start(out=dtt[:], in_=proj[r0:r0 + 128, C_DT:C_DT + 16]), w=["dtt"])
        sp(lambda e, r0=r0: e.dma_start(out=zt[:], in_=proj[r0:r0 + 128, C_Z:C_Z + 1024]), w=["zt"])
        dve(lambda e: e.tensor_tensor(out=dtt[:], in0=dtt[:], in1=mv[:, 0:16], op=ALU.add), r=["dtt", "mv"], w=["dtt"])
        act(lambda e: e.activation(out=dtt[:], in_=dtt[:], func=AF.Exp), r=["dtt"], w=["dtt"])
        act(lambda e: e.activation(out=dtt[:], in_=dtt[:], func=AF.Ln, bias=1.0), r=["dtt"], w=["dtt"])
        dve(lambda e: e.tensor_tensor(out=a_t[:], in0=dtt[:], in1=Aneg[:], op=ALU.mult), r=["dtt", "Aneg"], w=["a_t"])
        dve(lambda e: e.tensor_tensor(out=xdt[:], in0=hv(u[:, 0:1024]),
                                      in1=dtt[:].unsqueeze(2).to_broadcast([128, 16, 64]), op=ALU.mult),
            r=["u0", "dtt"], w=["xdt"])
        act(lambda e: e.activation(out=xdtb[:], in_=xdt[:], func=AF.Copy), r=["xdt"], w=["xdtb"])
        act(lambda e: e.activation(out=ub[:], in_=u[:, 1024:2048], func=AF.Copy), r=["u1"], w=["ub"])
        pst = bank_bf(0)
        for j in range(8):
            pe(lambda e, j=j, pst=pst: e.transpose(out=pst[:, j * 128:(j + 1) * 128], in_=ub[:, j * 128:(j + 1) * 128],
                                                   identity=ident[:]), r=["ub", "ident"], w=["ps0"])
        dve(lambda e, pst=pst: e.tensor_copy(out=BT[:], in_=pst[:, 0:512].rearrange("p (g c) -> p g c", g=4)),
            r=["ps0"], w=["BT"])
        dve(lambda e, pst=pst: e.tensor_copy(out=CT[:], in_=pst[:, 512:1024].rearrange("p (g c) -> p g c", g=4)),
            r=["ps0"], w=["CT"])
        mm1(psb[0][:, 0:16], UT, a_t[:], ["mtab", "a_t"], ["ps0"])
        mm1(psb[0][:, 16:32], ON, a_t[:], ["mtab", "a_t"], ["ps0"])
        if smp:
            for b in range(16):
                mm1(psb[0][:, 32 + 16 * b:48 + 16 * b], T_ROWSEL[:, b, :], a_t[:], ["mtab", "a_t"], ["ps0"])
            act(lambda e: e.activation(out=cds[:], in_=psb[0][:, 32:288].rearrange("p (b h) -> p b h", b=16),
                                       func=AF.Exp), r=["ps0"], w=["cds"])
        dve(lambda e: e.tensor_copy(out=acs[:], in_=psb[0][:, 0:32]), r=["ps0"], w=["acs"])
        dve(lambda e: e.tensor_copy(out=a_rep[:], in_=a_t[:].unsqueeze(2).to_broadcast([128, 16, 128])),
            r=["a_t"], w=["a_rep"])
        for h in range(16):
            bk = 1 + h // 4
            mm1(psb[bk][:, (h % 4) * 128:(h % 4 + 1) * 128], a_rep[:, h, :], UT, ["a_rep", "mtab"], [f"ps{bk}"])
        for j in range(4):
            dve(lambda e, j=j: e.tensor_tensor(
                out=tmpE[:, 4 * j:4 * j + 4, :], in0=psb[1 + j][:].rearrange("p (h c) -> p h c", h=4),
                in1=acs[:, 4 * j:4 * j + 4].unsqueeze(2).to_broadcast([128, 4, 128]), op=ALU.subtract),
                r=[f"ps{1 + j}", "acs"], w=[f"tmpE{j}"])
            pool(lambda e, j=j, NEG=NEG: e.tensor_tensor(
                out=tmpE[:, 4 * j:4 * j + 4, :], in0=tmpE[:, 4 * j:4 * j + 4, :],
                in1=NEG.unsqueeze(1).to_broadcast([128, 4, 128]), op=ALU.add),
                r=[f"tmpE{j}", "mtab"], w=[f"tmpE{j}"])
            act(lambda e, j=j: e.activation(out=tmpE[:, 4 * j:4 * j + 4, :], in_=tmpE[:, 4 * j:4 * j + 4, :],
                                            func=AF.Exp), r=[f"tmpE{j}"], w=[f"tmpE{j}"])
        for g in range(4):
            mm1(psb[5][:, g * 128:(g + 1) * 128], BT[:, g, :], CT[:, g, :], ["BT", "CT"], ["ps5"])
        for g in range(4):
            dve(lambda e, g=g: e.tensor_tensor(
                out=Wt[:, 4 * g:4 * g + 4, :], in0=tmpE[:, 4 * g:4 * g + 4, :],
                in1=psb[5][:, g * 128:(g + 1) * 128].unsqueeze(1).to_broadcast([128, 4, 128]), op=ALU.mult),
                r=[f"tmpE{g}", "ps5"], w=[f"Wt{g}"])
        for h in range(16):
            bk = 6 + h // 8
            mm1(psb[bk][:, (h % 8) * 64:(h % 8 + 1) * 64], Wt[:, h, :], xdtb[:, h, :], [f"Wt{h // 4}", "xdtb"],
                [f"ps{bk}"])
        act(lambda e: e.activation(out=ea[:], in_=acs[:, 0:16], func=AF.Exp), r=["acs"], w=["ea"])
        dve(lambda e: e.tensor_tensor(out=dcy[:], in0=acs[:, 16:32], in1=acs[:, 0:16], op=ALU.subtract),
            r=["acs"], w=["dcy"])
        act(lambda e: e.activation(out=dcy[:], in_=dcy[:], func=AF.Exp), r=["dcy"], w=["dcy"])
        act(lambda e: e.activation(out=cd[:], in_=acs[:, 16:32], func=AF.Exp), r=["acs"], w=["cd"])
        dve(lambda e: e.tensor_tensor(out=xdd[:], in0=xdt[:], in1=dcy[:].unsqueeze(2).to_broadcast([128, 16, 64]),
                                      op=ALU.mult), r=["xdt", "dcy"], w=["xdd"])
        if not smp:
            for g in range(4):
                bk = 1 + g // 2
                mm1(psb[bk][:, (g % 2) * 256:(g % 2 + 1) * 256], CT[:, g, :], hTb[:, g * 256:(g + 1) * 256],
                    ["CT", "hTb"], [f"ps{bk}"])
            for g in range(4):
                bk = 3 + g // 2
                mm1(psb[bk][:, (g % 2) * 256:(g % 2 + 1) * 256], ub[:, g * 128:(g + 1) * 128],
                    xdd[:, 4 * g:4 * g + 4, :], ["ub", "xdd"], [f"ps{bk}"])
            dve(lambda e: e.tensor_tensor(out=hv(hT[:]), in0=hv(hT[:]),
                                          in1=cd[:].unsqueeze(2).to_broadcast([128, 16, 64]), op=ALU.mult),
                r=["hT", "cd"], w=["hT"])
            for j in range(2):
                dve(lambda e, j=j: e.tensor_tensor(out=hT[:, j * 512:(j + 1) * 512], in0=hT[:, j * 512:(j + 1) * 512],
                                                   in1=psb[3 + j][:], op=ALU.add), r=["hT", f"ps{3 + j}"], w=["hT"])
        else:
            for g in range(4):
                dve(lambda e, g=g: e.tensor_copy(
                    out=CTpad[:, g, :].rearrange("p (b c) -> p b c", c=136)[:, :, 0:8],
                    in_=CT[:, g, :].rearrange("p (b c) -> p b c", c=8)), r=["CT"], w=["CTpad"])
            for b in range(16):
                sp(lambda e, b=b: e.dma_start(out=sst[:], in_=st_ssm[b].rearrange("(j q) n -> q j n", q=128)),
                   w=["sst"])
                for j in range(8):
                    bk = 3 + j // 4
                    pe(lambda e, j=j, bk=bk: e.transpose(out=psb[bk][:, (j % 4) * 128:(j % 4 + 1) * 128],
                                                         in_=sst[:, j, :], identity=T_IDF),
                       r=["sst", "mtab"], w=[f"ps{bk}"])
                for j in range(2):
                    dve(lambda e, j=j: e.tensor_copy(out=hTs[:, j * 512:(j + 1) * 512], in_=psb[3 + j][:]),
                        r=[f"ps{3 + j}"], w=["hTs"])
                    act(lambda e, j=j: e.activation(out=hTsb[:, j * 512:(j + 1) * 512], in_=psb[3 + j][:], func=AF.Copy),
                        r=[f"ps{3 + j}"], w=["hTsb"])
                for g in range(4):
                    bk = 1 + g // 2
                    pe(lambda e, g=g, b=b, bk=bk: e.matmul(
                        psb[bk][:, (g % 2) * 256:(g % 2 + 1) * 256], lhsT=CTpad[:, g, b * 128:(b + 1) * 128],
                        rhs=hTsb[:, g * 256:(g + 1) * 256], start=(b == 0 and g % 2 == 0), stop=(b == 15),
                        skip_group_check=True), r=["CTpad", "hTsb"], w=[f"ps{bk}"])
                dve(lambda e, b=b: e.tensor_scalar(out=xddm[:], in0=xdd[:], scalar1=T_ROWM[:, b:b + 1], scalar2=None,
                                                   op0=ALU.mult), r=["xdd", "mtab"], w=["xddm"])
                for g in range(4):
                    bk = 3 + g // 2
                    mm1(psb[bk][:, (g % 2) * 256:(g % 2 + 1) * 256], ub[:, g * 128:(g + 1) * 128],
                        xddm[:, 4 * g:4 * g + 4, :], ["ub", "xddm"], [f"ps{bk}"])
                dve(lambda e, b=b: e.tensor_tensor(out=hv(hTs[:]), in0=hv(hTs[:]),
                                                   in1=cds[:, b, :].unsqueeze(2).to_broadcast([128, 16, 64]),
                                                   op=ALU.mult), r=["hTs", "cds"], w=["hTs"])
                for j in range(2):
                    dve(lambda e, j=j: e.tensor_tensor(out=hTs[:, j * 512:(j + 1) * 512],
                                                       in0=hTs[:, j * 512:(j + 1) * 512], in1=psb[3 + j][:], op=ALU.add),
                        r=["hTs", f"ps{3 + j}"], w=["hTs"])
                for j in range(8):
                    bk = 3 + j // 4
                    pe(lambda e, j=j, bk=bk: e.transpose(out=psb[bk][:, (j % 4) * 128:(j % 4 + 1) * 128],
                                                         in_=hTs[:, j * 128:(j + 1) * 128], identity=T_IDF),
                       r=["hTs", "mtab"], w=[f"ps{bk}"])
                for j in range(2):
                    dve(lambda e, j=j: e.tensor_copy(out=sso[:, 4 * j:4 * j + 4, :],
                                                     in_=psb[3 + j][:].rearrange("p (a c) -> p a c", a=4)),
                        r=[f"ps{3 + j}"], w=["sso"])
                sp(lambda e, b=b: e.dma_start(out=ssms_out[b].rearrange("(j q) n -> q j n", q=128), in_=sso[:]),
                   r=["sso"], w=[("ssms", b)])
        for j in range(2):
            dve(lambda e, j=j: e.tensor_tensor(
                out=yb[:, 8 * j:8 * j + 8, :], in0=hv(psb[1 + j][:]),
                in1=ea[:, 8 * j:8 * j + 8].unsqueeze(2).to_broadcast([128, 8, 64]), op=ALU.mult),
                r=[f"ps{1 + j}", "ea"], w=[f"yb{j}"])
            dve(lambda e, j=j: e.tensor_tensor(out=yb[:, 8 * j:8 * j + 8, :], in0=yb[:, 8 * j:8 * j + 8, :],
                                               in1=hv(psb[6 + j][:]), op=ALU.add), r=[f"yb{j}", f"ps{6 + j}"],
                w=[f"yb{j}"])
        if not smp:
            act(lambda e: e.activation(out=hTb[:], in_=hT[:], func=AF.Copy), r=["hT"], w=["hTb"])
        pool(lambda e: e.tensor_tensor(out=y2[:], in0=hv(u[:, 0:1024]),
                                       in1=mv[:, 32:48].unsqueeze(2).to_broadcast([128, 16, 64]), op=ALU.mult),
             r=["u0", "mv"], w=["y2"])
        dve(lambda e: e.tensor_tensor(out=yb[:], in0=yb[:], in1=y2[:], op=ALU.add), r=["yb0", "yb1", "y2"],
            w=["yb0", "yb1"])
        act(lambda e: e.activation(out=zt[:], in_=zt[:], func=AF.Silu), r=["zt"], w=["zt"])
        ybf = yb.rearrange("p h d -> p (h d)")
        dve(lambda e: e.tensor_tensor(out=ybf, in0=ybf, in1=zt[:], op=ALU.mult), r=["yb0", "yb1", "zt"],
            w=["yb0", "yb1"])
        pool(lambda e: e.tensor_tensor(out=msq[:], in0=ybf, in1=ybf, op=ALU.mult), r=["yb0", "yb1"], w=["msq"])
        dve(lambda e: e.tensor_reduce(out=mss[:], in_=msq[:].rearrange("p (g c) -> p g c", g=4), axis=AX.X,
                                      op=ALU.add), r=["msq"], w=["mss"])
        dve(lambda e: e.tensor_scalar(out=mss[:], in0=mss[:], scalar1=1.0 / 256, scalar2=EPS, op0=ALU.mult,
                                      op1=ALU.add), r=["mss"], w=["mss"])
        act(lambda e: e.activation(out=mss[:], in_=mss[:], func=AF.Sqrt), r=["mss"], w=["mss"])
        dve(lambda e: e.reciprocal(out=mss[:], in_=mss[:]), r=["mss"], w=["mss"])
        dve(lambda e: e.tensor_tensor(out=msq[:].rearrange("p (g c) -> p g c", g=4),
                                      in0=ybf.rearrange("p (g c) -> p g c", g=4),
                                      in1=mss[:].unsqueeze(2).to_broadcast([128, 4, 256]), op=ALU.mult),
            r=["yb0", "yb1", "mss"], w=["msq"])
        dve(lambda e: e.tensor_tensor(out=Mb[:], in0=msq[:], in1=gs[:], op=ALU.mult), r=["msq", "gs"], w=["Mb"])
        sp(lambda e, r0=r0: e.dma_start(out=Mscr[r0:r0 + 128, :], in_=Mb[:]), r=["Mb"], w=[("Mscr", t)])
        if t == NT - 2:
            for j in range(8):
                bk = 3 + j // 4
                pe(lambda e, j=j, bk=bk: e.transpose(out=psb[bk][:, (j % 4) * 128:(j % 4 + 1) * 128],
                                                     in_=hT[:, j * 128:(j + 1) * 128], identity=T_IDF),
                   r=["hT", "mtab"], w=[f"ps{bk}"])
            for j in range(2):
                dve(lambda e, j=j: e.tensor_copy(out=sso[:, 4 * j:4 * j + 4, :],
                                                 in_=psb[3 + j][:].rearrange("p (a c) -> p a c", a=4)),
                    r=[f"ps{3 + j}"], w=["sso"])
            sp(lambda e: e.dma_start(out=ssmp_out.rearrange("(j q) n -> q j n", q=128), in_=sso[:]),
               r=["sso"], w=["ssmp"])


    B.release(m0)
    m4 = B.mark()
    atab = B.sb("atab", [128, AT_W])
    sp(lambda e: e.dma_start(out=atab[:], in_=atab_in), w=["atab"])
    o_ = [0]

    def tab(w):
        a = atab[:, o_[0]:o_[0] + w]
        o_[0] += w
        return a
    A_GQ = tab(1024)
    A_GK0 = tab(128)
    A_COVER = tab(32)
    A_BASEC = tab(2048).rearrange("p (k c) -> p k c", k=2)
    A_TC = tab(128)
    A_BASE = tab(2048).rearrange("p (k c) -> p k c", k=2)
    A_BASEK = tab(2048).rearrange("p (k c) -> p k c", k=2)
    A_BASEA = tab(2048).rearrange("p (k c) -> p k c", k=2)
    A_CVEC = tab(65 * 16).rearrange("p (d h) -> p d h", h=16)
    A_FT = tab(62)
    A_UT = tab(62)
    A_PET = tab(64).rearrange("p (k j) -> p k j", k=2)
    assert o_[0] == AT_W, o_[0]
    EX = B.sb("EX", [32, 16, 128], BF16)
    sp(lambda e: e.dma_start(out=EX, in_=ex_in), w=["EX"])
    W1 = B.sb("W1", [128, 2, 32, 128], BF16)
    for kv in range(2):
        S.dma("pool", lambda e, kv=kv: e.dma_start(out=W1[:, kv], in_=w1_in[:, kv]), writes=[f"W1{kv}"])
    W2 = B.sb("W2", [128, 2, 64], BF16)
    S.dma("pool", lambda e: e.dma_start(out=W2[:], in_=w2_in), writes=["W2"])
    peTb = B.sb("peTb", [128, 2, 32], BF16)
    dve(lambda e: e.tensor_copy(out=peTb[:], in_=A_PET), r=["atab"], w=["peTb"])

    qT = B.sb("qT", [128, 16, 8, 128], BF16)
    ckT = B.sb("ckT", [128, TP], BF16)
    cvT = B.sb("cvT", [128, TP], BF16)
    skT = B.sb("skT", [128, TP], BF16)
    wkT = B.sb("wkT", [128, TP], BF16)
    svA = B.sb("svA", [128, 16, 2, 65], BF16)
    wvA = B.sb("wvA", [128, 16, 2, 65], BF16)
    gates = B.sb("gates", [128, 16, 48])
    qf = B.sb("qf", [128, 1024])
    qb = B.sb("qb", [128, 1024], BF16)
    kvf = B.sb("kvf", [128, 768])
    kvb = B.sb("kvb", [128, 768], BF16)
    q_sq = B.sb("q_sq", [128, 1024])
    q_ss = B.sb("q_ss", [128, 16])
    q_tmp = B.sb("q_tmp", [128, 1024])
    dve(lambda e: e.memset(svA[:], 1.0), w=["svA"])
    dve(lambda e: e.memset(wvA[:], 1.0), w=["wvA"])

    for t in range(16):
        r0 = t * 128
        sp(lambda e, r0=r0: e.dma_start(out=qf[:], in_=proj[r0:r0 + 128, C_Q:C_Q + 1024]), w=["qf"])
        sp(lambda e, r0=r0: e.dma_start(out=kvf[:], in_=kvn[r0:r0 + 128, :]), w=["kvf"])
        sp(lambda e, r0=r0, t=t: e.dma_start(out=gates[:, t, :], in_=proj[r0:r0 + 128, C_AG:C_AG + 48]),
           w=[("gates", t)])
        act(lambda e, t=t: e.activation(out=gates[:, t, :], in_=gates[:, t, :], func=AF.Sigmoid),
            r=[("gates", t)], w=[("gates", t)])
        rms_heads(qf[:], A_GQ, 16, 0.125, ["qf", "atab"], "qf", q_sq, q_ss, q_tmp)
        act(lambda e: e.activation(out=qb[:].rearrange("p (g k d) -> p k g d", g=8, k=2),
                                   in_=qf[:].rearrange("p (k g d) -> p k g d", k=2, g=8), func=AF.Copy),
            r=["qf"], w=["qb"])
        act(lambda e: e.activation(out=kvb[:], in_=kvf[:], func=AF.Copy), r=["kvf"], w=["kvb"])
        dve(lambda e, t=t: e.tensor_copy(out=svA[:, t, :, 0:64], in_=kvb[:, 384:512].rearrange("p (k d) -> p k d", k=2)),
            r=["kvb"], w=["svA"])
        dve(lambda e, t=t: e.tensor_copy(out=wvA[:, t, :, 0:64], in_=kvb[:, 640:768].rearrange("p (k d) -> p k d", k=2)),
            r=["kvb"], w=["wvA"])
        pq = bank_bf(0)
        for g in range(8):
            src = qb[:, g * 128:(g + 1) * 128]
            pe(lambda e, g=g, src=src, pq=pq: e.transpose(out=pq[:, g * 128:(g + 1) * 128], in_=src, identity=ident[:]),
               r=["qb", "ident"], w=["ps0"])
        dve(lambda e, pq=pq, t=t: e.tensor_copy(out=qT[:, t, :, :], in_=pq.rearrange("p (g c) -> p g c", g=8)),
            r=["ps0"], w=[("qT", t)])
        pk = bank_bf(1)
        for n_, c0 in enumerate((0, 128, 256, 512)):
            pe(lambda e, n_=n_, c0=c0, pk=pk: e.transpose(out=pk[:, n_ * 128:(n_ + 1) * 128], in_=kvb[:, c0:c0 + 128],
                                                          identity=ident[:]), r=["kvb", "ident"], w=["ps1"])
        for n_, dst in enumerate((ckT, cvT, skT, wkT)):
            act(lambda e, n_=n_, dst=dst, pk=pk, r0=r0: e.activation(out=dst[:, r0:r0 + 128],
                                                                  in_=pk[:, n_ * 128:(n_ + 1) * 128], func=AF.Copy),
                r=["ps1"], w=[("kT", n_, t)])
    kT_keys = lambda n_: [("kT", n_, t) for t in range(16)]

    gT = B.sb("gT", [128, 4, 128], BF16)
    xb_ = B.sb("xb_", [128, 512])
    x3_ = B.sb("x3_", [128, 512])
    peb = B.sb("peb", [128, 2])
    kcf = B.sb("kcf", [128, 256])
    kcb = B.sb("kcb", [128, 128], BF16)
    kcT = B.sb("kcT", [128, 128], BF16)
    vcA = B.sb("vcA", [128, 2, 97], BF16)
    dve(lambda e: e.memset(vcA[:], 1.0), w=["vcA"])
    dve(lambda e: e.memset(gT[:], 0.0), w=["gT"])
    for kv in range(2):
        for j in range(32):
            pe(lambda e, kv=kv, j=j: e.matmul(psb[2][:, kv:kv + 1], lhsT=W1[0:64, kv, j, :], rhs=peTb[0:64, kv, j:j + 1],
                                              start=(j == 0), stop=(j == 31)), r=[f"W1{kv}", "peTb"], w=["ps2"])
    dve(lambda e: e.tensor_copy(out=peb[:], in_=psb[2][:, 0:2]), r=["ps2"], w=["peb"])
    for kv, srcT in enumerate((ckT, cvT)):
        for kvh in range(2):
            grp = kv * 2 + kvh
            rows = slice(kvh * 64, (kvh + 1) * 64)
            sv_ = srcT[:].rearrange("p (n s) -> p n s", s=16)
            for j in range(32):
                pe(lambda e, kv=kv, j=j, grp=grp, rows=rows, sv_=sv_: e.matmul(
                    psb[3][:, grp * 127:(grp + 1) * 127], lhsT=W1[rows, kv, j, :],
                    rhs=sv_[rows, (j // 16):(j // 16) + 127, j % 16], start=(j == 0), stop=(j == 31)),
                    r=[f"W1{kv}"] + kT_keys(kv), w=["ps3"])
            act(lambda e, grp=grp, kv=kv: e.activation(out=xb_[:, grp * 127:(grp + 1) * 127],
                                                       in_=psb[3][:, grp * 127:(grp + 1) * 127], func=AF.Identity,
                                                       bias=peb[:, kv:kv + 1]), r=["ps3", "peb"], w=["xb_"])
    dve(lambda e: e.tensor_tensor(out=x3_[:, 0:508], in0=xb_[:, 0:508], in1=xb_[:, 0:508], op=ALU.mult), r=["xb_"], w=["x3_"])
    dve(lambda e: e.tensor_tensor(out=x3_[:, 0:508], in0=x3_[:, 0:508], in1=xb_[:, 0:508], op=ALU.mult), r=["x3_", "xb_"], w=["x3_"])
    dve(lambda e: e.scalar_tensor_tensor(out=x3_[:, 0:508], in0=x3_[:, 0:508], scalar=0.044715, in1=xb_[:, 0:508],
                                         op0=ALU.mult, op1=ALU.add), r=["x3_", "xb_"], w=["x3_"])
    act(lambda e: e.activation(out=x3_[:, 0:508], in_=x3_[:, 0:508], func=AF.Sigmoid, scale=1.5957691216057308),
        r=["x3_"], w=["x3_"])
    dve(lambda e: e.tensor_tensor(out=gT[:, :, 0:127], in0=xb_[:, 0:508].rearrange("p (g n) -> p g n", g=4),
                                  in1=x3_[:, 0:508].rearrange("p (g n) -> p g n", g=4), op=ALU.mult),
        r=["x3_", "xb_"], w=["gT"])
    for kv in range(2):
        for kvh in range(2):
            grp = kv * 2 + kvh
            mm1(psb[2][0:127, grp * 64:(grp + 1) * 64], gT[:, grp, 0:127], W2[:, kv, :], ["gT", "W2"], ["ps2"])
    dve(lambda e: e.memset(kcf[:], 0.0), w=["kcf"])
    dve(lambda e: e.tensor_copy(out=kcf[0:127, :], in_=psb[2][0:127, 0:256]), r=["ps2"], w=["kcf"])
    rms_heads(kcf[:, 0:128], A_GK0, 2, 1.0, ["kcf", "atab"], "kcf", q_sq, q_ss, q_tmp)
    act(lambda e: e.activation(out=kcb[:], in_=kcf[:, 0:128], func=AF.Copy), r=["kcf"], w=["kcb"])
    pe(lambda e: e.transpose(out=bank_bf(2)[:, 0:128], in_=kcb[:], identity=ident[:]), r=["kcb", "ident"], w=["ps2"])
    dve(lambda e: e.tensor_copy(out=kcT[:], in_=bank_bf(2)[:, 0:128]), r=["ps2"], w=["kcT"])
    dve(lambda e: e.tensor_copy(out=vcA[:, :, 0:64], in_=kcf[:, 128:256].rearrange("p (k d) -> p k d", k=2)),
        r=["kcf"], w=["vcA"])
    for kvh in range(2):
        dve(lambda e, kvh=kvh: e.tensor_copy(out=vcA[:, kvh, 65:97], in_=A_COVER), r=["atab"], w=["vcA"])

    S.barrier()
    tmps = [B.sb(f"tmps{i}", [128, 1024]) for i in range(2)]
    pexp = [B.sb(f"pexp{i}", [128, 8, 128], BF16) for i in range(2)]
    mk = B.sb("mk", [128, 128])
    Aacc = B.sb("Aacc", [128, 1024])
    Ab = B.sb("Ab", [128, 1024], BF16)
    rd = B.sb("rd", [128, 8])
    gsc = B.sb("gsc", [128, 8])
    impt = B.sb("impt", [128, 8, 32])
    imp = B.sb("imp", [128, 32])
    cmpb = B.sb("cmpb", [128, 32, 32])
    cnt = B.sb("cnt", [128, 32])
    nsel = B.sb("nsel", [128, 32], BF16)
    nsTg = B.sb("nsTg", [32, 8, 128], BF16)
    otmp = B.sb("otmp", [128, 8, 64])
    it_ = [0]

    def attn_block(i, kvh, keysT, nk, Vt, nv, base_ap, delta, out_banks, first, extra=None, selmask=None):
        rows = slice(kvh * 64, (kvh + 1) * 64)
        sb_ = it_[0] % 2
        it_[0] += 1
        sbanks = (0, 1) if sb_ == 0 else (2, 3)
        for hb in range(2):
            bk = sbanks[hb]
            pe(lambda e, bk=bk, hb=hb: e.matmul(
                psb[bk][0:nk, :], lhsT=keysT, rhs=qT[rows, i, 4 * hb:4 * hb + 4, :],
                start=True, stop=(selmask is None)), r=["kT_any", ("qT", i)], w=[f"ps{bk}"])
            if selmask is not None:
                pe(lambda e, bk=bk, hb=hb: e.matmul(
                    psb[bk][0:nk, :], lhsT=selmask, rhs=nsTg[:, 4 * hb:4 * hb + 4, :], start=False, stop=True),
                    r=["EX", "nsTg"], w=[f"ps{bk}"])
        tm = tmps[sb_]
        px = pexp[sb_]
        dve(lambda e: e.tensor_tensor(out=tm[0:nk, :], in0=psall[0:nk, sbanks[0] * 512:sbanks[0] * 512 + 1024],
                                      in1=base_ap, op=ALU.add),
            r=[f"ps{sbanks[0]}", f"ps{sbanks[1]}", "atab"], w=[f"tmps{sb_}"])
        if extra is not None:
            pool(lambda e: e.tensor_tensor(out=tm[0:nk, :].rearrange("p (g c) -> p g c", g=8),
                                           in0=tm[0:nk, :].rearrange("p (g c) -> p g c", g=8),
                                           in1=extra[0:nk, :].unsqueeze(1).to_broadcast([nk, 8, 128]), op=ALU.add),
                 r=[f"tmps{sb_}", "mk"], w=[f"tmps{sb_}"])
        for g in range(8):
            act(lambda e, g=g: e.activation(out=px[0:nk, g, :], in_=tm[0:nk, g * 128:(g + 1) * 128], func=AF.Exp,
                                            bias=A_CVEC[0:nk, delta, kvh * 8 + g:kvh * 8 + g + 1]),
                r=[f"tmps{sb_}", "atab"], w=[f"pexp{sb_}"])
        for g in range(8):
            bk = out_banks[g // 4]
            pe(lambda e, g=g, bk=bk: e.matmul(
                psb[bk][:, (g % 4) * nv:(g % 4 + 1) * nv], lhsT=px[0:nk, g, :], rhs=Vt,
                start=(first and g % 4 == 0), stop=False, skip_group_check=True),
                r=[f"pexp{sb_}", "V_any"], w=[f"ps{bk}"])

    def finish_branch(i, kvh, out_banks, nv, br, accumulate):
        for hb in range(2):
            bk = out_banks[hb]
            ov = psb[bk][:, 0:4 * nv].rearrange("p (g c) -> p g c", g=4)
            dve(lambda e, hb=hb, ov=ov: e.tensor_scalar(out=rd[:, 4 * hb:4 * hb + 4], in0=ov[:, :, 64], scalar1=1e-30,
                                                        scalar2=None, op0=ALU.max), r=[f"ps{bk}"], w=["rd"])
        dve(lambda e: e.reciprocal(out=rd[:], in_=rd[:]), r=["rd"], w=["rd"])
        gv = gates[:, i, kvh * 24:(kvh + 1) * 24].rearrange("p (g b) -> p g b", b=3)[:, :, br]
        dve(lambda e: e.tensor_tensor(out=gsc[:], in0=rd[:], in1=gv, op=ALU.mult), r=["rd", ("gates", i)], w=["gsc"])
        for hb in range(2):
            bk = out_banks[hb]
            ov = psb[bk][:, 0:4 * nv].rearrange("p (g c) -> p g c", g=4)
            dst = Aacc[:, kvh * 512 + hb * 256:kvh * 512 + (hb + 1) * 256].rearrange("p (g d) -> p g d", g=4)
            if not accumulate:
                dve(lambda e, ov=ov, dst=dst, hb=hb: e.tensor_tensor(
                    out=dst, in0=ov[:, :, 0:64], in1=gsc[:, 4 * hb:4 * hb + 4].unsqueeze(2).to_broadcast([128, 4, 64]),
                    op=ALU.mult), r=[f"ps{bk}", "gsc"], w=["Aacc"])
            else:
                dve(lambda e, ov=ov, hb=hb: e.tensor_tensor(
                    out=otmp[:, 4 * hb:4 * hb + 4, :], in0=ov[:, :, 0:64],
                    in1=gsc[:, 4 * hb:4 * hb + 4].unsqueeze(2).to_broadcast([128, 4, 64]), op=ALU.mult),
                    r=[f"ps{bk}", "gsc"], w=["otmp"])
                pool(lambda e, dst=dst, hb=hb: e.tensor_tensor(out=dst, in0=dst, in1=otmp[:, 4 * hb:4 * hb + 4, :],
                                                               op=ALU.add), r=["otmp", "Aacc"], w=["Aacc"])

    for i in ([] if "nsap" in DEV_SKIP else range(16)):
        pool(lambda e, i=i: e.tensor_scalar(out=mk[:], in0=A_TC, scalar1=float(128 * i), scalar2=-30000.0,
                                            op0=ALU.is_gt, op1=ALU.mult), r=["atab"], w=["mk"])
        for kvh in range(2):
            rows = slice(kvh * 64, (kvh + 1) * 64)
            attn_block(i, kvh, kcT[rows, 0:127], 127, vcA[0:127, kvh, :], 97, A_BASEC[0:127, kvh, :], i, (4, 5), True,
                       extra=mk)
            for hb in range(2):
                bk = 4 + hb
                ov = psb[bk][:, 0:388].rearrange("p (g c) -> p g c", g=4)
                dve(lambda e, hb=hb, ov=ov: e.tensor_scalar(out=rd[:, 4 * hb:4 * hb + 4], in0=ov[:, :, 64],
                                                            scalar1=1e-30, scalar2=None, op0=ALU.max),
                    r=[f"ps{bk}"], w=["rd"])
            dve(lambda e: e.reciprocal(out=rd[:], in_=rd[:]), r=["rd"], w=["rd"])
            for hb in range(2):
                bk = 4 + hb
                ov = psb[bk][:, 0:388].rearrange("p (g c) -> p g c", g=4)
                dve(lambda e, hb=hb, ov=ov: e.tensor_tensor(
                    out=impt[:, 4 * hb:4 * hb + 4, :], in0=ov[:, :, 65:97],
                    in1=rd[:, 4 * hb:4 * hb + 4].unsqueeze(2).to_broadcast([128, 4, 32]), op=ALU.mult),
                    r=[f"ps{bk}", "rd"], w=["impt"])
            dve(lambda e: e.tensor_reduce(out=imp[:], in_=impt[:].rearrange("p g j -> p j g"), axis=AX.X, op=ALU.add),
                r=["impt"], w=["imp"])
            finish_branch(i, kvh, (4, 5), 97, 0, False)
            dve(lambda e, i=i: e.tensor_tensor(out=imp[:], in0=imp[:], in1=A_FT[:, 30 - 2 * i:62 - 2 * i], op=ALU.max),
                r=["imp", "atab"], w=["imp"])
            dve(lambda e: e.memset(imp[:, 0:1], 1e30), r=["imp"], w=["imp"])
            dve(lambda e, i=i: e.tensor_tensor(out=imp[:], in0=imp[:], in1=A_UT[:, 30 - 2 * i:62 - 2 * i], op=ALU.min),
                r=["imp", "atab"], w=["imp"])
            dve(lambda e: e.tensor_tensor(out=cmpb[:], in0=imp[:].unsqueeze(1).to_broadcast([128, 32, 32]),
                                          in1=imp[:].unsqueeze(2).to_broadcast([128, 32, 32]), op=ALU.is_gt),
                r=["imp"], w=["cmpb"])
            dve(lambda e: e.tensor_reduce(out=cnt[:], in_=cmpb[:], axis=AX.X, op=ALU.add), r=["cmpb"], w=["cnt"])
            dve(lambda e: e.tensor_scalar(out=nsel[:], in0=cnt[:], scalar1=15.5, scalar2=-30000.0, op0=ALU.is_gt,
                                          op1=ALU.mult), r=["cnt"], w=["nsel"])
            pe(lambda e: e.transpose(out=bank_bf(6)[0:32, 0:128], in_=nsel[:], identity=ident[:]),
               r=["nsel", "ident"], w=["ps6"])
            dve(lambda e: e.tensor_copy(out=nsTg[:], in_=bank_bf(6)[0:32, 0:128].unsqueeze(1).to_broadcast([32, 8, 128])),
                r=["ps6"], w=["nsTg"])
            for kt in range(i + 1):
                attn_block(i, kvh, skT[rows, kt * 128:(kt + 1) * 128], 128, svA[:, kt, kvh, :], 65,
                           (A_BASEK if kt == i else A_BASE)[:, kvh, :], i - kt, (6, 7), kt == 0,
                           selmask=EX[:, kt, :])
            finish_branch(i, kvh, (6, 7), 65, 1, True)
            k0 = max(0, i - 4)
            for kt in range(k0, i + 1):
                bs = A_BASEK if kt == i else (A_BASEA if kt == i - 4 else A_BASE)
                attn_block(i, kvh, wkT[rows, kt * 128:(kt + 1) * 128], 128, wvA[:, kt, kvh, :], 65,
                           bs[:, kvh, :], i - kt, (4, 5), kt == k0)
            finish_branch(i, kvh, (4, 5), 65, 2, True)
        act(lambda e: e.activation(out=Ab[:], in_=Aacc[:], func=AF.Copy), r=["Aacc"], w=["Ab"])
        sp(lambda e, i=i: e.dma_start(out=Ascr[i * 128:(i + 1) * 128, :], in_=Ab[:]), r=["Ab"], w=[("Ascr", i)])
    B.release(m4)


    m4s = B.mark()
    stab = B.sb("stab", [128, ST_W])
    sp(lambda e: e.dma_start(out=stab[:], in_=stab_in), w=["stab"])
    o_[0] = 0

    def stb(w):
        a = stab[:, o_[0]:o_[0] + w]
        o_[0] += w
        return a
    S_GQ = stb(1024)
    S_GK0 = stb(512)
    S_BIASC = stb(512)
    S_COVER = stb(516).rearrange("p (n j) -> p n j", n=4)
    S_BASE2 = stb(128).rearrange("p (k c) -> p k c", k=2)
    S_CVEC2 = stb(1040).rearrange("p (t k g) -> p t k g", t=65, k=2)
    S_BIASW = stb(640).rearrange("p (t k c) -> p t k c", t=5, k=2)
    S_FS = stb(129)
    S_PET = stb(64).rearrange("p (k j) -> p k j", k=2)
    S_PIOTA = stb(1)
    assert o_[0] <= ST_W, o_[0]
    EXW = B.sb("EXW", [128, 32, 128], BF16)
    sp(lambda e: e.dma_start(out=EXW, in_=exw_in), w=["EXW"])
    W1s = B.sb("W1s", [128, 2, 32, 128], BF16)
    for kv in range(2):
        S.dma("pool", lambda e, kv=kv: e.dma_start(out=W1s[:, kv], in_=w1_in[:, kv]), writes=[f"W1s{kv}"])
    W2s = B.sb("W2s", [128, 2, 64], BF16)
    S.dma("pool", lambda e: e.dma_start(out=W2s[:], in_=w2_in), writes=["W2s"])
    peTs = B.sb("peTs", [128, 2, 32], BF16)
    dve(lambda e: e.tensor_copy(out=peTs[:], in_=S_PET), r=["stab"], w=["peTs"])
    pebs = B.sb("pebs", [128, 2])
    for kv in range(2):
        for j in range(32):
            pe(lambda e, kv=kv, j=j: e.matmul(psb[2][:, kv:kv + 1], lhsT=W1s[0:64, kv, j, :], rhs=peTs[0:64, kv, j:j + 1],
                                              start=(j == 0), stop=(j == 31)), r=[f"W1s{kv}", "peTs"], w=["ps2"])
    dve(lambda e: e.tensor_copy(out=pebs[:], in_=psb[2][:, 0:2]), r=["ps2"], w=["pebs"])

    xbs = B.sb("xbs", [128, 4, 512])
    x3s = B.sb("x3s", [128, 4, 512])
    s_tmp_early = x3s[:, 2:4, :].rearrange("p a n -> p (a n)")
    pti = B.sb("pti", [128, 1024], I32)
    idx = pti
    sp(lambda e: e.dma_start(out=pti[:], in_=pt_in), w=["pti"])
    ptf = s_tmp_early
    dve(lambda e: e.tensor_copy(out=ptf[:], in_=pti[:]), r=["pti"], w=["ptf"])
    dve(lambda e: e.tensor_scalar(out=ptf[:], in0=ptf[:], scalar1=128.0, scalar2=S_PIOTA, op0=ALU.mult, op1=ALU.add),
        r=["ptf", "stab"], w=["ptf"])
    dve(lambda e: e.tensor_copy(out=idx[:], in_=ptf[:]), r=["ptf"], w=["pti"])

    qfs = xbs[:, 0:2, :].rearrange("p a n -> p (a n)")
    kvfs = xbs[:, 2:4, :].rearrange("p a n -> p (a n)")[:, 0:768]
    s_sq = x3s[:, 0:2, :].rearrange("p a n -> p (a n)")
    s_tmp = x3s[:, 2:4, :].rearrange("p a n -> p (a n)")
    qbs = B.sb("qbs", [128, 1024], BF16)
    s_ss = B.sb("s_ss", [128, 16])
    qTs = B.sb("qTs", [128, 8, 128], BF16)
    qTs2 = B.sb("qTs2", [128, 16, 64], BF16)
    kvbs = B.sb("kvbs", [128, 768], BF16)
    newT = B.sb("newT", [128, 4, 128], BF16)
    sp(lambda e: e.dma_start(out=qfs, in_=proj[TP:TT, C_Q:C_Q + 1024]), w=["qfs"])
    sp(lambda e: e.dma_start(out=kvfs, in_=kvn[TP:TT, :]), w=["kvfs"])
    rms_heads(qfs, S_GQ, 16, 0.125, ["qfs", "stab"], "qfs", s_sq, s_ss, s_tmp)
    act(lambda e: e.activation(out=qbs[:].rearrange("p (g k d) -> p k g d", g=8, k=2),
                               in_=qfs.rearrange("p (k g d) -> p k g d", k=2, g=8), func=AF.Copy),
        r=["qfs"], w=["qbs"])
    act(lambda e: e.activation(out=kvbs[:], in_=kvfs, func=AF.Copy), r=["kvfs"], w=["kvbs"])
    pq = bank_bf(0)
    for g in range(8):
        pe(lambda e, g=g, pq=pq: e.transpose(out=pq[:, g * 128:(g + 1) * 128], in_=qbs[:, g * 128:(g + 1) * 128],
                                             identity=ident[:]), r=["qbs", "ident"], w=["ps0"])
    dve(lambda e, pq=pq: e.tensor_copy(out=qTs[:], in_=pq.rearrange("p (g c) -> p g c", g=8)), r=["ps0"], w=["qTs"])
    dve(lambda e: e.tensor_copy(out=qTs2[:].rearrange("p b (g t) -> p b g t", g=8),
                                in_=qTs[:].rearrange("p g (b t) -> p b g t", b=16)), r=["qTs"], w=["qTs2"])
    pk = bank_bf(1)
    for n_, c0 in enumerate((0, 128, 256, 512)):
        pe(lambda e, n_=n_, c0=c0, pk=pk: e.transpose(out=pk[:, n_ * 128:(n_ + 1) * 128], in_=kvbs[:, c0:c0 + 128],
                                                      identity=ident[:]), r=["kvbs", "ident"], w=["ps1"])
    dve(lambda e, pk=pk: e.tensor_copy(out=newT[:], in_=pk[:, 0:512].rearrange("p (n c) -> p n c", n=4)),
        r=["ps1"], w=["newT"])

    kT3 = B.sb("kT3", [128, 3, 8320], BF16)
    svAs = B.sb("svAs", [128, 65, 2, 65], BF16)
    dve(lambda e: e.memset(svAs[:], 1.0), w=["svAs"])
    pgf = [B.sb(f"pgf{i}", [128, 512]) for i in range(2)]
    pgb = [B.sb(f"pgb{i}", [128, 512], BF16) for i in range(2)]
    st8 = B.sb("st8", [8, 768 + 48])
    cwf = B.sb("cwf", [128, 4, 256])
    cwb = B.sb("cwb", [128, 4, 256], BF16)
    wkTs = B.sb("wkTs", [128, 640], BF16)
    wvAs = B.sb("wvAs", [128, 5, 2, 65], BF16)
    dve(lambda e: e.memset(wvAs[:], 1.0), w=["wvAs"])
    gTs = B.sb("gTs", [128, 4, 512], BF16)
    kcfs = B.sb("kcfs", [128, 4, 128])
    kcbs = B.sb("kcbs", [128, 4, 128], BF16)
    kcTs = B.sb("kcTs", [128, 512], BF16)
    vcAs = B.sb("vcAs", [128, 4, 2, 194], BF16)
    dve(lambda e: e.memset(vcAs[:], 1.0), w=["vcAs"])
    for kvh in range(2):
        dve(lambda e, kvh=kvh: e.tensor_copy(out=vcAs[:, :, kvh, 65:194], in_=S_COVER), r=["stab"], w=["vcAs"])
    S.barrier()
    dve(lambda e: e.memset(xbs[:], 0.0), w=["xbs"])
    tmpS = [B.sb(f"tmpS{i}", [128, 512]) for i in range(2)]
    pS = [B.sb(f"pS{i}", [128, 512], BF16) for i in range(2)]
    gts = B.sb("gts", [8, 48])
    rds = B.sb("rds", [8, 16])
    gss = B.sb("gss", [8, 16])
    imps = B.sb("imps", [8, 8, 129])
    impv = B.sb("impv", [8, 129])
    cmps = B.sb("cmps", [8, 43, 129], BF16)
    cnts = B.sb("cnts", [8, 129])
    nsels = B.sb("nsels", [8, 136], BF16)
    nselW = B.sb("nselW", [8, 2, 2, 64], BF16)
    nsTs = B.sb("nsTs", [128, 2, 8, 8], BF16)
    As = B.sb("As", [8, 1024])
    Abs_ = B.sb("Abs_", [8, 1024], BF16)
    ots = B.sb("ots", [8, 16, 64])
    si_ = [0]

    def s_scores(keysT_fn, nkt, kt0, kvh, b, nk, selmask):
        rows = slice(kvh * 64, (kvh + 1) * 64)
        bk = si_[0] % 2
        for q_ in range(nkt):
            kt = kt0 + q_
            pe(lambda e, q_=q_, kt=kt, bk=bk: e.matmul(
                psb[bk][0:nk, q_ * 64:(q_ + 1) * 64], lhsT=keysT_fn(rows, kt), rhs=qTs2[rows, b, :],
                start=True, stop=not (selmask and kt < 64 and "s_mask" not in DEV_SKIP)), r=["keys_any", "qTs2"], w=[f"ps{bk}"])
            if selmask and kt < 64 and "s_mask" not in DEV_SKIP:
                lt, rt = EXW[rows, kt % 32, :], nsTs[rows, kt // 32, :, :]
                pe(lambda e, q_=q_, bk=bk, lt=lt, rt=rt: e.matmul(
                    psb[bk][0:nk, q_ * 64:(q_ + 1) * 64], lhsT=lt, rhs=rt, start=False, stop=True),
                    r=["EXW", "nsTs"], w=[f"ps{bk}"])
        return bk

    def s_pv(bk_unused, px, nkt, kt0, kvh, nk, Vfn, nv, acc_banks, hpb, first_kt):
        for q_ in ([] if "s_pvsel" in DEV_SKIP else range(nkt)):
            kt = kt0 + q_
            for g in range(8):
                hh = kvh * 8 + g if len(acc_banks) * hpb >= 16 else g
                bk = acc_banks[hh // hpb]
                pe(lambda e, q_=q_, kt=kt, g=g, bk=bk, hh=hh: e.matmul(
                    psb[bk][0:8, (hh % hpb) * nv:(hh % hpb + 1) * nv], lhsT=px[0:nk, q_ * 64 + g * 8:q_ * 64 + g * 8 + 8],
                    rhs=Vfn(kt, kvh), start=(kt == first_kt and hh % hpb == 0), stop=False, skip_group_check=True),
                    r=["px_any", "V_any"], w=[f"ps{bk}"])

    for b in ([] if "nsas" in DEV_SKIP else range(DEV_NSEQ)):
        S.barrier()
        for pg in ([] if "s_gather" in DEV_SKIP else range(64)):
            fb = pg % 2
            S.dma("pool", lambda e, b=b, pg=pg, fb=fb: e.indirect_dma_start(
                out=pgf[fb][:], out_offset=None, in_=ckv_in[:, :],
                in_offset=bass.IndirectOffsetOnAxis(ap=idx[:, b * 64 + pg:b * 64 + pg + 1], axis=0)),
                reads=["pti"], writes=[f"pgf{fb}"])
            cb = pg % 2
            act(lambda e, fb=fb, cb=cb: e.activation(out=pgb[cb][:], in_=pgf[fb][:], func=AF.Copy),
                r=[f"pgf{fb}"], w=[f"pgb{cb}"])
            pool(lambda e, pg=pg, cb=cb: e.tensor_copy(out=svAs[:, pg, :, 0:64],
                                                       in_=pgb[cb][:, 384:512].rearrange("p (k d) -> p k d", k=2)),
                 r=[f"pgb{cb}"], w=["svAs"])
            tb = 2 + pg % 2
            pt_ = bank_bf(tb)
            for n_ in range(3):
                pe(lambda e, n_=n_, cb=cb, pt_=pt_: e.transpose(out=pt_[:, n_ * 128:(n_ + 1) * 128],
                                                               in_=pgb[cb][:, n_ * 128:(n_ + 1) * 128], identity=ident[:]),
                   r=[f"pgb{cb}", "ident"], w=[f"ps{tb}"])
            dve(lambda e, pg=pg, pt_=pt_: e.tensor_copy(out=kT3[:, :, pg * 128:(pg + 1) * 128],
                                                        in_=pt_[:, 0:384].rearrange("p (n c) -> p n c", n=3)),
                r=[f"ps{tb}"], w=["kT3"])
        dve(lambda e, b=b: e.tensor_copy(out=kT3[:, :, 8192:8200], in_=newT[:, 0:3, b * 8:(b + 1) * 8]),
            r=["newT"], w=["kT3"])
        sp(lambda e, b=b: e.dma_start(out=st8[:, 0:768], in_=kvn[TP + 8 * b:TP + 8 * b + 8, :]), w=["st8"])
        sp(lambda e, b=b: e.dma_start(out=st8[:, 768:816], in_=proj[TP + 8 * b:TP + 8 * b + 8, C_AG:C_AG + 48]), w=["st8"])
        dve(lambda e: e.tensor_copy(out=svAs[0:8, 64, :, 0:64], in_=st8[:, 384:512].rearrange("p (k d) -> p k d", k=2)),
            r=["st8"], w=["svAs"])
        dve(lambda e: e.tensor_copy(out=wvAs[0:8, 4, :, 0:64], in_=st8[:, 640:768].rearrange("p (k d) -> p k d", k=2)),
            r=["st8"], w=["wvAs"])
        act(lambda e: e.activation(out=gts[:], in_=st8[:, 768:816], func=AF.Sigmoid), r=["st8"], w=["gts"])
        sp(lambda e, b=b: e.dma_start(out=cwf[:], in_=cwin[b].rearrange("(n p) c -> p n c", p=128)), w=["cwf"])
        act(lambda e: e.activation(out=cwb[:], in_=cwf[:], func=AF.Copy), r=["cwf"], w=["cwb"])
        dve(lambda e: e.tensor_copy(out=wvAs[:, 0:4, :, 0:64], in_=cwb[:, :, 128:256].rearrange("p n (k d) -> p n k d", k=2)),
            r=["cwb"], w=["wvAs"])
        pw = bank_bf(2)
        for n_ in range(4):
            pe(lambda e, n_=n_, pw=pw: e.transpose(out=pw[:, n_ * 128:(n_ + 1) * 128], in_=cwb[:, n_, 0:128],
                                                   identity=ident[:]), r=["cwb", "ident"], w=["ps2"])
        dve(lambda e, pw=pw: e.tensor_copy(out=wkTs[:, 0:512], in_=pw[:, 0:512]), r=["ps2"], w=["wkTs"])
        dve(lambda e, b=b: e.tensor_copy(out=wkTs[:, 512:520], in_=newT[:, 3, b * 8:(b + 1) * 8]), r=["newT"], w=["wkTs"])
        S.barrier()
        for kv in ([] if "s_cmp" in DEV_SKIP else range(2)):
            sv_ = kT3[:, kv, :].rearrange("p (n s) -> p n s", s=16)
            for kvh in range(2):
                grp = kv * 2 + kvh
                rows = slice(kvh * 64, (kvh + 1) * 64)
                bk = 2 + grp % 2
                for j in range(32):
                    pe(lambda e, kv=kv, j=j, rows=rows, sv_=sv_, bk=bk: e.matmul(
                        psb[bk][:, 0:511], lhsT=W1s[rows, kv, j, :], rhs=sv_[rows, (j // 16):(j // 16) + 511, j % 16],
                        start=(j == 0), stop=(j == 31)), r=[f"W1s{kv}", "kT3"], w=[f"ps{bk}"])
                act(lambda e, grp=grp, kv=kv, bk=bk: e.activation(out=xbs[:, grp, 0:511], in_=psb[bk][:, 0:511],
                                                                  func=AF.Identity, bias=pebs[:, kv:kv + 1]),
                    r=[f"ps{bk}", "pebs"], w=["xbs"])
        dve(lambda e: e.tensor_tensor(out=x3s[:], in0=xbs[:], in1=xbs[:], op=ALU.mult), r=["xbs"], w=["x3s"])
        pool(lambda e: e.tensor_tensor(out=x3s[:], in0=x3s[:], in1=xbs[:], op=ALU.mult), r=["x3s", "xbs"], w=["x3s"])
        dve(lambda e: e.scalar_tensor_tensor(out=x3s[:].rearrange("p g n -> p (g n)"),
                                             in0=x3s[:].rearrange("p g n -> p (g n)"), scalar=0.044715,
                                             in1=xbs[:].rearrange("p g n -> p (g n)"), op0=ALU.mult, op1=ALU.add),
            r=["x3s", "xbs"], w=["x3s"])
        act(lambda e: e.activation(out=x3s[:], in_=x3s[:], func=AF.Sigmoid, scale=1.5957691216057308),
            r=["x3s"], w=["x3s"])
        dve(lambda e: e.tensor_tensor(out=gTs[:], in0=xbs[:], in1=x3s[:], op=ALU.mult), r=["x3s", "xbs"], w=["gTs"])
        for nt in range(4):
            bk = 2 + nt // 2
            for kv in range(2):
                for kvh in range(2):
                    grp = kv * 2 + kvh
                    c0 = (nt % 2) * 256 + grp * 64
                    mm1(psb[bk][:, c0:c0 + 64], gTs[:, grp, nt * 128:(nt + 1) * 128], W2s[:, kv, :], ["gTs", "W2s"],
                        [f"ps{bk}"])
        for nt in range(4):
            bk = 2 + nt // 2
            c0 = (nt % 2) * 256
            dve(lambda e, nt=nt, bk=bk, c0=c0: e.tensor_copy(out=kcfs[:, nt, :], in_=psb[bk][:, c0:c0 + 128]),
                r=[f"ps{bk}"], w=["kcfs"])
            act(lambda e, nt=nt, bk=bk, c0=c0: e.activation(
                out=vcAs[:, nt, :, 0:64], in_=psb[bk][:, c0 + 128:c0 + 256].rearrange("p (k d) -> p k d", k=2),
                func=AF.Copy), r=[f"ps{bk}"], w=["vcAs"])
        rms_heads(kcfs[:].rearrange("p n c -> p (n c)"), S_GK0, 8, 1.0, ["kcfs", "stab"], "kcfs", s_sq, s_ss, s_tmp)
        act(lambda e: e.activation(out=kcbs[:], in_=kcfs[:], func=AF.Copy), r=["kcfs"], w=["kcbs"])
        pc_ = bank_bf(2)
        for nt in range(4):
            pe(lambda e, nt=nt, pc_=pc_: e.transpose(out=pc_[:, nt * 128:(nt + 1) * 128], in_=kcbs[:, nt, :],
                                                     identity=ident[:]), r=["kcbs", "ident"], w=["ps2"])
        dve(lambda e, pc_=pc_: e.tensor_copy(out=kcTs[:], in_=pc_[:, 0:512]), r=["ps2"], w=["kcTs"])
        S.barrier()
        for kvh in ([] if "s_attn" in DEV_SKIP else range(2)):
            si_[0] += 1
            bk = s_scores(lambda rows, kt: kcTs[rows, kt * 128:(kt + 1) * 128], 4, 0, kvh, b, 128, False)
            tm, px = tmpS[si_[0] % 2], pS[si_[0] % 2]
            dve(lambda e, bk=bk, tm=tm, kvh=kvh: e.tensor_tensor(
                out=tm[:, 0:256].rearrange("p (n c) -> p n c", n=4), in0=psb[bk][:, 0:256].rearrange("p (n c) -> p n c", n=4),
                in1=S_BIASC.rearrange("p (n k c) -> p n k c", n=4, k=2)[:, :, kvh, :], op=ALU.add),
                r=[f"ps{bk}", "stab"], w=["tm_any"])
            act(lambda e, tm=tm, px=px: e.activation(out=px[:, 0:256], in_=tm[:, 0:256], func=AF.Exp), r=["tm_any"], w=["px_any"])
            for nt in range(4):
                for g in range(8):
                    bk2 = 4 + g // 2
                    pe(lambda e, nt=nt, g=g, bk2=bk2, px=px, kvh=kvh: e.matmul(
                        psb[bk2][0:8, (g % 2) * 194:(g % 2 + 1) * 194], lhsT=px[:, nt * 64 + g * 8:nt * 64 + g * 8 + 8],
                        rhs=vcAs[:, nt, kvh, :], start=(nt == 0 and g % 2 == 0), stop=False, skip_group_check=True),
                        r=["px_any", "vcAs"], w=[f"ps{bk2}"])
            for j in range(4):
                ov = psb[4 + j][0:8, 0:388].rearrange("p (g c) -> p g c", g=2)
                dve(lambda e, j=j, ov=ov: e.tensor_scalar(out=rds[:, 2 * j:2 * j + 2], in0=ov[:, :, 64], scalar1=1e-30,
                                                          scalar2=None, op0=ALU.max), r=[f"ps{4 + j}"], w=["rds"])
            dve(lambda e: e.reciprocal(out=rds[:, 0:8], in_=rds[:, 0:8]), r=["rds"], w=["rds"])
            gv = gts[:, kvh * 24:(kvh + 1) * 24].rearrange("p (g x) -> p g x", x=3)
            dve(lambda e, gv=gv: e.tensor_tensor(out=gss[:, 0:8], in0=rds[:, 0:8], in1=gv[:, :, 0], op=ALU.mult),
                r=["rds", "gts"], w=["gss"])
            for j in range(4):
                ov = psb[4 + j][0:8, 0:388].rearrange("p (g c) -> p g c", g=2)
                dve(lambda e, j=j, ov=ov: e.tensor_tensor(
                    out=imps[:, 2 * j:2 * j + 2, :], in0=ov[:, :, 65:194],
                    in1=rds[:, 2 * j:2 * j + 2].unsqueeze(2).to_broadcast([8, 2, 129]), op=ALU.mult),
                    r=[f"ps{4 + j}", "rds"], w=["imps"])
                dve(lambda e, j=j, ov=ov, kvh=kvh: e.tensor_tensor(
                    out=As[:, kvh * 512 + j * 128:kvh * 512 + (j + 1) * 128].rearrange("p (g d) -> p g d", g=2),
                    in0=ov[:, :, 0:64], in1=gss[:, 2 * j:2 * j + 2].unsqueeze(2).to_broadcast([8, 2, 64]), op=ALU.mult),
                    r=[f"ps{4 + j}", "gss"], w=["As"])
            dve(lambda e: e.tensor_reduce(out=impv[:], in_=imps[:].rearrange("p g j -> p j g"), axis=AX.X, op=ALU.add),
                r=["imps"], w=["impv"])
            dve(lambda e: e.tensor_tensor(out=impv[:], in0=impv[:], in1=S_FS[0:8, :], op=ALU.max), r=["impv", "stab"], w=["impv"])
            for c3 in range(3):
                dve(lambda e, c3=c3: e.tensor_tensor(
                    out=cmps[:], in0=impv[:].unsqueeze(1).to_broadcast([8, 43, 129]),
                    in1=impv[:, c3 * 43:(c3 + 1) * 43].unsqueeze(2).to_broadcast([8, 43, 129]), op=ALU.is_gt),
                    r=["impv"], w=["cmps"])
                dve(lambda e, c3=c3: e.tensor_reduce(out=cnts[:, c3 * 43:(c3 + 1) * 43], in_=cmps[:], axis=AX.X, op=ALU.add),
                    r=["cmps"], w=["cnts"])
            dve(lambda e: e.memset(nsels[:], 0.0), w=["nsels"])
            dve(lambda e: e.tensor_scalar(out=nsels[:, 0:129], in0=cnts[:], scalar1=15.5, scalar2=-30000.0, op0=ALU.is_gt,
                                          op1=ALU.mult), r=["cnts"], w=["nsels"])
            dve(lambda e: e.tensor_copy(out=nselW[:], in_=nsels[:, 0:128].rearrange("p (w j) -> p w j", w=2)
                                        .unsqueeze(2).to_broadcast([8, 2, 2, 64])), r=["nsels"], w=["nselW"])
            for w2 in range(2):
                pe(lambda e, w2=w2: e.transpose(out=bank_bf(3)[:, 8 * w2:8 * w2 + 8],
                                                in_=nselW[:, w2, :, :].rearrange("p a j -> p (a j)"),
                                                identity=ident[0:8, 0:8]), r=["nselW", "ident"], w=["ps3"])
            dve(lambda e: e.tensor_copy(out=nsTs[:], in_=bank_bf(3)[:, 0:16].rearrange("p (w t) -> p w t", w=2)
                                        .unsqueeze(2).to_broadcast([128, 2, 8, 8])), r=["ps3"], w=["nsTs"])
            acc = (4, 5, 6, 7)
            for bt in ([] if "s_sel" in DEV_SKIP else range(9)):
                kt0 = bt * 8
                nkt = 8 if bt < 8 else 1
                nk = 128 if bt < 8 else 8
                si_[0] += 1
                bk = s_scores(lambda rows, kt: kT3[rows, 2, kt * 128:kt * 128 + (128 if kt < 64 else 8)], nkt, kt0, kvh, b,
                              nk, True)
                tm, px = tmpS[si_[0] % 2], pS[si_[0] % 2]
                w_ = nkt * 64
                if bt == 8:
                    dve(lambda e, bk=bk, tm=tm, kvh=kvh: e.tensor_tensor(out=tm[0:8, 0:64], in0=psb[bk][0:8, 0:64],
                                                                         in1=S_BIASW[0:8, 4, kvh, :], op=ALU.add),
                        r=[f"ps{bk}", "stab"], w=["tm_any"])
                else:
                    dve(lambda e, bk=bk, tm=tm, kvh=kvh, nk=nk, nkt=nkt, w_=w_: e.tensor_tensor(
                        out=tm[0:nk, 0:w_].rearrange("p (n c) -> p n c", n=nkt),
                        in0=psb[bk][0:nk, 0:w_].rearrange("p (n c) -> p n c", n=nkt),
                        in1=S_BASE2[0:nk, kvh, :].unsqueeze(1).to_broadcast([nk, nkt, 64]), op=ALU.add),
                        r=[f"ps{bk}", "stab"], w=["tm_any"])
                if bt < 8:
                  dve(lambda e, tm=tm, kvh=kvh, nk=nk, nkt=nkt, w_=w_, kt0=kt0: e.tensor_tensor(
                    out=tm[0:nk, 0:w_].rearrange("p (n g t) -> p n g t", n=nkt, g=8),
                    in0=tm[0:nk, 0:w_].rearrange("p (n g t) -> p n g t", n=nkt, g=8),
                    in1=S_CVEC2[0:nk, kt0:kt0 + nkt, kvh, :].unsqueeze(3).to_broadcast([nk, nkt, 8, 8]), op=ALU.add),
                    r=["tm_any", "stab"], w=["tm_any"])
                act(lambda e, tm=tm, px=px, nk=nk, w_=w_: e.activation(out=px[0:nk, 0:w_], in_=tm[0:nk, 0:w_], func=AF.Exp),
                    r=["tm_any"], w=["px_any"])
                s_pv(bk, px, nkt, kt0, kvh, nk, lambda kt, kvh_: svAs[0:(128 if kt < 64 else 8), kt, kvh_, :], 65,
                     acc, 2, 0)
            for j in range(4):
                ov = psb[4 + j][0:8, 0:130].rearrange("p (g c) -> p g c", g=2)
                dve(lambda e, j=j, ov=ov: e.tensor_scalar(out=rds[:, 2 * j:2 * j + 2], in0=ov[:, :, 64], scalar1=1e-30,
                                                          scalar2=None, op0=ALU.max), r=[f"ps{4 + j}"], w=["rds"])
            dve(lambda e: e.reciprocal(out=rds[:, 0:8], in_=rds[:, 0:8]), r=["rds"], w=["rds"])
            dve(lambda e, gv=gv: e.tensor_tensor(out=gss[:, 0:8], in0=rds[:, 0:8], in1=gv[:, :, 1], op=ALU.mult),
                r=["rds", "gts"], w=["gss"])
            for j in range(4):
                ov = psb[4 + j][0:8, 0:130].rearrange("p (g c) -> p g c", g=2)
                dve(lambda e, j=j, ov=ov: e.tensor_tensor(
                    out=ots[:, 2 * j:2 * j + 2, :], in0=ov[:, :, 0:64],
                    in1=gss[:, 2 * j:2 * j + 2].unsqueeze(2).to_broadcast([8, 2, 64]), op=ALU.mult),
                    r=[f"ps{4 + j}", "gss"], w=["ots"])
            dve(lambda e, kvh=kvh: e.tensor_tensor(out=As[:, kvh * 512:(kvh + 1) * 512], in0=As[:, kvh * 512:(kvh + 1) * 512],
                                                   in1=ots[:, 0:8, :].rearrange("p g d -> p (g d)"), op=ALU.add),
                r=["As", "ots"], w=["As"])
            si_[0] += 1
            bk = s_scores(lambda rows, kt: wkTs[rows, kt * 128:(kt + 1) * 128], 4, 0, kvh, b, 128, False)
            pe(lambda e, bk=bk, kvh=kvh, b=b: e.matmul(psb[bk][0:8, 256:320], lhsT=wkTs[kvh * 64:(kvh + 1) * 64, 512:520],
                                                       rhs=qTs2[kvh * 64:(kvh + 1) * 64, b, :], start=True, stop=True),
               r=["wkTs", "qTs2"], w=[f"ps{bk}"])
            tm, px = tmpS[si_[0] % 2], pS[si_[0] % 2]
            dve(lambda e, bk=bk, tm=tm, kvh=kvh: e.tensor_tensor(
                out=tm[:, 0:256].rearrange("p (n c) -> p n c", n=4), in0=psb[bk][:, 0:256].rearrange("p (n c) -> p n c", n=4),
                in1=S_BIASW[:, 0:4, kvh, :], op=ALU.add), r=[f"ps{bk}", "stab"], w=["tm_any"])
            dve(lambda e, bk=bk, tm=tm, kvh=kvh: e.tensor_tensor(out=tm[0:8, 256:320], in0=psb[bk][0:8, 256:320],
                                                                 in1=S_BIASW[0:8, 4, kvh, :], op=ALU.add),
                r=[f"ps{bk}", "stab"], w=["tm_any"])
            act(lambda e, tm=tm, px=px: e.activation(out=px[:, 0:256], in_=tm[:, 0:256], func=AF.Exp), r=["tm_any"], w=["px_any"])
            act(lambda e, tm=tm, px=px: e.activation(out=px[0:8, 256:320], in_=tm[0:8, 256:320], func=AF.Exp),
                r=["tm_any"], w=["px_any"])
            for kt in range(5):
                nk = 128 if kt < 4 else 8
                for g in range(8):
                    bk2 = 4 + g // 2
                    pe(lambda e, kt=kt, g=g, bk2=bk2, px=px, nk=nk, kvh=kvh: e.matmul(
                        psb[bk2][0:8, (g % 2) * 65:(g % 2 + 1) * 65], lhsT=px[0:nk, kt * 64 + g * 8:kt * 64 + g * 8 + 8],
                        rhs=wvAs[0:nk, kt, kvh, :], start=(kt == 0 and g % 2 == 0), stop=False, skip_group_check=True),
                        r=["px_any", "wvAs"], w=[f"ps{bk2}"])
            for j in range(4):
                ov = psb[4 + j][0:8, 0:130].rearrange("p (g c) -> p g c", g=2)
                dve(lambda e, j=j, ov=ov: e.tensor_scalar(out=rds[:, 2 * j:2 * j + 2], in0=ov[:, :, 64], scalar1=1e-30,
                                                          scalar2=None, op0=ALU.max), r=[f"ps{4 + j}"], w=["rds"])
            dve(lambda e: e.reciprocal(out=rds[:, 0:8], in_=rds[:, 0:8]), r=["rds"], w=["rds"])
            dve(lambda e, gv=gv: e.tensor_tensor(out=gss[:, 0:8], in0=rds[:, 0:8], in1=gv[:, :, 2], op=ALU.mult),
                r=["rds", "gts"], w=["gss"])
            for j in range(4):
                ov = psb[4 + j][0:8, 0:130].rearrange("p (g c) -> p g c", g=2)
                dve(lambda e, j=j, ov=ov: e.tensor_tensor(
                    out=ots[:, 2 * j:2 * j + 2, :], in0=ov[:, :, 0:64],
                    in1=gss[:, 2 * j:2 * j + 2].unsqueeze(2).to_broadcast([8, 2, 64]), op=ALU.mult),
                    r=[f"ps{4 + j}", "gss"], w=["ots"])
            dve(lambda e, kvh=kvh: e.tensor_tensor(out=As[:, kvh * 512:(kvh + 1) * 512], in0=As[:, kvh * 512:(kvh + 1) * 512],
                                                   in1=ots[:, 0:8, :].rearrange("p g d -> p (g d)"), op=ALU.add),
                r=["As", "ots"], w=["As"])
        act(lambda e: e.activation(out=Abs_[:], in_=As[:], func=AF.Copy), r=["As"], w=["Abs_"])
        sp(lambda e, b=b: e.dma_start(out=Ascr[TP + 8 * b:TP + 8 * b + 8, :], in_=Abs_[:]), r=["Abs_"], w=[("Ascr_s", b)])
    B.release(m4s)
    yacc = B.sb("yacc", [128, NT, 1024])
    hnT = B.sb("hnT", [128, 8, TT], BF16)
    comb = B.sb("comb", [128, NT, 32])
    m5 = B.mark()
    Wa = B.sb("Wa", [128, 8, 1024], BF16)
    Wm = B.sb("Wm", [128, 8, 1024], BF16)
    Wo = B.sb("Wo", [128, 8, 1024], BF16)
    for nm, dst, src in (("Wa", Wa, wa_in), ("Wm", Wm, wm_in), ("Wo", Wo, wo_in)):
        sv = src.rearrange("(kc p) n -> p kc n", p=128)
        for hh in range(2):
            S.dma("pool", lambda e, dst=dst, sv=sv, hh=hh: e.dma_start(out=dst[:, 4 * hh:4 * hh + 4, :],
                                                                        in_=sv[:, 4 * hh:4 * hh + 4, :]),
                  writes=[nm + str(hh)])
    wr = B.sb("wr", [128, 8, 36])
    rbb = B.sb("rbb", [128, 36])
    g2t = B.sb("g2t", [128, 1024])
    sp(lambda e: e.dma_start(out=wr[:], in_=wr_in), w=["wr"])
    sp(lambda e: e.dma_start(out=rbb[:], in_=rb_in), w=["rbb"])
    sp(lambda e: e.dma_start(out=g2t[:], in_=g2_in), w=["g2t"])
    Ab5 = B.sb("Ab5", [128, 1024], BF16)
    Mb5 = B.sb("Mb5", [128, 1024], BF16)
    AT5 = B.sb("AT5", [128, 8, 128], BF16)
    MT5 = B.sb("MT5", [128, 8, 128], BF16)
    mgt = B.sb("mgt", [128, 2048])
    mix = B.sb("mix", [128, 1024])
    mixb = B.sb("mixb", [128, 1024], BF16)
    xT5 = AT5
    x5 = B.sb("x5", [128, 1024])
    hn5 = B.sb("hn5", [128, 1024])
    hnb = B.sb("hnb", [128, 1024], BF16)
    hnTf = B.sb("hnTf", [128, 8, 128])
    s5 = B.sb("s5", [128, 8])
    lg = B.sb("lg", [128, 36])
    r5 = B.sb("r5", [128, 64])
    r5b = B.sb("r5b", [128, 4, 8])
    idf5 = B.sb("idf5", [128, 128])
    sp(lambda e: e.dma_start(out=idf5[:], in_=mtab_in[:, 768:896]), w=["idf5"])

    def tr8(src_bf, dstT, bank, rk, wk):
        pv = bank_bf(bank)
        for kc in range(8):
            pe(lambda e, kc=kc, pv=pv: e.transpose(out=pv[:, kc * 128:(kc + 1) * 128],
                                                   in_=src_bf[:, kc * 128:(kc + 1) * 128], identity=ident[:]),
               r=[rk, "ident"], w=[f"ps{bank}"])
        dve(lambda e, pv=pv: e.tensor_copy(out=dstT, in_=pv.rearrange("p (k c) -> p k c", k=8)),
            r=[f"ps{bank}"], w=[wk])

    def proj1024(lT, W, wname, banks, rk):
        for hh in range(2):
            for kc in range(8):
                pe(lambda e, hh=hh, kc=kc: e.matmul(psb[banks[hh]][:, :], lhsT=lT[:, kc, :],
                                                    rhs=W[:, kc, hh * 512:(hh + 1) * 512],
                                                    start=(kc == 0), stop=(kc == 7)),
                   r=[rk, wname + "0", wname + "1"], w=[f"ps{banks[hh]}"])

    for t in ([] if "moe" in DEV_SKIP else range(NT)):
        r0 = t * 128
        sp(lambda e, r0=r0: e.dma_start(out=Ab5[:], in_=Ascr[r0:r0 + 128, :]), w=["Ab5"])
        sp(lambda e, r0=r0: e.dma_start(out=Mb5[:], in_=Mscr[r0:r0 + 128, :]), w=["Mb5"])
        sp(lambda e, r0=r0: e.dma_start(out=mgt[:], in_=proj[r0:r0 + 128, C_MG:C_MG + 2048]), w=["mgt"])
        sp(lambda e, r0=r0: e.dma_start(out=x5[:], in_=x[r0:r0 + 128, :]), w=["x5"])
        act(lambda e: e.activation(out=mgt[:], in_=mgt[:], func=AF.Sigmoid), r=["mgt"], w=["mgt"])
        tr8(Ab5, AT5[:], 0, "Ab5", "AT5")
        tr8(Mb5, MT5[:], 1, "Mb5", "MT5")
        proj1024(AT5, Wa, "Wa", (2, 3), "AT5")
        proj1024(MT5, Wm, "Wm", (4, 5), "MT5")
        dve(lambda e: e.tensor_tensor(out=mix[:], in0=psall[:, 1024:2048], in1=mgt[:, 0:1024], op=ALU.mult),
            r=["ps2", "ps3", "mgt"], w=["mix"])
        dve(lambda e: e.tensor_tensor(out=hn5[:], in0=psall[:, 2048:3072], in1=mgt[:, 1024:2048], op=ALU.mult),
            r=["ps4", "ps5", "mgt"], w=["hn5"])
        dve(lambda e: e.tensor_tensor(out=mixb[:], in0=mix[:], in1=hn5[:], op=ALU.add), r=["mix", "hn5"], w=["mixb"])
        tr8(mixb, xT5[:], 6, "mixb", "AT5")
        proj1024(xT5, Wo, "Wo", (2, 3), "AT5")
        dve(lambda e, t=t: e.tensor_tensor(out=yacc[:, t, :], in0=psall[:, 1024:2048], in1=x5[:], op=ALU.add),
            r=["ps2", "ps3", "x5"], w=[("yacc", t)])
        act(lambda e, t=t: e.activation(out=mix[:], in_=yacc[:, t, :], func=AF.Square, accum_out=s5[:, 0:1]),
            r=[("yacc", t)], w=["mix", "s5"])
        dve(lambda e: e.tensor_scalar(out=s5[:, 0:1], in0=s5[:, 0:1], scalar1=1.0 / D, scalar2=EPS, op0=ALU.mult,
                                      op1=ALU.add), r=["s5"], w=["s5"])
        act(lambda e: e.activation(out=s5[:, 0:1], in_=s5[:, 0:1], func=AF.Sqrt), r=["s5"], w=["s5"])
        dve(lambda e: e.reciprocal(out=s5[:, 0:1], in_=s5[:, 0:1]), r=["s5"], w=["s5"])
        dve(lambda e, t=t: e.scalar_tensor_tensor(out=hn5[:], in0=yacc[:, t, :], scalar=s5[:, 0:1], in1=g2t[:],
                                                  op0=ALU.mult, op1=ALU.mult), r=[("yacc", t), "s5", "g2t"], w=["hn5"])
        act(lambda e: e.activation(out=hnb[:], in_=hn5[:], func=AF.Copy), r=["hn5"], w=["hnb"])
        tr8(hnb, hnT[:, :, r0:r0 + 128], 7, "hnb", ("hnT", t))
        for kc in range(8):
            bk = 4 + kc // 4
            pe(lambda e, kc=kc, bk=bk: e.transpose(out=psb[bk][:, (kc % 4) * 128:(kc % 4 + 1) * 128],
                                                   in_=hn5[:, kc * 128:(kc + 1) * 128], identity=idf5[:]),
               r=["hn5", "idf5"], w=[f"ps{bk}"])
        dve(lambda e: e.tensor_copy(out=hnTf[:], in_=psall[:, 2048:3072].rearrange("p (k c) -> p k c", k=8)),
            r=["ps4", "ps5"], w=["hnTf"])
        for kc in range(8):
            pe(lambda e, kc=kc: e.matmul(psb[6][:, 0:36], lhsT=hnTf[:, kc, :], rhs=wr[:, kc, :], start=(kc == 0),
                                         stop=(kc == 7)), r=["hnTf", "wr"], w=["ps6"])
        dve(lambda e: e.tensor_tensor(out=lg[:], in0=psb[6][:, 0:36], in1=rbb[:], op=ALU.add), r=["ps6", "rbb"], w=["lg"])
        R_ = lambda a, b_=None: r5[:, a:(a + 1 if b_ is None else b_)]
        dve(lambda e: e.tensor_reduce(out=R_(0), in_=lg[:, 0:4], axis=AX.X, op=ALU.max), r=["lg"], w=["r5"])
        dve(lambda e: e.tensor_scalar(out=R_(1), in0=R_(0), scalar1=-1.0, scalar2=None, op0=ALU.mult), r=["r5"], w=["r5"])
        dve(lambda e: e.tensor_scalar(out=R_(16, 20), in0=lg[:, 0:4], scalar1=R_(0), scalar2=None, op0=ALU.is_ge),
            r=["lg", "r5"], w=["r5"])
        act(lambda e: e.activation(out=R_(20, 24), in_=lg[:, 0:4], func=AF.Exp, bias=R_(1), accum_out=R_(2)),
            r=["lg", "r5"], w=["r5"])
        dve(lambda e: e.reciprocal(out=R_(3), in_=R_(2)), r=["r5"], w=["r5"])
        dve(lambda e: e.tensor_tensor(out=r5b[:], in0=lg[:, 4:36].rearrange("p (g x) -> p g x", g=4),
                                      in1=R_(16, 20).unsqueeze(2).to_broadcast([128, 4, 8]), op=ALU.mult),
            r=["lg", "r5"], w=["r5b"])
        dve(lambda e: e.tensor_reduce(out=R_(24, 32), in_=r5b[:].rearrange("p g x -> p x g"), axis=AX.X, op=ALU.add),
            r=["r5b"], w=["r5"])
        dve(lambda e: e.tensor_reduce(out=R_(4), in_=R_(24, 32), axis=AX.X, op=ALU.max), r=["r5"], w=["r5"])
        dve(lambda e: e.tensor_scalar(out=R_(32, 40), in0=R_(24, 32), scalar1=R_(4), scalar2=None, op0=ALU.is_ge),
            r=["r5"], w=["r5"])
        dve(lambda e: e.scalar_tensor_tensor(out=R_(40, 48), in0=R_(32, 40), scalar=-1e30, in1=R_(24, 32),
                                             op0=ALU.mult, op1=ALU.add), r=["r5"], w=["r5"])
        dve(lambda e: e.tensor_reduce(out=R_(5), in_=R_(40, 48), axis=AX.X, op=ALU.max), r=["r5"], w=["r5"])
        dve(lambda e: e.tensor_scalar(out=R_(48, 56), in0=R_(40, 48), scalar1=R_(5), scalar2=None, op0=ALU.is_ge),
            r=["r5"], w=["r5"])
        dve(lambda e: e.tensor_tensor(out=R_(6), in0=R_(5), in1=R_(4), op=ALU.subtract), r=["r5"], w=["r5"])
        act(lambda e: e.activation(out=R_(6), in_=R_(6), func=AF.Exp), r=["r5"], w=["r5"])
        dve(lambda e: e.tensor_scalar(out=R_(7), in0=R_(6), scalar1=1.0, scalar2=None, op0=ALU.add), r=["r5"], w=["r5"])
        dve(lambda e: e.reciprocal(out=R_(7), in_=R_(7)), r=["r5"], w=["r5"])
        dve(lambda e: e.tensor_tensor(out=R_(8), in0=R_(6), in1=R_(7), op=ALU.mult), r=["r5"], w=["r5"])
        dve(lambda e: e.tensor_scalar(out=R_(7, 9), in0=R_(7, 9), scalar1=R_(3), scalar2=None, op0=ALU.mult),
            r=["r5"], w=["r5"])
        dve(lambda e: e.tensor_scalar(out=R_(56, 64), in0=R_(32, 40), scalar1=R_(7), scalar2=None, op0=ALU.mult),
            r=["r5"], w=["r5"])
        dve(lambda e: e.scalar_tensor_tensor(out=R_(56, 64), in0=R_(48, 56), scalar=R_(8), in1=R_(56, 64),
                                             op0=ALU.mult, op1=ALU.add), r=["r5"], w=["r5"])
        dve(lambda e, t=t: e.tensor_tensor(out=comb[:, t, :].rearrange("p (g x) -> p g x", g=4),
                                           in0=R_(16, 20).unsqueeze(2).to_broadcast([128, 4, 8]),
                                           in1=R_(56, 64).unsqueeze(1).to_broadcast([128, 4, 8]), op=ALU.mult),
            r=["r5"], w=[("comb", t)])
    B.release(m5)

    wg = [B.sb(f"wg{i}", [128, 8, 512], BF16) for i in range(2)]
    wu = [B.sb(f"wu{i}", [128, 8, 512], BF16) for i in range(2)]
    wd = [B.sb(f"wd{i}", [128, 4, 1024], BF16) for i in range(2)]
    sg6 = [B.sb(f"sg6{i}", [128, 512]) for i in range(2)]
    hm6 = [B.sb(f"hm6{i}", [128, 4, 512], BF16) for i in range(2)]
    blocks = [(0, 4), (4, 4), (8, 4), (12, 4), (16, 1)]
    hi_ = 0
    for ex in ([] if "moe" in DEV_SKIP else range(32)):
        wb = ex % 2
        S.dma("pool", lambda e, ex=ex, wb=wb: e.dma_start(out=wg[wb][:], in_=weg_in[ex].rearrange("(kc p) n -> p kc n", p=128)),
              writes=[f"wg{wb}"])
        S.dma("pool", lambda e, ex=ex, wb=wb: e.dma_start(out=wu[wb][:], in_=weu_in[ex].rearrange("(kc p) n -> p kc n", p=128)),
              writes=[f"wu{wb}"])
        S.dma("pool", lambda e, ex=ex, wb=wb: e.dma_start(out=wd[wb][:], in_=wed_in[ex].rearrange("(kc p) n -> p kc n", p=128)),
              writes=[f"wd{wb}"])
        for (t0, ntl) in blocks:
            ncol = ntl * 128
            hb_ = hi_ % 2
            hi_ += 1
            for fc in range(4):
                gb, ub_ = (0, 1) if fc % 2 == 0 else (2, 3)
                for kc in range(8):
                    pe(lambda e, kc=kc, fc=fc, gb=gb, t0=t0, ncol=ncol, wb=wb: e.matmul(
                        psb[gb][:, 0:ncol], lhsT=wg[wb][:, kc, fc * 128:(fc + 1) * 128],
                        rhs=hnT[:, kc, t0 * 128:t0 * 128 + ncol], start=(kc == 0), stop=(kc == 7)),
                        r=[f"wg{wb}"] + [("hnT", t0 + q_) for q_ in range(ntl)], w=[f"ps{gb}"])
                for kc in range(8):
                    pe(lambda e, kc=kc, fc=fc, ub_=ub_, t0=t0, ncol=ncol, wb=wb: e.matmul(
                        psb[ub_][:, 0:ncol], lhsT=wu[wb][:, kc, fc * 128:(fc + 1) * 128],
                        rhs=hnT[:, kc, t0 * 128:t0 * 128 + ncol], start=(kc == 0), stop=(kc == 7)),
                        r=[f"wu{wb}"] + [("hnT", t0 + q_) for q_ in range(ntl)], w=[f"ps{ub_}"])
                sgb = fc % 2
                act(lambda e, gb=gb, sgb=sgb, ncol=ncol: e.activation(out=sg6[sgb][:, 0:ncol], in_=psb[gb][:, 0:ncol],
                                                                      func=AF.Silu), r=[f"ps{gb}"], w=[f"sg6{sgb}"])
                dve(lambda e, ub_=ub_, sgb=sgb, fc=fc, hb_=hb_, ncol=ncol: e.tensor_tensor(
                    out=hm6[hb_][:, fc, 0:ncol], in0=sg6[sgb][:, 0:ncol], in1=psb[ub_][:, 0:ncol], op=ALU.mult),
                    r=[f"sg6{sgb}", f"ps{ub_}"], w=[f"hm6{hb_}"])
            for q_ in range(ntl):
                t = t0 + q_
                ob = (4, 5) if q_ % 2 == 0 else (6, 7)
                for hh in range(2):
                    for fc in range(4):
                        pe(lambda e, hh=hh, fc=fc, q_=q_, ob=ob, hb_=hb_, wb=wb: e.matmul(
                            psb[ob[hh]][:, :], lhsT=hm6[hb_][:, fc, q_ * 128:(q_ + 1) * 128],
                            rhs=wd[wb][:, fc, hh * 512:(hh + 1) * 512], start=(fc == 0), stop=(fc == 3)),
                            r=[f"hm6{hb_}", f"wd{wb}"], w=[f"ps{ob[hh]}"])
                dve(lambda e, t=t, ob=ob, ex=ex: e.scalar_tensor_tensor(
                    out=yacc[:, t, :], in0=psall[:, ob[0] * 512:ob[0] * 512 + 1024], scalar=comb[:, t, ex:ex + 1],
                    in1=yacc[:, t, :], op0=ALU.mult, op1=ALU.add),
                    r=[f"ps{ob[0]}", f"ps{ob[1]}", ("comb", t), ("yacc", t)], w=[("yacc", t)])
    for t in range(NT):
        sp(lambda e, t=t: e.dma_start(out=y_out[t * 128:(t + 1) * 128, :], in_=yacc[:, t, :]),
           r=[("yacc", t)], w=[("y_out", t)])
    S.finish("sp")
    S.emit(nc, B.es)
    B.es.close()
    return nc


_PROGRAM = None


def _bf16(a):
    return np.asarray(a).astype(ml_dtypes.bfloat16)


def _mamba_tables():
    i = np.arange(128)
    same = (i[:, None] // 8) == (i[None, :] // 8)
    le = i[:, None] <= i[None, :]
    tabs = [le, le & same, np.ones((128, 128), bool), same]
    out = [t.astype(np.float32) for t in tabs]
    out.append(np.where(le, 0.0, -1e4).astype(np.float32))
    out.append(np.where(le & same, 0.0, -1e4).astype(np.float32))
    out.append(np.eye(128, dtype=np.float32))
    rowm = (i[:, None] // 8 == np.arange(16)[None, :]).astype(np.float32)
    out.append(rowm)
    out.append(np.broadcast_to(rowm[:, :, None], (128, 16, 128)).reshape(128, 2048))
    return np.ascontiguousarray(np.concatenate(out, axis=1).astype(np.float32))


def _slopes():
    h = np.arange(1, 17, dtype=np.float64)
    return np.exp2(-8.0 * h / 16).astype(np.float32).astype(np.float64)


def _attn_tables(q_norm, k_norm0, cmp_pe):
    sl = _slopes().reshape(2, 8)
    p = np.arange(128, dtype=np.float64)[:, None]
    f = np.arange(128, dtype=np.float64)[None, :]
    cols = []
    cols.append(np.broadcast_to(np.tile(q_norm, 16)[None, :], (128, 1024)))
    cols.append(np.broadcast_to(np.tile(k_norm0, 2)[None, :], (128, 128)))
    n = np.arange(128)[:, None]
    j = np.arange(32)[None, :]
    cover = ((16 * n < 64 * (j + 1)) & (16 * n + 31 >= 64 * j)).astype(np.float64)
    cover[127] = 0
    cols.append(cover)
    basec = -sl[None, :, :, None] * (f[:, None, None, :] - 16 * p[:, :, None, None] - 31)
    cols.append(basec.reshape(128, 2048))
    cols.append(16 * p + 31 - f)
    base = -sl[None, :, :, None] * (f[:, None, None, :] - p[:, :, None, None])
    cols.append(base.reshape(128, 2048))
    cols.append((base + np.where(p > f, -30000.0, 0.0)[:, None, None, :]).reshape(128, 2048))
    cols.append((base + np.where(p <= f, -30000.0, 0.0)[:, None, None, :]).reshape(128, 2048))
    dl = np.arange(65, dtype=np.float64)
    cvec = -(dl[:, None] * 128.0) * _slopes()[None, :]
    cols.append(np.broadcast_to(cvec.reshape(1, 65 * 16), (128, 65 * 16)))
    c = np.arange(62)[None, :] - 30
    hi = (np.arange(128)[:, None] >= 64).astype(np.int64)
    back = hi - c
    cols.append(np.where((back >= 0) & (back <= 1), 1e30, 0.0))
    cols.append(np.where(back < 0, -1.0, 3e38))
    pet = np.transpose(cmp_pe, (2, 0, 1)).reshape(64, 64)
    cols.append(np.concatenate([pet, pet], axis=0))
    out = np.concatenate([np.asarray(c_, np.float64) for c_ in cols], axis=1)
    assert out.shape == (128, AT_W), out.shape
    return np.ascontiguousarray(out.astype(np.float32))


def _sample_tables(q_norm, k_norm0, cmp_pe):
    sl = _slopes().reshape(2, 8)
    p = np.arange(128, dtype=np.float64)
    tq = np.arange(8, dtype=np.float64)
    cols = []
    cols.append(np.broadcast_to(np.tile(q_norm, 16)[None, :], (128, 1024)))
    cols.append(np.broadcast_to(np.tile(k_norm0, 8)[None, :], (128, 512)))
    n = 128 * np.arange(4)[None, :] + p[:, None]
    dist = 8192 + tq[None, None, None, None, :] - (16 * n[:, :, None, None, None] + 31)
    bc = -sl[None, None, :, :, None] * dist
    bc = np.where((n >= 511)[:, :, None, None, None], -30000.0, bc)
    cols.append(bc.reshape(128, 512))
    j = np.arange(129)[None, None, :]
    cov = ((16 * n[:, :, None] < 64 * (j + 1)) & (16 * n[:, :, None] + 31 >= 64 * j) & (n[:, :, None] < 511))
    cols.append(cov.astype(np.float64).reshape(128, 516))
    base2 = -sl[None, :, :, None] * (tq[None, None, None, :] - p[:, None, None, None])
    cols.append(base2.reshape(128, 128))
    kt = np.arange(65, dtype=np.float64)
    cv2 = -sl[None, :, :] * (8192 - 128 * kt)[:, None, None]
    cols.append(np.broadcast_to(cv2.reshape(1, 1040), (128, 1040)))
    w = 128 * np.arange(5)[None, :] + p[:, None]
    dw = 512 + tq[None, None, None, None, :] - w[:, :, None, None, None]
    bw = -sl[None, None, :, :, None] * dw
    bw = np.where((dw >= 0) & (dw < 512) & (w[:, :, None, None, None] < 520), bw, -30000.0)
    cols.append(bw.reshape(128, 640))
    fs = np.zeros((128, 129)); fs[:, [0, 127, 128]] = 1e30
    cols.append(fs)
    pet = np.transpose(cmp_pe, (2, 0, 1)).reshape(64, 64)
    cols.append(np.concatenate([pet, pet], axis=0))
    cols.append(p[:, None])
    out = np.concatenate([np.asarray(c_, np.float64) for c_ in cols], axis=1)
    pad = ST_W - out.shape[1]
    assert pad >= 0, out.shape
    out = np.concatenate([out, np.zeros((128, pad))], axis=1)
    return np.ascontiguousarray(out.astype(np.float32))


def _exw_table():
    r = np.arange(128)[:, None, None] % 64
    kk = np.arange(32)[None, :, None]
    m = np.arange(128)[None, None, :]
    return np.ascontiguousarray((r == 2 * kk + m // 64).astype(np.float32).astype(ml_dtypes.bfloat16))


def _ex_table():
    jj = np.arange(32)[:, None, None]
    kt = np.arange(16)[None, :, None]
    m = np.arange(128)[None, None, :]
    return np.ascontiguousarray((jj == 2 * kt + m // 64).astype(np.float32).astype(ml_dtypes.bfloat16))


def kernel(**inputs):
    global _PROGRAM
    f = lambda k: np.ascontiguousarray(np.asarray(inputs[k]))
    x_prompt, x_sample = f("x_prompt"), f("x_sample")
    k_norm = f("k_norm")[0]
    if _PROGRAM is None:
        _PROGRAM = build_program()
    nc = _PROGRAM
    shared = {
        "w_in": f("w_in")[0],
        "g1_bc": np.ascontiguousarray(np.broadcast_to(f("norm1")[0][None, :], (128, D))),
        "gk1_bc": np.ascontiguousarray(np.broadcast_to(np.tile(k_norm[1], 2)[None, :], (128, 128))),
        "gk2_bc": np.ascontiguousarray(np.broadcast_to(np.tile(k_norm[2], 2)[None, :], (128, 128))),
        "ident_bf": np.eye(128, dtype=np.float32).astype(ml_dtypes.bfloat16),
        "conv_wb": np.ascontiguousarray(np.broadcast_to(
            np.concatenate([f("conv_w")[0], f("conv_b")], axis=0)[None], (128, 5, 2048))),
        "mvec": np.ascontiguousarray(np.broadcast_to(
            np.concatenate([f("dt_bias")[0], f("a_log")[0], f("d_skip")[0]])[None, :], (128, 48))),
        "gssm_bc": np.ascontiguousarray(np.broadcast_to(f("ssm_norm")[0][None, :], (128, 1024))),
        "mtab": _mamba_tables(),
        "atab": _attn_tables(f("q_norm")[0], k_norm[0], f("cmp_pe")[0]),
        "ex_bf": _ex_table(),
        "exw_bf": _exw_table(),
        "stab": _sample_tables(f("q_norm")[0], k_norm[0], f("cmp_pe")[0]),
        "cache_kv": f("cache_kv")[0].reshape(10240 * 128, 512),
        "w_branch_attn": f("w_branch_attn")[0],
        "w_branch_ssm": f("w_branch_ssm")[0],
        "w_out": f("w_out")[0],
        "wr": np.ascontiguousarray(np.transpose(
            np.concatenate([f("w_router_group")[0], f("w_router_expert")[0]], axis=1).reshape(8, 128, 36), (1, 0, 2))),
        "rb_bc": np.ascontiguousarray(np.broadcast_to(
            np.concatenate([f("b_router_group")[0], f("b_router_expert")[0]])[None, :], (128, 36))),
        "g2_bc": np.ascontiguousarray(np.broadcast_to(f("norm2")[0][None, :], (128, D))),
        "w_exp_gate": f("w_exp_gate")[0][:(1 if "moe" in DEV_SKIP else 32)],
        "w_exp_up": f("w_exp_up")[0][:(1 if "moe" in DEV_SKIP else 32)],
        "w_exp_down": f("w_exp_down")[0][:(1 if "moe" in DEV_SKIP else 32)],
        "w1h": np.ascontiguousarray(np.tile(
            np.transpose(f("cmp_w1")[0].reshape(2, 32, 64, 128), (2, 0, 1, 3)), (2, 1, 1, 1))),
        "w2h": np.ascontiguousarray(np.transpose(f("cmp_w2")[0], (1, 0, 2))),
    }
    st_conv = f("state_conv")[0]
    page_table = f("page_table")
    st_ssm = f("state_ssm")[0].reshape(128, 1024, 128)
    cwin = f("cache_win_kv")[0].reshape(128, 512, 256)
    in_maps = []
    for c in range(NCORES):
        m = dict(shared)
        m["x"] = np.ascontiguousarray(np.concatenate(
            [x_prompt[c], x_sample[16 * c:16 * c + 16].reshape(TS, D)], axis=0))
        m["cache_win"] = np.ascontiguousarray(cwin[16 * c:16 * c + 16])
        m["state_conv"] = np.ascontiguousarray(st_conv[16 * c:16 * c + 16])
        m["pt_bc"] = np.ascontiguousarray(np.broadcast_to(
            page_table[16 * c:16 * c + 16].reshape(1, 1024), (128, 1024)).astype(np.int32))
        m["state_ssm"] = np.ascontiguousarray(st_ssm[16 * c:16 * c + 16])
        in_maps.append(m)
    res = run_bass_kernel_spmd(nc, in_maps, core_ids=list(range(NCORES)))
    R = res.results

    def cat(name, sl=None):
        return np.stack([np.asarray(r[name]) if sl is None else np.asarray(r[name])[sl] for r in R], axis=0)

    kv = cat("kv_out")
    kv_prompt = kv[:, :TP].reshape(1, 8, 2048, 4, 2, 64)
    kv_sample = kv[:, TP:].reshape(1, 128, 8, 4, 2, 64)
    win_prompt = cat("winp_out").reshape(1, 8, 512, 2, 2, 64)
    win_sample = cat("wins_out").reshape(1, 128, 512, 2, 2, 64)
    conv_prompt = cat("convp_out").reshape(1, 8, 3, 2048)
    conv_sample = cat("convs_out").reshape(1, 128, 3, 2048)
    yy = cat("y_out")
    y_prompt = np.ascontiguousarray(yy[:, :TP])
    y_sample = np.ascontiguousarray(yy[:, TP:].reshape(128, 8, 1024))
    ssm_prompt = cat("ssmp_out").reshape(1, 8, 16, 64, 128)
    ssm_sample = cat("ssms_out").reshape(1, 128, 16, 64, 128)
    global DEBUG_OUT
    DEBUG_OUT = {"Ascr": np.asarray(R[0]["Ascr"]).astype(np.float32)}
    return (y_prompt, y_sample, kv_prompt, win_prompt, conv_prompt, ssm_prompt,
            kv_sample, win_sample, conv_sample, ssm_sample)
```

```python
import numpy as np
import ml_dtypes
from contextlib import ExitStack
import concourse.bass as bass
import concourse.mybir as mybir
from concourse.bass_utils import run_bass_kernel_spmd

F32 = mybir.dt.float32
BF16 = mybir.dt.bfloat16
I32 = mybir.dt.int32
U32 = mybir.dt.uint32
ALU = mybir.AluOpType
AF = mybir.ActivationFunctionType
AX = mybir.AxisListType

NCORES = 8
D = 1024
TP = 2048
TS = 128
TT = TP + TS
NT = TT // 128
DIN = 6976
EPS = 1e-6
C_Q, C_KV, C_AG, C_Z, C_XBC, C_DT, C_MG = 0, 1024, 1792, 1840, 2864, 4912, 4928

MT_W = 912 + 2048
AT_W = 1024 + 128 + 32 + 2048 + 128 + 3 * 2048 + 65 * 16 + 62 + 62 + 64
ST_W = 4568
EPOCH = 2048
SAME_ENGINE_SYNC = True


class Sched:
    def __init__(self, n_dma_sems=24):
        self.streams = {e: [] for e in ("pe", "act", "dve", "pool", "sp")}
        self.cnt = {e: 0 for e in self.streams}
        self.lastw = {}
        self.readers = {}
        self.waited = {e: {} for e in self.streams}
        self.n_dma = n_dma_sems
        self.n_sw = 20
        self.sw_rr = 0
        self.dma_uses = [0] * (n_dma_sems + self.n_sw)
        self.dma_rr = 0
        self.sem_keys = set()

    def _need(self, eng, tok, waits):
        if tok is None:
            return
        s, v = tok
        if s[0] == eng and (eng == "pe" or not SAME_ENGINE_SYNC) and s[0] != "dma":
            return
        if self.waited[eng].get(s, 0) >= v:
            return
        if waits.get(s, 0) < v:
            waits[s] = v

    def _deps(self, eng, reads, writes):
        waits = {}
        ps_r = [k for k in reads if isinstance(k, str) and k.startswith("ps")]
        if ps_r:
            writes = list(writes) + ps_r
        for k in reads:
            self._need(eng, self.lastw.get(k), waits)
        for k in writes:
            self._need(eng, self.lastw.get(k), waits)
            for s, v in self.readers.get(k, {}).items():
                self._need(eng, (s, v), waits)
        for s, v in waits.items():
            self.waited[eng][s] = v
        return waits

    def _commit(self, tok, reads, writes):
        s, v = tok
        ps_r = [k for k in reads if isinstance(k, str) and k.startswith("ps")]
        if ps_r:
            writes = list(writes) + ps_r
        for k in reads:
            r = self.readers.setdefault(k, {})
            if r.get(s, 0) < v:
                r[s] = v
        for k in writes:
            self.lastw[k] = tok
            self.readers[k] = {}

    def op(self, eng, fn, reads=(), writes=()):
        waits = self._deps(eng, reads, writes)
        c = self.cnt[eng]
        self.cnt[eng] = c + 1
        tok = ((eng, c // EPOCH), (c % EPOCH) + 1)
        self.sem_keys.add(tok[0])
        self.streams[eng].append((list(waits.items()), fn, tok, 1))
        self._commit(tok, reads, writes)
        return tok

    def dma(self, q, fn, reads=(), writes=()):
        if q == "pool":
            i = self.n_dma + self.sw_rr
            self.sw_rr = (self.sw_rr + 1) % self.n_sw
        else:
            i = self.dma_rr
            self.dma_rr = (i + 1) % self.n_dma
        n_prev = self.dma_uses[i]
        self.dma_uses[i] = n_prev + 1
        key = ("dma", i)
        self.sem_keys.add(key)
        waits = self._deps(q, reads, writes)
        if n_prev > 0 and self.waited[q].get(key, 0) < 16 * n_prev:
            waits[key] = 16 * n_prev
            self.waited[q][key] = 16 * n_prev
        tok = (key, 16 * (n_prev + 1))
        self.streams[q].append((list(waits.items()), fn, tok, 16))
        self._commit(tok, reads, writes)
        return tok

    def barrier(self):
        waits = []
        for i in range(len(self.dma_uses)):
            if self.dma_uses[i]:
                waits.append((("dma", i), 16 * self.dma_uses[i]))
        for e, c in self.cnt.items():
            if c:
                waits.append(((e, (c - 1) // EPOCH), ((c - 1) % EPOCH) + 1))
        for q in self.streams:
            w = [(s_, v) for s_, v in waits if not (s_[0] == q and q in ('pe', 'sp')) and self.waited[q].get(s_, 0) < v]
            for s_, v in w:
                self.waited[q][s_] = v
            if w:
                self.streams[q].append((w, None, None, 0))
        self.lastw = {}
        self.readers = {}

    def finish(self, q="sp"):
        waits = []
        for i in range(len(self.dma_uses)):
            if self.dma_uses[i]:
                waits.append((("dma", i), 16 * self.dma_uses[i]))
        for e, c in self.cnt.items():
            if c and e != q:
                waits.append(((e, (c - 1) // EPOCH), ((c - 1) % EPOCH) + 1))
        self.streams[q].append((waits, None, None, 0))

    def emit(self, nc, es):
        sems = {}
        for k in sorted(self.sem_keys, key=str):
            sems[k] = es.enter_context(nc.semaphore("s_" + "_".join(str(x) for x in k)))
        block = es.enter_context(nc.Block())

        def run(stream):
            def body(eng):
                for waits, fn, tok, inc in stream:
                    for s, v in waits:
                        eng.wait_ge(sems[s], v)
                    if fn is not None:
                        fn(eng).then_inc(sems[tok[0]], inc)
            return body

        block.tensor(run(self.streams["pe"]))
        block.scalar(run(self.streams["act"]))
        block.vector(run(self.streams["dve"]))
        block.gpsimd(run(self.streams["pool"]))
        block.sync(run(self.streams["sp"]))


class Builder:
    ARENA = 51200

    def __init__(self):
        self.nc = bass.Bass("TRN2", target_bir_lowering=False)
        self.es = ExitStack()
        self.S = Sched()
        self.alt = 0
        self.arena = self.es.enter_context(self.nc.sbuf_tensor("arena", [128, self.ARENA], F32))
        self.top = 0

    def inp(self, name, shape, dt=F32):
        return self.nc.dram_tensor(name, list(shape), dt, kind="ExternalInput").ap()

    def outp(self, name, shape, dt=F32):
        return self.nc.dram_tensor(name, list(shape), dt, kind="ExternalOutput").ap()

    def scratch(self, name, shape, dt=F32):
        return self.nc.dram_tensor(name, list(shape), dt, kind="Internal").ap()

    def sb(self, name, shape, dt=F32):
        n = int(np.prod(shape[1:]))
        nf = n if dt in (F32, I32, U32) else (n + 1) // 2
        nf = (nf + 7) // 8 * 8
        assert self.top + nf <= self.ARENA, f"SBUF arena overflow at {name}: {self.top}+{nf}"
        ap = self.arena[:, self.top:self.top + nf]
        self.top += nf
        if dt != F32:
            ap = ap.bitcast(dt)
        ap = ap[:, 0:n]
        if len(shape) == 3:
            ap = ap.rearrange("p (a b) -> p a b", a=shape[1])
        elif len(shape) == 4:
            ap = ap.rearrange("p (a b c) -> p a b c", a=shape[1], b=shape[2])
        if shape[0] != 128:
            ap = ap[0:shape[0]]
        return ap

    def mark(self):
        return self.top

    def release(self, m):
        self.S.barrier()
        self.top = m

    def ps(self, name, shape, dt=F32):
        return self.es.enter_context(self.nc.psum_tensor(name, list(shape), dt))

    def evac_eng(self):
        self.alt ^= 1
        return "act" if self.alt else "dve"


DEV_SKIP = set()
DEV_NSEQ = 16
POOL_PAGES = 10240


def build_program():
    B = Builder()
    nc, S = B.nc, B.S
    x = B.inp("x", [TT, D])
    w_in = B.inp("w_in", [D, DIN])
    g1 = B.inp("g1_bc", [128, D])
    gk1 = B.inp("gk1_bc", [128, 128])
    gk2 = B.inp("gk2_bc", [128, 128])
    ident_in = B.inp("ident_bf", [128, 128], BF16)
    cwin = B.inp("cache_win", [16, 512, 256])

    kv_out = B.outp("kv_out", [TT, 512])
    winp_out = B.outp("winp_out", [512, 256])
    wins_out = B.outp("wins_out", [16, 512, 256])
    convp_out = B.outp("convp_out", [3, 2048])
    convs_out = B.outp("convs_out", [16, 3, 2048])

    conv_wb = B.inp("conv_wb", [128, 5, 2048])
    mvec = B.inp("mvec", [128, 48])
    gssm = B.inp("gssm_bc", [128, 1024])
    mtab_in = B.inp("mtab", [128, MT_W])
    st_conv = B.inp("state_conv", [16, 3, 2048])
    st_ssm = B.inp("state_ssm", [16, 1024, 128])
    ssmp_out = B.outp("ssmp_out", [1024, 128])
    ssms_out = B.outp("ssms_out", [16, 1024, 128])

    atab_in = B.inp("atab", [128, AT_W])
    ex_in = B.inp("ex_bf", [32, 16, 128], BF16)
    exw_in = B.inp("exw_bf", [128, 32, 128], BF16)
    w1_in = B.inp("w1h", [128, 2, 32, 128])
    w2_in = B.inp("w2h", [128, 2, 64])

    wa_in = B.inp("w_branch_attn", [D, D])
    wm_in = B.inp("w_branch_ssm", [D, D])
    wo_in = B.inp("w_out", [D, D])
    wr_in = B.inp("wr", [128, 8, 36])
    rb_in = B.inp("rb_bc", [128, 36])
    g2_in = B.inp("g2_bc", [128, D])
    NEX = 1 if "moe" in DEV_SKIP else 32
    weg_in = B.inp("w_exp_gate", [NEX, D, 512])
    weu_in = B.inp("w_exp_up", [NEX, D, 512])
    wed_in = B.inp("w_exp_down", [NEX, 512, D])
    y_out = B.outp("y_out", [TT, D])

    stab_in = B.inp("stab", [128, ST_W])
    pt_in = B.inp("pt_bc", [128, 1024], I32)
    ckv_in = B.inp("cache_kv", [POOL_PAGES * 128, 512])

    proj = B.scratch("proj", [TT, DIN])
    Mscr = B.scratch("Mscr", [TT, 1024], BF16)
    fullx = B.scratch("fullx", [16, 11, 2048])
    kvn = B.scratch("kvn", [TT, 768])
    Ascr = B.outp("Ascr", [TT, 1024], BF16)

    ident = B.sb("ident", [128, 128], BF16)
    gk1t = B.sb("gk1t", [128, 128])
    gk2t = B.sb("gk2t", [128, 128])
    m0 = B.mark()
    g1t = B.sb("g1t", [128, D])
    xT = B.sb("xT", [128, 8, TT], BF16)
    rstd = B.sb("rstd", [128, NT])
    ssq = B.sb("ssq", [128, NT])
    xt = [B.sb(f"xt{i}", [128, D]) for i in range(2)]
    xg = [B.sb(f"xg{i}", [128, D], BF16) for i in range(2)]
    junk = B.sb("junk", [128, D])
    wbuf = [B.sb(f"wbuf{i}", [128, 8, 512], BF16) for i in range(2)]
    obuf = [B.sb(f"obuf{i}", [128, 512]) for i in range(3)]
    kvt = [B.sb(f"kvt{i}", [128, 768]) for i in range(2)]
    nsq0 = B.sb("nsq", [128, 128])
    nss0 = B.sb("nss", [128, 4])
    ntmp0 = B.sb("ntmp", [128, 128])
    psall = B.ps("psall", [128, 4096])
    psb = [psall[:, i * 512:(i + 1) * 512] for i in range(8)]

    S.dma("sp", lambda e: e.dma_start(out=ident[:], in_=ident_in), writes=["ident"])
    S.dma("sp", lambda e: e.dma_start(out=g1t[:], in_=g1), writes=["g1t"])
    S.dma("sp", lambda e: e.dma_start(out=gk1t[:], in_=gk1), writes=["gk1t"])
    S.dma("sp", lambda e: e.dma_start(out=gk2t[:], in_=gk2), writes=["gk2t"])
    S.dma("sp", lambda e: e.dma_start(out=wins_out[:, 0:504, :], in_=cwin[:, 8:512, :]),
          writes=["wins_old"])

    for t in range(NT):
        b = t % 2
        S.dma("sp", lambda e, t=t, b=b: e.dma_start(out=xt[b][:], in_=x[t * 128:(t + 1) * 128, :]),
              writes=[f"xt{b}"])
        S.op("act", lambda e, t=t, b=b: e.activation(out=junk[:], in_=xt[b][:], func=AF.Square,
                                                     accum_out=ssq[:, t:t + 1]),
             reads=[f"xt{b}"], writes=["junk", ("ssq", t)])
        S.op("dve", lambda e, b=b: e.tensor_tensor(out=xg[b][:], in0=xt[b][:], in1=g1t[:], op=ALU.mult),
             reads=[f"xt{b}", "g1t"], writes=[f"xg{b}"])
        pb = t % 2
        pst = psb[pb][:].bitcast(BF16)
        for kc in range(8):
            S.op("pe", lambda e, kc=kc, b=b, pst=pst: e.transpose(
                out=pst[:, kc * 128:(kc + 1) * 128], in_=xg[b][:, kc * 128:(kc + 1) * 128], identity=ident[:]),
                reads=[f"xg{b}", "ident"], writes=[f"ps{pb}"])
        ev = B.evac_eng()
        if ev == "act":
            S.op("act", lambda e, t=t, pst=pst: e.activation(
                out=xT[:, :, t * 128:(t + 1) * 128], in_=pst.rearrange("p (k c) -> p k c", k=8), func=AF.Copy),
                reads=[f"ps{pb}"], writes=[("xT", t)])
        else:
            S.op("dve", lambda e, t=t, pst=pst: e.tensor_copy(
                out=xT[:, :, t * 128:(t + 1) * 128], in_=pst.rearrange("p (k c) -> p k c", k=8)),
                reads=[f"ps{pb}"], writes=[("xT", t)])
    S.op("dve", lambda e: e.tensor_scalar(out=rstd[:], in0=ssq[:], scalar1=1.0 / D, scalar2=EPS,
                                          op0=ALU.mult, op1=ALU.add),
         reads=[("ssq", t) for t in range(NT)], writes=["rstd"])
    S.op("act", lambda e: e.activation(out=rstd[:], in_=rstd[:], func=AF.Sqrt), reads=["rstd"], writes=["rstd"])
    S.op("dve", lambda e: e.reciprocal(out=rstd[:], in_=rstd[:]), reads=["rstd"], writes=["rstd"])

    w_v = w_in.rearrange("(kc p) n -> p kc n", p=128)
    nchunks = (DIN + 511) // 512
    oi = 0
    for c in range(nchunks):
        c0 = c * 512
        ncol = min(512, DIN - c0)
        wb = c % 2
        S.dma("pool", lambda e, wb=wb, c0=c0, ncol=ncol: e.dma_start(out=wbuf[wb][:, :, 0:ncol],
                                                                      in_=w_v[:, :, c0:c0 + ncol]),
              writes=[f"wbuf{wb}"])
        for t in range(NT):
            pb = 2 + (oi % 4)
            for kc in range(8):
                S.op("pe", lambda e, kc=kc, t=t, wb=wb, pb=pb, ncol=ncol: e.matmul(
                    psb[pb][:, 0:ncol], lhsT=xT[:, kc, t * 128:(t + 1) * 128], rhs=wbuf[wb][:, kc, 0:ncol],
                    start=(kc == 0), stop=(kc == 7)),
                    reads=[("xT", t), f"wbuf{wb}"], writes=[f"ps{pb}"])
            ob = oi % 3
            ev = B.evac_eng()
            if ev == "act":
                S.op("act", lambda e, t=t, pb=pb, ob=ob, ncol=ncol: e.activation(
                    out=obuf[ob][:, 0:ncol], in_=psb[pb][:, 0:ncol], func=AF.Copy, scale=rstd[:, t:t + 1]),
                    reads=[f"ps{pb}", "rstd"], writes=[f"obuf{ob}"])
            else:
                S.op("dve", lambda e, t=t, pb=pb, ob=ob, ncol=ncol: e.tensor_scalar(
                    out=obuf[ob][:, 0:ncol], in0=psb[pb][:, 0:ncol], scalar1=rstd[:, t:t + 1], scalar2=None,
                    op0=ALU.mult),
                    reads=[f"ps{pb}", "rstd"], writes=[f"obuf{ob}"])
            S.dma("sp", lambda e, t=t, ob=ob, c0=c0, ncol=ncol: e.dma_start(
                out=proj[t * 128:(t + 1) * 128, c0:c0 + ncol], in_=obuf[ob][:, 0:ncol]),
                reads=[f"obuf{ob}"], writes=[("proj", t, c)])
            oi += 1

    def proj_keys(t, lo, hi):
        return [("proj", t, c) for c in range(lo // 512, (hi - 1) // 512 + 1)]

    def rms_heads(src, gain, nh, scale, rkeys, wkey, nsq=None, nss=None, ntmp=None):
        nsq = nsq if nsq is not None else nsq0
        nss = nss if nss is not None else nss0
        ntmp = ntmp if ntmp is not None else ntmp0
        w = nh * 64
        S.op("dve", lambda e: e.tensor_tensor(out=nsq[:, 0:w], in0=src, in1=src, op=ALU.mult),
             reads=rkeys, writes=["nsq"])
        S.op("dve", lambda e: e.tensor_reduce(out=nss[:, 0:nh], in_=nsq[:, 0:w].rearrange("p (h d) -> p h d", d=64),
                                              axis=AX.X, op=ALU.add), reads=["nsq"], writes=["nss"])
        S.op("dve", lambda e: e.tensor_scalar(out=nss[:, 0:nh], in0=nss[:, 0:nh], scalar1=1.0 / 64, scalar2=EPS,
                                              op0=ALU.mult, op1=ALU.add), reads=["nss"], writes=["nss"])
        S.op("act", lambda e: e.activation(out=nss[:, 0:nh], in_=nss[:, 0:nh], func=AF.Sqrt),
             reads=["nss"], writes=["nss"])
        S.op("dve", lambda e: e.reciprocal(out=nss[:, 0:nh], in_=nss[:, 0:nh]), reads=["nss"], writes=["nss"])
        S.op("dve", lambda e: e.tensor_tensor(
            out=ntmp[:, 0:w].rearrange("p (h d) -> p h d", d=64), in0=src.rearrange("p (h d) -> p h d", d=64),
            in1=nss[:, 0:nh].unsqueeze(2).to_broadcast([128, nh, 64]), op=ALU.mult),
            reads=rkeys + ["nss"], writes=["ntmp"])
        if scale == 1.0:
            S.op("dve", lambda e: e.tensor_tensor(out=src, in0=ntmp[:, 0:w], in1=gain, op=ALU.mult),
                 reads=["ntmp"], writes=[wkey])
        else:
            S.op("dve", lambda e: e.scalar_tensor_tensor(out=src, in0=ntmp[:, 0:w], scalar=scale, in1=gain,
                                                         op0=ALU.mult, op1=ALU.mult),
                 reads=["ntmp"], writes=[wkey])

    for t in range(NT):
        b = t % 2
        S.dma("sp", lambda e, t=t, b=b: e.dma_start(out=kvt[b][:], in_=proj[t * 128:(t + 1) * 128, C_KV:C_KV + 768]),
              reads=proj_keys(t, C_KV, C_KV + 768), writes=[f"kvt{b}"])
        rms_heads(kvt[b][:, 256:384], gk1t[:], 2, 1.0, [f"kvt{b}"], f"kvt{b}")
        rms_heads(kvt[b][:, 512:640], gk2t[:], 2, 1.0, [f"kvt{b}"], f"kvt{b}")
        S.dma("sp", lambda e, t=t, b=b: e.dma_start(out=kv_out[t * 128:(t + 1) * 128, :], in_=kvt[b][:, 0:512]),
              reads=[f"kvt{b}"], writes=[("kv_out", t)])
        S.dma("sp", lambda e, t=t, b=b: e.dma_start(out=kvn[t * 128:(t + 1) * 128, :], in_=kvt[b][:]),
              reads=[f"kvt{b}"], writes=[("kvn", t)])
        if 12 <= t < 16:
            S.dma("sp", lambda e, t=t, b=b: e.dma_start(out=winp_out[(t - 12) * 128:(t - 11) * 128, :],
                                                         in_=kvt[b][:, 512:768]),
                  reads=[f"kvt{b}"], writes=[("winp_out", t)])
        if t == 16:
            for sq in range(16):
                S.dma("sp", lambda e, b=b, sq=sq: e.dma_start(
                    out=wins_out[sq, 504:512, :], in_=kvt[b][sq * 8:(sq + 1) * 8, 512:768]),
                    reads=[f"kvt{b}"], writes=[("wins_new", sq)])
    S.dma("sp", lambda e: e.dma_start(out=convp_out, in_=proj[TP - 3:TP, C_XBC:C_XBC + 2048]),
          reads=proj_keys(15, C_XBC, C_XBC + 2048), writes=["convp"])
    S.dma("sp", lambda e: e.dma_start(
        out=convs_out, in_=proj[TP:TT, C_XBC:C_XBC + 2048].rearrange("(b t) n -> b t n", t=8)[:, 5:8, :]),
        reads=proj_keys(16, C_XBC, C_XBC + 2048), writes=["convs"])


    B.release(m0)
    def dve(fn, r=(), w=()):
        return S.op("dve", fn, list(r), list(w))

    def act(fn, r=(), w=()):
        return S.op("act", fn, list(r), list(w))

    def pool(fn, r=(), w=()):
        return S.op("pool", fn, list(r), list(w))

    def pe(fn, r=(), w=()):
        return S.op("pe", fn, list(r), list(w))

    def sp(fn, r=(), w=()):
        return S.dma("sp", fn, list(r), list(w))

    cw = B.sb("cw", [128, 5, 2048])
    mv = B.sb("mv", [128, 48])
    gs = B.sb("gs", [128, 1024])
    mtab = B.sb("mtab", [128, MT_W])
    T_UTp, T_UTs, T_ONp, T_ONs, T_NEGp, T_NEGs, T_IDF = [mtab[:, i * 128:(i + 1) * 128] for i in range(7)]
    T_ROWM = mtab[:, 896:912]
    T_ROWSEL = mtab[:, 912:912 + 2048].rearrange("p (b c) -> p b c", b=16)
    Aneg = B.sb("Aneg", [128, 16])
    hT = B.sb("hT", [128, 1024])
    hTb = B.sb("hTb", [128, 1024], BF16)
    xsh = [B.sb(f"xsh{k}", [128, 1024]) for k in range(4)]
    u = B.sb("u", [128, 2048])
    ub = B.sb("ub", [128, 1024], BF16)
    zt = B.sb("zt", [128, 1024])
    dtt = B.sb("dtt", [128, 16])
    a_t = B.sb("a_t", [128, 16])
    xdt = B.sb("xdt", [128, 16, 64])
    xdtb = B.sb("xdtb", [128, 16, 64], BF16)
    xdd = B.sb("xdd", [128, 16, 64], BF16)
    xddm = B.sb("xddm", [128, 16, 64], BF16)
    a_rep = B.sb("a_rep", [128, 16, 128])
    acs = B.sb("acs", [128, 32])
    ea = B.sb("ea", [128, 16])
    dcy = B.sb("dcy", [128, 16])
    cd = B.sb("cd", [128, 16])
    cds = B.sb("cds", [128, 16, 16])
    tmpE = B.sb("tmpE", [128, 16, 128])
    Wt = B.sb("Wt", [128, 16, 128], BF16)
    BT = B.sb("BT", [128, 4, 128], BF16)
    CT = B.sb("CT", [128, 4, 128], BF16)
    CTpad = B.sb("CTpad", [128, 4, 2176], BF16)
    yb = B.sb("yb", [128, 16, 64])
    y2 = B.sb("y2", [128, 16, 64])
    msq = B.sb("msq", [128, 1024])
    mss = B.sb("mss", [128, 4])
    Mb = B.sb("Mb", [128, 1024], BF16)
    sst = B.sb("sst", [128, 8, 128])
    hTs = B.sb("hTs", [128, 1024])
    hTsb = B.sb("hTsb", [128, 1024], BF16)
    sso = B.sb("sso", [128, 8, 128])

    sp(lambda e: e.dma_start(out=cw[:], in_=conv_wb), w=["cw"])
    sp(lambda e: e.dma_start(out=mv[:], in_=mvec), w=["mv"])
    sp(lambda e: e.dma_start(out=gs[:], in_=gssm), w=["gs"])
    sp(lambda e: e.dma_start(out=mtab[:], in_=mtab_in), w=["mtab"])
    sp(lambda e: e.dma_start(out=fullx[:, 0:3, :], in_=st_conv), w=["fullx_a"])
    sp(lambda e: e.dma_start(out=fullx[:, 3:11, :],
                             in_=proj[TP:TT, C_XBC:C_XBC + 2048].rearrange("(b t) n -> b t n", t=8)),
       w=["fullx_b"])
    act(lambda e: e.activation(out=Aneg[:], in_=mv[:, 16:32], func=AF.Exp), r=["mv"], w=["Aneg"])
    dve(lambda e: e.tensor_scalar(out=Aneg[:], in0=Aneg[:], scalar1=-1.0, scalar2=None, op0=ALU.mult),
        r=["Aneg"], w=["Aneg"])
    dve(lambda e: e.memset(hT[:], 0.0), w=["hT"])
    dve(lambda e: e.memset(hTb[:], 0.0), w=["hTb"])
    dve(lambda e: e.memset(CTpad[:], 0.0), w=["CTpad"])

    hv = lambda ap: ap.rearrange("p (h d) -> p h d", d=64)
    bank_bf = lambda i: psb[i][:].bitcast(BF16)

    def mm1(out, lhsT, rhs, r, w):
        pe(lambda e: e.matmul(out, lhsT=lhsT, rhs=rhs, start=True, stop=True), r=r, w=w)

    for t in ([] if "mamba" in DEV_SKIP else range(NT)):
        smp = (t == NT - 1)
        UT, ON, NEG = (T_UTs, T_ONs, T_NEGs) if smp else (T_UTp, T_ONp, T_NEGp)
        r0 = t * 128
        for half in range(2):
            c0 = half * 1024
            for k in range(4):
                sh = 3 - k
                if smp:
                    for b in range(16):
                        sp(lambda e, k=k, b=b, c0=c0: e.dma_start(
                            out=xsh[k][b * 8:(b + 1) * 8, :], in_=fullx[b, k:k + 8, c0:c0 + 1024]),
                            r=["fullx_a", "fullx_b"], w=[f"xsh{k}"])
                elif t == 0 and sh > 0:
                    dve(lambda e, k=k: e.memset(xsh[k][:], 0.0), w=[f"xsh{k}"])
                    sp(lambda e, k=k, sh=sh, c0=c0: e.dma_start(
                        out=xsh[k][sh:128, :], in_=proj[0:128 - sh, C_XBC + c0:C_XBC + c0 + 1024]),
                        w=[f"xsh{k}"])
                else:
                    sp(lambda e, k=k, sh=sh, c0=c0, r0=r0: e.dma_start(
                        out=xsh[k][:], in_=proj[r0 - sh:r0 - sh + 128, C_XBC + c0:C_XBC + c0 + 1024]),
                        w=[f"xsh{k}"])
            uk = f"u{half}"
            for k in range(3):
                pool(lambda e, k=k, c0=c0: e.tensor_tensor(out=xsh[k][:], in0=xsh[k][:], in1=cw[:, k, c0:c0 + 1024],
                                                           op=ALU.mult), r=[f"xsh{k}", "cw"], w=[f"xsh{k}"])
            dve(lambda e, c0=c0: e.tensor_tensor(out=u[:, c0:c0 + 1024], in0=xsh[3][:], in1=cw[:, 3, c0:c0 + 1024],
                                                 op=ALU.mult), r=["xsh3", "cw"], w=[uk])
            for k in range(3):
                dve(lambda e, k=k, c0=c0: e.tensor_tensor(out=u[:, c0:c0 + 1024], in0=u[:, c0:c0 + 1024],
                                                          in1=xsh[k][:], op=ALU.add), r=[uk, f"xsh{k}"], w=[uk])
            dve(lambda e, c0=c0: e.tensor_tensor(out=u[:, c0:c0 + 1024], in0=u[:, c0:c0 + 1024],
                                                 in1=cw[:, 4, c0:c0 + 1024], op=ALU.add), r=[uk, "cw"], w=[uk])
            act(lambda e, c0=c0: e.activation(out=u[:, c0:c0 + 1024], in_=u[:, c0:c0 + 1024], func=AF.Silu),
                r=[uk], w=[uk])
        sp(lambda e, r0=r0: e.dma_start(out=dtt[:], in_=proj[r0:r0 + 128, C_DT:C_DT + 16]), w=["dtt"])
        sp(lambda e, r0=r0: e.dma_start(out=zt[:], in_=proj[r0:r0 + 128, C_Z:C_Z + 1024]), w=["zt"])
        dve(lambda e: e.tensor_tensor(out=dtt[:], in0=dtt[:], in1=mv[:, 0:16], op=ALU.add), r=["dtt", "mv"], w=["dtt"])
        act(lambda e: e.activation(out=dtt[:], in_=dtt[:], func=AF.Exp), r=["dtt"], w=["dtt"])
        act(lambda e: e.activation(out=dtt[:], in_=dtt[:], func=AF.Ln, bias=1.0), r=["dtt"], w=["dtt"])
        dve(lambda e: e.tensor_tensor(out=a_t[:], in0=dtt[:], in1=Aneg[:], op=ALU.mult), r=["dtt", "Aneg"], w=["a_t"])
        dve(lambda e: e.tensor_tensor(out=xdt[:], in0=hv(u[:, 0:1024]),
                                      in1=dtt[:].unsqueeze(2).to_broadcast([128, 16, 64]), op=ALU.mult),
            r=["u0", "dtt"], w=["xdt"])
        act(lambda e: e.activation(out=xdtb[:], in_=xdt[:], func=AF.Copy), r=["xdt"], w=["xdtb"])
        act(lambda e: e.activation(out=ub[:], in_=u[:, 1024:2048], func=AF.Copy), r=["u1"], w=["ub"])
        pst = bank_bf(0)
        for j in range(8):
            pe(lambda e, j=j, pst=pst: e.transpose(out=pst[:, j * 128:(j + 1) * 128], in_=ub[:, j * 128:(j + 1) * 128],
                                                   identity=ident[:]), r=["ub", "ident"], w=["ps0"])
        dve(lambda e, pst=pst: e.tensor_copy(out=BT[:], in_=pst[:, 0:512].rearrange("p (g c) -> p g c", g=4)),
            r=["ps0"], w=["BT"])
        dve(lambda e, pst=pst: e.tensor_copy(out=CT[:], in_=pst[:, 512:1024].rearrange("p (g c) -> p g c", g=4)),
            r=["ps0"], w=["CT"])
        mm1(psb[0][:, 0:16], UT, a_t[:], ["mtab", "a_t"], ["ps0"])
        mm1(psb[0][:, 16:32], ON, a_t[:], ["mtab", "a_t"], ["ps0"])
        if smp:
            for b in range(16):
                mm1(psb[0][:, 32 + 16 * b:48 + 16 * b], T_ROWSEL[:, b, :], a_t[:], ["mtab", "a_t"], ["ps0"])
            act(lambda e: e.activation(out=cds[:], in_=psb[0][:, 32:288].rearrange("p (b h) -> p b h", b=16),
                                       func=AF.Exp), r=["ps0"], w=["cds"])
        dve(lambda e: e.tensor_copy(out=acs[:], in_=psb[0][:, 0:32]), r=["ps0"], w=["acs"])
        dve(lambda e: e.tensor_copy(out=a_rep[:], in_=a_t[:].unsqueeze(2).to_broadcast([128, 16, 128])),
            r=["a_t"], w=["a_rep"])
        for h in range(16):
            bk = 1 + h // 4
            mm1(psb[bk][:, (h % 4) * 128:(h % 4 + 1) * 128], a_rep[:, h, :], UT, ["a_rep", "mtab"], [f"ps{bk}"])
        for j in range(4):
            dve(lambda e, j=j: e.tensor_tensor(
                out=tmpE[:, 4 * j:4 * j + 4, :], in0=psb[1 + j][:].rearrange("p (h c) -> p h c", h=4),
                in1=acs[:, 4 * j:4 * j + 4].unsqueeze(2).to_broadcast([128, 4, 128]), op=ALU.subtract),
                r=[f"ps{1 + j}", "acs"], w=[f"tmpE{j}"])
            pool(lambda e, j=j, NEG=NEG: e.tensor_tensor(
                out=tmpE[:, 4 * j:4 * j + 4, :], in0=tmpE[:, 4 * j:4 * j + 4, :],
                in1=NEG.unsqueeze(1).to_broadcast([128, 4, 128]), op=ALU.add),
                r=[f"tmpE{j}", "mtab"], w=[f"tmpE{j}"])
            act(lambda e, j=j: e.activation(out=tmpE[:, 4 * j:4 * j + 4, :], in_=tmpE[:, 4 * j:4 * j + 4, :],
                                            func=AF.Exp), r=[f"tmpE{j}"], w=[f"tmpE{j}"])
        for g in range(4):
            mm1(psb[5][:, g * 128:(g + 1) * 128], BT[:, g, :], CT[:, g, :], ["BT", "CT"], ["ps5"])
        for g in range(4):
            dve(lambda e, g=g: e.tensor_tensor(
                out=Wt[:, 4 * g:4 * g + 4, :], in0=tmpE[:, 4 * g:4 * g + 4, :],
                in1=psb[5][:, g * 128:(g + 1) * 128].unsqueeze(1).to_broadcast([128, 4, 128]), op=ALU.mult),
                r=[f"tmpE{g}", "ps5"], w=[f"Wt{g}"])
        for h in range(16):
            bk = 6 + h // 8
            mm1(psb[bk][:, (h % 8) * 64:(h % 8 + 1) * 64], Wt[:, h, :], xdtb[:, h, :], [f"Wt{h // 4}", "xdtb"],
                [f"ps{bk}"])
        act(lambda e: e.activation(out=ea[:], in_=acs[:, 0:16], func=AF.Exp), r=["acs"], w=["ea"])
        dve(lambda e: e.tensor_tensor(out=dcy[:], in0=acs[:, 16:32], in1=acs[:, 0:16], op=ALU.subtract),
            r=["acs"], w=["dcy"])
        act(lambda e: e.activation(out=dcy[:], in_=dcy[:], func=AF.Exp), r=["dcy"], w=["dcy"])
        act(lambda e: e.activation(out=cd[:], in_=acs[:, 16:32], func=AF.Exp), r=["acs"], w=["cd"])
        dve(lambda e: e.tensor_tensor(out=xdd[:], in0=xdt[:], in1=dcy[:].unsqueeze(2).to_broadcast([128, 16, 64]),
                                      op=ALU.mult), r=["xdt", "dcy"], w=["xdd"])
        if not smp:
            for g in range(4):
                bk = 1 + g // 2
                mm1(psb[bk][:, (g % 2) * 256:(g % 2 + 1) * 256], CT[:, g, :], hTb[:, g * 256:(g + 1) * 256],
                    ["CT", "hTb"], [f"ps{bk}"])
            for g in range(4):
                bk = 3 + g // 2
                mm1(psb[bk][:, (g % 2) * 256:(g % 2 + 1) * 256], ub[:, g * 128:(g + 1) * 128],
                    xdd[:, 4 * g:4 * g + 4, :], ["ub", "xdd"], [f"ps{bk}"])
            dve(lambda e: e.tensor_tensor(out=hv(hT[:]), in0=hv(hT[:]),
                                          in1=cd[:].unsqueeze(2).to_broadcast([128, 16, 64]), op=ALU.mult),
                r=["hT", "cd"], w=["hT"])
            for j in range(2):
                dve(lambda e, j=j: e.tensor_tensor(out=hT[:, j * 512:(j + 1) * 512], in0=hT[:, j * 512:(j + 1) * 512],
                                                   in1=psb[3 + j][:], op=ALU.add), r=["hT", f"ps{3 + j}"], w=["hT"])
        else:
            for g in range(4):
                dve(lambda e, g=g: e.tensor_copy(
                    out=CTpad[:, g, :].rearrange("p (b c) -> p b c", c=136)[:, :, 0:8],
                    in_=CT[:, g, :].rearrange("p (b c) -> p b c", c=8)), r=["CT"], w=["CTpad"])
            for b in range(16):
                sp(lambda e, b=b: e.dma_start(out=sst[:], in_=st_ssm[b].rearrange("(j q) n -> q j n", q=128)),
                   w=["sst"])
                for j in range(8):
                    bk = 3 + j // 4
                    pe(lambda e, j=j, bk=bk: e.transpose(out=psb[bk][:, (j % 4) * 128:(j % 4 + 1) * 128],
                                                         in_=sst[:, j, :], identity=T_IDF),
                       r=["sst", "mtab"], w=[f"ps{bk}"])
                for j in range(2):
                    dve(lambda e, j=j: e.tensor_copy(out=hTs[:, j * 512:(j + 1) * 512], in_=psb[3 + j][:]),
                        r=[f"ps{3 + j}"], w=["hTs"])
                    act(lambda e, j=j: e.activation(out=hTsb[:, j * 512:(j + 1) * 512], in_=psb[3 + j][:], func=AF.Copy),
                        r=[f"ps{3 + j}"], w=["hTsb"])
                for g in range(4):
                    bk = 1 + g // 2
                    pe(lambda e, g=g, b=b, bk=bk: e.matmul(
                        psb[bk][:, (g % 2) * 256:(g % 2 + 1) * 256], lhsT=CTpad[:, g, b * 128:(b + 1) * 128],
                        rhs=hTsb[:, g * 256:(g + 1) * 256], start=(b == 0 and g % 2 == 0), stop=(b == 15),
                        skip_group_check=True), r=["CTpad", "hTsb"], w=[f"ps{bk}"])
                dve(lambda e, b=b: e.tensor_scalar(out=xddm[:], in0=xdd[:], scalar1=T_ROWM[:, b:b + 1], scalar2=None,
                                                   op0=ALU.mult), r=["xdd", "mtab"], w=["xddm"])
                for g in range(4):
                    bk = 3 + g // 2
                    mm1(psb[bk][:, (g % 2) * 256:(g % 2 + 1) * 256], ub[:, g * 128:(g + 1) * 128],
                        xddm[:, 4 * g:4 * g + 4, :], ["ub", "xddm"], [f"ps{bk}"])
                dve(lambda e, b=b: e.tensor_tensor(out=hv(hTs[:]), in0=hv(hTs[:]),
                                                   in1=cds[:, b, :].unsqueeze(2).to_broadcast([128, 16, 64]),
                                                   op=ALU.mult), r=["hTs", "cds"], w=["hTs"])
                for j in range(2):
                    dve(lambda e, j=j: e.tensor_tensor(out=hTs[:, j * 512:(j + 1) * 512],
                                                       in0=hTs[:, j * 512:(j + 1) * 512], in1=psb[3 + j][:], op=ALU.add),
                        r=["hTs", f"ps{3 + j}"], w=["hTs"])
                for j in range(8):
                    bk = 3 + j // 4
                    pe(lambda e, j=j, bk=bk: e.transpose(out=psb[bk][:, (j % 4) * 128:(j % 4 + 1) * 128],
                                                         in_=hTs[:, j * 128:(j + 1) * 128], identity=T_IDF),
                       r=["hTs", "mtab"], w=[f"ps{bk}"])
                for j in range(2):
                    dve(lambda e, j=j: e.tensor_copy(out=sso[:, 4 * j:4 * j + 4, :],
                                                     in_=psb[3 + j][:].rearrange("p (a c) -> p a c", a=4)),
                        r=[f"ps{3 + j}"], w=["sso"])
                sp(lambda e, b=b: e.dma_start(out=ssms_out[b].rearrange("(j q) n -> q j n", q=128), in_=sso[:]),
                   r=["sso"], w=[("ssms", b)])
        for j in range(2):
            dve(lambda e, j=j: e.tensor_tensor(
                out=yb[:, 8 * j:8 * j + 8, :], in0=hv(psb[1 + j][:]),
                in1=ea[:, 8 * j:8 * j + 8].unsqueeze(2).to_broadcast([128, 8, 64]), op=ALU.mult),
                r=[f"ps{1 + j}", "ea"], w=[f"yb{j}"])
            dve(lambda e, j=j: e.tensor_tensor(out=yb[:, 8 * j:8 * j + 8, :], in0=yb[:, 8 * j:8 * j + 8, :],
                                               in1=hv(psb[6 + j][:]), op=ALU.add), r=[f"yb{j}", f"ps{6 + j}"],
                w=[f"yb{j}"])
        if not smp:
            act(lambda e: e.activation(out=hTb[:], in_=hT[:], func=AF.Copy), r=["hT"], w=["hTb"])
        pool(lambda e: e.tensor_tensor(out=y2[:], in0=hv(u[:, 0:1024]),
                                       in1=mv[:, 32:48].unsqueeze(2).to_broadcast([128, 16, 64]), op=ALU.mult),
             r=["u0", "mv"], w=["y2"])
        dve(lambda e: e.tensor_tensor(out=yb[:], in0=yb[:], in1=y2[:], op=ALU.add), r=["yb0", "yb1", "y2"],
            w=["yb0", "yb1"])
        act(lambda e: e.activation(out=zt[:], in_=zt[:], func=AF.Silu), r=["zt"], w=["zt"])
        ybf = yb.rearrange("p h d -> p (h d)")
        dve(lambda e: e.tensor_tensor(out=ybf, in0=ybf, in1=zt[:], op=ALU.mult), r=["yb0", "yb1", "zt"],
            w=["yb0", "yb1"])
        pool(lambda e: e.tensor_tensor(out=msq[:], in0=ybf, in1=ybf, op=ALU.mult), r=["yb0", "yb1"], w=["msq"])
        dve(lambda e: e.tensor_reduce(out=mss[:], in_=msq[:].rearrange("p (g c) -> p g c", g=4), axis=AX.X,
                                      op=ALU.add), r=["msq"], w=["mss"])
        dve(lambda e: e.tensor_scalar(out=mss[:], in0=mss[:], scalar1=1.0 / 256, scalar2=EPS, op0=ALU.mult,
                                      op1=ALU.add), r=["mss"], w=["mss"])
        act(lambda e: e.activation(out=mss[:], in_=mss[:], func=AF.Sqrt), r=["mss"], w=["mss"])
        dve(lambda e: e.reciprocal(out=mss[:], in_=mss[:]), r=["mss"], w=["mss"])
        dve(lambda e: e.tensor_tensor(out=msq[:].rearrange("p (g c) -> p g c", g=4),
                                      in0=ybf.rearrange("p (g c) -> p g c", g=4),
                                      in1=mss[:].unsqueeze(2).to_broadcast([128, 4, 256]), op=ALU.mult),
            r=["yb0", "yb1", "mss"], w=["msq"])
        dve(lambda e: e.tensor_tensor(out=Mb[:], in0=msq[:], in1=gs[:], op=ALU.mult), r=["msq", "gs"], w=["Mb"])
        sp(lambda e, r0=r0: e.dma_start(out=Mscr[r0:r0 + 128, :], in_=Mb[:]), r=["Mb"], w=[("Mscr", t)])
        if t == NT - 2:
            for j in range(8):
                bk = 3 + j // 4
                pe(lambda e, j=j, bk=bk: e.transpose(out=psb[bk][:, (j % 4) * 128:(j % 4 + 1) * 128],
                                                     in_=hT[:, j * 128:(j + 1) * 128], identity=T_IDF),
                   r=["hT", "mtab"], w=[f"ps{bk}"])
            for j in range(2):
                dve(lambda e, j=j: e.tensor_copy(out=sso[:, 4 * j:4 * j + 4, :],
                                                 in_=psb[3 + j][:].rearrange("p (a c) -> p a c", a=4)),
                    r=[f"ps{3 + j}"], w=["sso"])
            sp(lambda e: e.dma_start(out=ssmp_out.rearrange("(j q) n -> q j n", q=128), in_=sso[:]),
               r=["sso"], w=["ssmp"])


    B.release(m0)
    m4 = B.mark()
    atab = B.sb("atab", [128, AT_W])
    sp(lambda e: e.dma_start(out=atab[:], in_=atab_in), w=["atab"])
    o_ = [0]

    def tab(w):
        a = atab[:, o_[0]:o_[0] + w]
        o_[0] += w
        return a
    A_GQ = tab(1024)
    A_GK0 = tab(128)
    A_COVER = tab(32)
    A_BASEC = tab(2048).rearrange("p (k c) -> p k c", k=2)
    A_TC = tab(128)
    A_BASE = tab(2048).rearrange("p (k c) -> p k c", k=2)
    A_BASEK = tab(2048).rearrange("p (k c) -> p k c", k=2)
    A_BASEA = tab(2048).rearrange("p (k c) -> p k c", k=2)
    A_CVEC = tab(65 * 16).rearrange("p (d h) -> p d h", h=16)
    A_FT = tab(62)
    A_UT = tab(62)
    A_PET = tab(64).rearrange("p (k j) -> p k j", k=2)
    assert o_[0] == AT_W, o_[0]
    EX = B.sb("EX", [32, 16, 128], BF16)
    sp(lambda e: e.dma_start(out=EX, in_=ex_in), w=["EX"])
    W1 = B.sb("W1", [128, 2, 32, 128], BF16)
    for kv in range(2):
        S.dma("pool", lambda e, kv=kv: e.dma_start(out=W1[:, kv], in_=w1_in[:, kv]), writes=[f"W1{kv}"])
    W2 = B.sb("W2", [128, 2, 64], BF16)
    S.dma("pool", lambda e: e.dma_start(out=W2[:], in_=w2_in), writes=["W2"])
    peTb = B.sb("peTb", [128, 2, 32], BF16)
    dve(lambda e: e.tensor_copy(out=peTb[:], in_=A_PET), r=["atab"], w=["peTb"])

    qT = B.sb("qT", [128, 16, 8, 128], BF16)
    ckT = B.sb("ckT", [128, TP], BF16)
    cvT = B.sb("cvT", [128, TP], BF16)
    skT = B.sb("skT", [128, TP], BF16)
    wkT = B.sb("wkT", [128, TP], BF16)
    svA = B.sb("svA", [128, 16, 2, 65], BF16)
    wvA = B.sb("wvA", [128, 16, 2, 65], BF16)
    gates = B.sb("gates", [128, 16, 48])
    qf = B.sb("qf", [128, 1024])
    qb = B.sb("qb", [128, 1024], BF16)
    kvf = B.sb("kvf", [128, 768])
    kvb = B.sb("kvb", [128, 768], BF16)
    q_sq = B.sb("q_sq", [128, 1024])
    q_ss = B.sb("q_ss", [128, 16])
    q_tmp = B.sb("q_tmp", [128, 1024])
    dve(lambda e: e.memset(svA[:], 1.0), w=["svA"])
    dve(lambda e: e.memset(wvA[:], 1.0), w=["wvA"])

    for t in range(16):
        r0 = t * 128
        sp(lambda e, r0=r0: e.dma_start(out=qf[:], in_=proj[r0:r0 + 128, C_Q:C_Q + 1024]), w=["qf"])
        sp(lambda e, r0=r0: e.dma_start(out=kvf[:], in_=kvn[r0:r0 + 128, :]), w=["kvf"])
        sp(lambda e, r0=r0, t=t: e.dma_start(out=gates[:, t, :], in_=proj[r0:r0 + 128, C_AG:C_AG + 48]),
           w=[("gates", t)])
        act(lambda e, t=t: e.activation(out=gates[:, t, :], in_=gates[:, t, :], func=AF.Sigmoid),
            r=[("gates", t)], w=[("gates", t)])
        rms_heads(qf[:], A_GQ, 16, 0.125, ["qf", "atab"], "qf", q_sq, q_ss, q_tmp)
        act(lambda e: e.activation(out=qb[:].rearrange("p (g k d) -> p k g d", g=8, k=2),
                                   in_=qf[:].rearrange("p (k g d) -> p k g d", k=2, g=8), func=AF.Copy),
            r=["qf"], w=["qb"])
        act(lambda e: e.activation(out=kvb[:], in_=kvf[:], func=AF.Copy), r=["kvf"], w=["kvb"])
        dve(lambda e, t=t: e.tensor_copy(out=svA[:, t, :, 0:64], in_=kvb[:, 384:512].rearrange("p (k d) -> p k d", k=2)),
            r=["kvb"], w=["svA"])
        dve(lambda e, t=t: e.tensor_copy(out=wvA[:, t, :, 0:64], in_=kvb[:, 640:768].rearrange("p (k d) -> p k d", k=2)),
            r=["kvb"], w=["wvA"])
        pq = bank_bf(0)
        for g in range(8):
            src = qb[:, g * 128:(g + 1) * 128]
            pe(lambda e, g=g, src=src, pq=pq: e.transpose(out=pq[:, g * 128:(g + 1) * 128], in_=src, identity=ident[:]),
               r=["qb", "ident"], w=["ps0"])
        dve(lambda e, pq=pq, t=t: e.tensor_copy(out=qT[:, t, :, :], in_=pq.rearrange("p (g c) -> p g c", g=8)),
            r=["ps0"], w=[("qT", t)])
        pk = bank_bf(1)
        for n_, c0 in enumerate((0, 128, 256, 512)):
            pe(lambda e, n_=n_, c0=c0, pk=pk: e.transpose(out=pk[:, n_ * 128:(n_ + 1) * 128], in_=kvb[:, c0:c0 + 128],
                                                          identity=ident[:]), r=["kvb", "ident"], w=["ps1"])
        for n_, dst in enumerate((ckT, cvT, skT, wkT)):
            act(lambda e, n_=n_, dst=dst, pk=pk, r0=r0: e.activation(out=dst[:, r0:r0 + 128],
                                                                  in_=pk[:, n_ * 128:(n_ + 1) * 128], func=AF.Copy),
                r=["ps1"], w=[("kT", n_, t)])
    kT_keys = lambda n_: [("kT", n_, t) for t in range(16)]

    gT = B.sb("gT", [128, 4, 128], BF16)
    xb_ = B.sb("xb_", [128, 512])
    x3_ = B.sb("x3_", [128, 512])
    peb = B.sb("peb", [128, 2])
    kcf = B.sb("kcf", [128, 256])
    kcb = B.sb("kcb", [128, 128], BF16)
    kcT = B.sb("kcT", [128, 128], BF16)
    vcA = B.sb("vcA", [128, 2, 97], BF16)
    dve(lambda e: e.memset(vcA[:], 1.0), w=["vcA"])
    dve(lambda e: e.memset(gT[:], 0.0), w=["gT"])
    for kv in range(2):
        for j in range(32):
            pe(lambda e, kv=kv, j=j: e.matmul(psb[2][:, kv:kv + 1], lhsT=W1[0:64, kv, j, :], rhs=peTb[0:64, kv, j:j + 1],
                                              start=(j == 0), stop=(j == 31)), r=[f"W1{kv}", "peTb"], w=["ps2"])
    dve(lambda e: e.tensor_copy(out=peb[:], in_=psb[2][:, 0:2]), r=["ps2"], w=["peb"])
    for kv, srcT in enumerate((ckT, cvT)):
        for kvh in range(2):
            grp = kv * 2 + kvh
            rows = slice(kvh * 64, (kvh + 1) * 64)
            sv_ = srcT[:].rearrange("p (n s) -> p n s", s=16)
            for j in range(32):
                pe(lambda e, kv=kv, j=j, grp=grp, rows=rows, sv_=sv_: e.matmul(
                    psb[3][:, grp * 127:(grp + 1) * 127], lhsT=W1[rows, kv, j, :],
                    rhs=sv_[rows, (j // 16):(j // 16) + 127, j % 16], start=(j == 0), stop=(j == 31)),
                    r=[f"W1{kv}"] + kT_keys(kv), w=["ps3"])
            act(lambda e, grp=grp, kv=kv: e.activation(out=xb_[:, grp * 127:(grp + 1) * 127],
                                                       in_=psb[3][:, grp * 127:(grp + 1) * 127], func=AF.Identity,
                                                       bias=peb[:, kv:kv + 1]), r=["ps3", "peb"], w=["xb_"])
    dve(lambda e: e.tensor_tensor(out=x3_[:, 0:508], in0=xb_[:, 0:508], in1=xb_[:, 0:508], op=ALU.mult), r=["xb_"], w=["x3_"])
    dve(lambda e: e.tensor_tensor(out=x3_[:, 0:508], in0=x3_[:, 0:508], in1=xb_[:, 0:508], op=ALU.mult), r=["x3_", "xb_"], w=["x3_"])
    dve(lambda e: e.scalar_tensor_tensor(out=x3_[:, 0:508], in0=x3_[:, 0:508], scalar=0.044715, in1=xb_[:, 0:508],
                                         op0=ALU.mult, op1=ALU.add), r=["x3_", "xb_"], w=["x3_"])
    act(lambda e: e.activation(out=x3_[:, 0:508], in_=x3_[:, 0:508], func=AF.Sigmoid, scale=1.5957691216057308),
        r=["x3_"], w=["x3_"])
    dve(lambda e: e.tensor_tensor(out=gT[:, :, 0:127], in0=xb_[:, 0:508].rearrange("p (g n) -> p g n", g=4),
                                  in1=x3_[:, 0:508].rearrange("p (g n) -> p g n", g=4), op=ALU.mult),
        r=["x3_", "xb_"], w=["gT"])
    for kv in range(2):
        for kvh in range(2):
            grp = kv * 2 + kvh
            mm1(psb[2][0:127, grp * 64:(grp + 1) * 64], gT[:, grp, 0:127], W2[:, kv, :], ["gT", "W2"], ["ps2"])
    dve(lambda e: e.memset(kcf[:], 0.0), w=["kcf"])
    dve(lambda e: e.tensor_copy(out=kcf[0:127, :], in_=psb[2][0:127, 0:256]), r=["ps2"], w=["kcf"])
    rms_heads(kcf[:, 0:128], A_GK0, 2, 1.0, ["kcf", "atab"], "kcf", q_sq, q_ss, q_tmp)
    act(lambda e: e.activation(out=kcb[:], in_=kcf[:, 0:128], func=AF.Copy), r=["kcf"], w=["kcb"])
    pe(lambda e: e.transpose(out=bank_bf(2)[:, 0:128], in_=kcb[:], identity=ident[:]), r=["kcb", "ident"], w=["ps2"])
    dve(lambda e: e.tensor_copy(out=kcT[:], in_=bank_bf(2)[:, 0:128]), r=["ps2"], w=["kcT"])
    dve(lambda e: e.tensor_copy(out=vcA[:, :, 0:64], in_=kcf[:, 128:256].rearrange("p (k d) -> p k d", k=2)),
        r=["kcf"], w=["vcA"])
    for kvh in range(2):
        dve(lambda e, kvh=kvh: e.tensor_copy(out=vcA[:, kvh, 65:97], in_=A_COVER), r=["atab"], w=["vcA"])

    S.barrier()
    tmps = [B.sb(f"tmps{i}", [128, 1024]) for i in range(2)]
    pexp = [B.sb(f"pexp{i}", [128, 8, 128], BF16) for i in range(2)]
    mk = B.sb("mk", [128, 128])
    Aacc = B.sb("Aacc", [128, 1024])
    Ab = B.sb("Ab", [128, 1024], BF16)
    rd = B.sb("rd", [128, 8])
    gsc = B.sb("gsc", [128, 8])
    impt = B.sb("impt", [128, 8, 32])
    imp = B.sb("imp", [128, 32])
    cmpb = B.sb("cmpb", [128, 32, 32])
    cnt = B.sb("cnt", [128, 32])
    nsel = B.sb("nsel", [128, 32], BF16)
    nsTg = B.sb("nsTg", [32, 8, 128], BF16)
    otmp = B.sb("otmp", [128, 8, 64])
    it_ = [0]

    def attn_block(i, kvh, keysT, nk, Vt, nv, base_ap, delta, out_banks, first, extra=None, selmask=None):
        rows = slice(kvh * 64, (kvh + 1) * 64)
        sb_ = it_[0] % 2
        it_[0] += 1
        sbanks = (0, 1) if sb_ == 0 else (2, 3)
        for hb in range(2):
            bk = sbanks[hb]
            pe(lambda e, bk=bk, hb=hb: e.matmul(
                psb[bk][0:nk, :], lhsT=keysT, rhs=qT[rows, i, 4 * hb:4 * hb + 4, :],
                start=True, stop=(selmask is None)), r=["kT_any", ("qT", i)], w=[f"ps{bk}"])
            if selmask is not None:
                pe(lambda e, bk=bk, hb=hb: e.matmul(
                    psb[bk][0:nk, :], lhsT=selmask, rhs=nsTg[:, 4 * hb:4 * hb + 4, :], start=False, stop=True),
                    r=["EX", "nsTg"], w=[f"ps{bk}"])
        tm = tmps[sb_]
        px = pexp[sb_]
        dve(lambda e: e.tensor_tensor(out=tm[0:nk, :], in0=psall[0:nk, sbanks[0] * 512:sbanks[0] * 512 + 1024],
                                      in1=base_ap, op=ALU.add),
            r=[f"ps{sbanks[0]}", f"ps{sbanks[1]}", "atab"], w=[f"tmps{sb_}"])
        if extra is not None:
            pool(lambda e: e.tensor_tensor(out=tm[0:nk, :].rearrange("p (g c) -> p g c", g=8),
                                           in0=tm[0:nk, :].rearrange("p (g c) -> p g c", g=8),
                                           in1=extra[0:nk, :].unsqueeze(1).to_broadcast([nk, 8, 128]), op=ALU.add),
                 r=[f"tmps{sb_}", "mk"], w=[f"tmps{sb_}"])
        for g in range(8):
            act(lambda e, g=g: e.activation(out=px[0:nk, g, :], in_=tm[0:nk, g * 128:(g + 1) * 128], func=AF.Exp,
                                            bias=A_CVEC[0:nk, delta, kvh * 8 + g:kvh * 8 + g + 1]),
                r=[f"tmps{sb_}", "atab"], w=[f"pexp{sb_}"])
        for g in range(8):
            bk = out_banks[g // 4]
            pe(lambda e, g=g, bk=bk: e.matmul(
                psb[bk][:, (g % 4) * nv:(g % 4 + 1) * nv], lhsT=px[0:nk, g, :], rhs=Vt,
                start=(first and g % 4 == 0), stop=False, skip_group_check=True),
                r=[f"pexp{sb_}", "V_any"], w=[f"ps{bk}"])

    def finish_branch(i, kvh, out_banks, nv, br, accumulate):
        for hb in range(2):
            bk = out_banks[hb]
            ov = psb[bk][:, 0:4 * nv].rearrange("p (g c) -> p g c", g=4)
            dve(lambda e, hb=hb, ov=ov: e.tensor_scalar(out=rd[:, 4 * hb:4 * hb + 4], in0=ov[:, :, 64], scalar1=1e-30,
                                                        scalar2=None, op0=ALU.max), r=[f"ps{bk}"], w=["rd"])
        dve(lambda e: e.reciprocal(out=rd[:], in_=rd[:]), r=["rd"], w=["rd"])
        gv = gates[:, i, kvh * 24:(kvh + 1) * 24].rearrange("p (g b) -> p g b", b=3)[:, :, br]
        dve(lambda e: e.tensor_tensor(out=gsc[:], in0=rd[:], in1=gv, op=ALU.mult), r=["rd", ("gates", i)], w=["gsc"])
        for hb in range(2):
            bk = out_banks[hb]
            ov = psb[bk][:, 0:4 * nv].rearrange("p (g c) -> p g c", g=4)
            dst = Aacc[:, kvh * 512 + hb * 256:kvh * 512 + (hb + 1) * 256].rearrange("p (g d) -> p g d", g=4)
            if not accumulate:
                dve(lambda e, ov=ov, dst=dst, hb=hb: e.tensor_tensor(
                    out=dst, in0=ov[:, :, 0:64], in1=gsc[:, 4 * hb:4 * hb + 4].unsqueeze(2).to_broadcast([128, 4, 64]),
                    op=ALU.mult), r=[f"ps{bk}", "gsc"], w=["Aacc"])
            else:
                dve(lambda e, ov=ov, hb=hb: e.tensor_tensor(
                    out=otmp[:, 4 * hb:4 * hb + 4, :], in0=ov[:, :, 0:64],
                    in1=gsc[:, 4 * hb:4 * hb + 4].unsqueeze(2).to_broadcast([128, 4, 64]), op=ALU.mult),
                    r=[f"ps{bk}", "gsc"], w=["otmp"])
                pool(lambda e, dst=dst, hb=hb: e.tensor_tensor(out=dst, in0=dst, in1=otmp[:, 4 * hb:4 * hb + 4, :],
                                                               op=ALU.add), r=["otmp", "Aacc"], w=["Aacc"])

    for i in ([] if "nsap" in DEV_SKIP else range(16)):
        pool(lambda e, i=i: e.tensor_scalar(out=mk[:], in0=A_TC, scalar1=float(128 * i), scalar2=-30000.0,
                                            op0=ALU.is_gt, op1=ALU.mult), r=["atab"], w=["mk"])
        for kvh in range(2):
            rows = slice(kvh * 64, (kvh + 1) * 64)
            attn_block(i, kvh, kcT[rows, 0:127], 127, vcA[0:127, kvh, :], 97, A_BASEC[0:127, kvh, :], i, (4, 5), True,
                       extra=mk)
            for hb in range(2):
                bk = 4 + hb
                ov = psb[bk][:, 0:388].rearrange("p (g c) -> p g c", g=4)
                dve(lambda e, hb=hb, ov=ov: e.tensor_scalar(out=rd[:, 4 * hb:4 * hb + 4], in0=ov[:, :, 64],
                                                            scalar1=1e-30, scalar2=None, op0=ALU.max),
                    r=[f"ps{bk}"], w=["rd"])
            dve(lambda e: e.reciprocal(out=rd[:], in_=rd[:]), r=["rd"], w=["rd"])
            for hb in range(2):
                bk = 4 + hb
                ov = psb[bk][:, 0:388].rearrange("p (g c) -> p g c", g=4)
                dve(lambda e, hb=hb, ov=ov: e.tensor_tensor(
                    out=impt[:, 4 * hb:4 * hb + 4, :], in0=ov[:, :, 65:97],
                    in1=rd[:, 4 * hb:4 * hb + 4].unsqueeze(2).to_broadcast([128, 4, 32]), op=ALU.mult),
                    r=[f"ps{bk}", "rd"], w=["impt"])
            dve(lambda e: e.tensor_reduce(out=imp[:], in_=impt[:].rearrange("p g j -> p j g"), axis=AX.X, op=ALU.add),
                r=["impt"], w=["imp"])
            finish_branch(i, kvh, (4, 5), 97, 0, False)
            dve(lambda e, i=i: e.tensor_tensor(out=imp[:], in0=imp[:], in1=A_FT[:, 30 - 2 * i:62 - 2 * i], op=ALU.max),
                r=["imp", "atab"], w=["imp"])
            dve(lambda e: e.memset(imp[:, 0:1], 1e30), r=["imp"], w=["imp"])
            dve(lambda e, i=i: e.tensor_tensor(out=imp[:], in0=imp[:], in1=A_UT[:, 30 - 2 * i:62 - 2 * i], op=ALU.min),
                r=["imp", "atab"], w=["imp"])
            dve(lambda e: e.tensor_tensor(out=cmpb[:], in0=imp[:].unsqueeze(1).to_broadcast([128, 32, 32]),
                                          in1=imp[:].unsqueeze(2).to_broadcast([128, 32, 32]), op=ALU.is_gt),
                r=["imp"], w=["cmpb"])
            dve(lambda e: e.tensor_reduce(out=cnt[:], in_=cmpb[:], axis=AX.X, op=ALU.add), r=["cmpb"], w=["cnt"])
            dve(lambda e: e.tensor_scalar(out=nsel[:], in0=cnt[:], scalar1=15.5, scalar2=-30000.0, op0=ALU.is_gt,
                                          op1=ALU.mult), r=["cnt"], w=["nsel"])
            pe(lambda e: e.transpose(out=bank_bf(6)[0:32, 0:128], in_=nsel[:], identity=ident[:]),
               r=["nsel", "ident"], w=["ps6"])
            dve(lambda e: e.tensor_copy(out=nsTg[:], in_=bank_bf(6)[0:32, 0:128].unsqueeze(1).to_broadcast([32, 8, 128])),
                r=["ps6"], w=["nsTg"])
            for kt in range(i + 1):
                attn_block(i, kvh, skT[rows, kt * 128:(kt + 1) * 128], 128, svA[:, kt, kvh, :], 65,
                           (A_BASEK if kt == i else A_BASE)[:, kvh, :], i - kt, (6, 7), kt == 0,
                           selmask=EX[:, kt, :])
            finish_branch(i, kvh, (6, 7), 65, 1, True)
            k0 = max(0, i - 4)
            for kt in range(k0, i + 1):
                bs = A_BASEK if kt == i else (A_BASEA if kt == i - 4 else A_BASE)
                attn_block(i, kvh, wkT[rows, kt * 128:(kt + 1) * 128], 128, wvA[:, kt, kvh, :], 65,
                           bs[:, kvh, :], i - kt, (4, 5), kt == k0)
            finish_branch(i, kvh, (4, 5), 65, 2, True)
        act(lambda e: e.activation(out=Ab[:], in_=Aacc[:], func=AF.Copy), r=["Aacc"], w=["Ab"])
        sp(lambda e, i=i: e.dma_start(out=Ascr[i * 128:(i + 1) * 128, :], in_=Ab[:]), r=["Ab"], w=[("Ascr", i)])
    B.release(m4)


    m4s = B.mark()
    stab = B.sb("stab", [128, ST_W])
    sp(lambda e: e.dma_start(out=stab[:], in_=stab_in), w=["stab"])
    o_[0] = 0

    def stb(w):
        a = stab[:, o_[0]:o_[0] + w]
        o_[0] += w
        return a
    S_GQ = stb(1024)
    S_GK0 = stb(512)
    S_BIASC = stb(512)
    S_COVER = stb(516).rearrange("p (n j) -> p n j", n=4)
    S_BASE2 = stb(128).rearrange("p (k c) -> p k c", k=2)
    S_CVEC2 = stb(1040).rearrange("p (t k g) -> p t k g", t=65, k=2)
    S_BIASW = stb(640).rearrange("p (t k c) -> p t k c", t=5, k=2)
    S_FS = stb(129)
    S_PET = stb(64).rearrange("p (k j) -> p k j", k=2)
    S_PIOTA = stb(1)
    assert o_[0] <= ST_W, o_[0]
    EXW = B.sb("EXW", [128, 32, 128], BF16)
    sp(lambda e: e.dma_start(out=EXW, in_=exw_in), w=["EXW"])
    W1s = B.sb("W1s", [128, 2, 32, 128], BF16)
    for kv in range(2):
        S.dma("pool", lambda e, kv=kv: e.dma_start(out=W1s[:, kv], in_=w1_in[:, kv]), writes=[f"W1s{kv}"])
    W2s = B.sb("W2s", [128, 2, 64], BF16)
    S.dma("pool", lambda e: e.dma_start(out=W2s[:], in_=w2_in), writes=["W2s"])
    peTs = B.sb("peTs", [128, 2, 32], BF16)
    dve(lambda e: e.tensor_copy(out=peTs[:], in_=S_PET), r=["stab"], w=["peTs"])
    pebs = B.sb("pebs", [128, 2])
    for kv in range(2):
        for j in range(32):
            pe(lambda e, kv=kv, j=j: e.matmul(psb[2][:, kv:kv + 1], lhsT=W1s[0:64, kv, j, :], rhs=peTs[0:64, kv, j:j + 1],
                                              start=(j == 0), stop=(j == 31)), r=[f"W1s{kv}", "peTs"], w=["ps2"])
    dve(lambda e: e.tensor_copy(out=pebs[:], in_=psb[2][:, 0:2]), r=["ps2"], w=["pebs"])

    xbs = B.sb("xbs", [128, 4, 512])
    x3s = B.sb("x3s", [128, 4, 512])
    s_tmp_early = x3s[:, 2:4, :].rearrange("p a n -> p (a n)")
    pti = B.sb("pti", [128, 1024], I32)
    idx = pti
    sp(lambda e: e.dma_start(out=pti[:], in_=pt_in), w=["pti"])
    ptf = s_tmp_early
    dve(lambda e: e.tensor_copy(out=ptf[:], in_=pti[:]), r=["pti"], w=["ptf"])
    dve(lambda e: e.tensor_scalar(out=ptf[:], in0=ptf[:], scalar1=128.0, scalar2=S_PIOTA, op0=ALU.mult, op1=ALU.add),
        r=["ptf", "stab"], w=["ptf"])
    dve(lambda e: e.tensor_copy(out=idx[:], in_=ptf[:]), r=["ptf"], w=["pti"])

    qfs = xbs[:, 0:2, :].rearrange("p a n -> p (a n)")
    kvfs = xbs[:, 2:4, :].rearrange("p a n -> p (a n)")[:, 0:768]
    s_sq = x3s[:, 0:2, :].rearrange("p a n -> p (a n)")
    s_tmp = x3s[:, 2:4, :].rearrange("p a n -> p (a n)")
    qbs = B.sb("qbs", [128, 1024], BF16)
    s_ss = B.sb("s_ss", [128, 16])
    qTs = B.sb("qTs", [128, 8, 128], BF16)
    qTs2 = B.sb("qTs2", [128, 16, 64], BF16)
    kvbs = B.sb("kvbs", [128, 768], BF16)
    newT = B.sb("newT", [128, 4, 128], BF16)
    sp(lambda e: e.dma_start(out=qfs, in_=proj[TP:TT, C_Q:C_Q + 1024]), w=["qfs"])
    sp(lambda e: e.dma_start(out=kvfs, in_=kvn[TP:TT, :]), w=["kvfs"])
    rms_heads(qfs, S_GQ, 16, 0.125, ["qfs", "stab"], "qfs", s_sq, s_ss, s_tmp)
    act(lambda e: e.activation(out=qbs[:].rearrange("p (g k d) -> p k g d", g=8, k=2),
                               in_=qfs.rearrange("p (k g d) -> p k g d", k=2, g=8), func=AF.Copy),
        r=["qfs"], w=["qbs"])
    act(lambda e: e.activation(out=kvbs[:], in_=kvfs, func=AF.Copy), r=["kvfs"], w=["kvbs"])
    pq = bank_bf(0)
    for g in range(8):
        pe(lambda e, g=g, pq=pq: e.transpose(out=pq[:, g * 128:(g + 1) * 128], in_=qbs[:, g * 128:(g + 1) * 128],
                                             identity=ident[:]), r=["qbs", "ident"], w=["ps0"])
    dve(lambda e, pq=pq: e.tensor_copy(out=qTs[:], in_=pq.rearrange("p (g c) -> p g c", g=8)), r=["ps0"], w=["qTs"])
    dve(lambda e: e.tensor_copy(out=qTs2[:].rearrange("p b (g t) -> p b g t", g=8),
                                in_=qTs[:].rearrange("p g (b t) -> p b g t", b=16)), r=["qTs"], w=["qTs2"])
    pk = bank_bf(1)
    for n_, c0 in enumerate((0, 128, 256, 512)):
        pe(lambda e, n_=n_, c0=c0, pk=pk: e.transpose(out=pk[:, n_ * 128:(n_ + 1) * 128], in_=kvbs[:, c0:c0 + 128],
                                                      identity=ident[:]), r=["kvbs", "ident"], w=["ps1"])
    dve(lambda e, pk=pk: e.tensor_copy(out=newT[:], in_=pk[:, 0:512].rearrange("p (n c) -> p n c", n=4)),
        r=["ps1"], w=["newT"])

    kT3 = B.sb("kT3", [128, 3, 8320], BF16)
    svAs = B.sb("svAs", [128, 65, 2, 65], BF16)
    dve(lambda e: e.memset(svAs[:], 1.0), w=["svAs"])
    pgf = [B.sb(f"pgf{i}", [128, 512]) for i in range(2)]
    pgb = [B.sb(f"pgb{i}", [128, 512], BF16) for i in range(2)]
    st8 = B.sb("st8", [8, 768 + 48])
    cwf = B.sb("cwf", [128, 4, 256])
    cwb = B.sb("cwb", [128, 4, 256], BF16)
    wkTs = B.sb("wkTs", [128, 640], BF16)
    wvAs = B.sb("wvAs", [128, 5, 2, 65], BF16)
    dve(lambda e: e.memset(wvAs[:], 1.0), w=["wvAs"])
    gTs = B.sb("gTs", [128, 4, 512], BF16)
    kcfs = B.sb("kcfs", [128, 4, 128])
    kcbs = B.sb("kcbs", [128, 4, 128], BF16)
    kcTs = B.sb("kcTs", [128, 512], BF16)
    vcAs = B.sb("vcAs", [128, 4, 2, 194], BF16)
    dve(lambda e: e.memset(vcAs[:], 1.0), w=["vcAs"])
    for kvh in range(2):
        dve(lambda e, kvh=kvh: e.tensor_copy(out=vcAs[:, :, kvh, 65:194], in_=S_COVER), r=["stab"], w=["vcAs"])
    S.barrier()
    dve(lambda e: e.memset(xbs[:], 0.0), w=["xbs"])
    tmpS = [B.sb(f"tmpS{i}", [128, 512]) for i in range(2)]
    pS = [B.sb(f"pS{i}", [128, 512], BF16) for i in range(2)]
    gts = B.sb("gts", [8, 48])
    rds = B.sb("rds", [8, 16])
    gss = B.sb("gss", [8, 16])
    imps = B.sb("imps", [8, 8, 129])
    impv = B.sb("impv", [8, 129])
    cmps = B.sb("cmps", [8, 43, 129], BF16)
    cnts = B.sb("cnts", [8, 129])
    nsels = B.sb("nsels", [8, 136], BF16)
    nselW = B.sb("nselW", [8, 2, 2, 64], BF16)
    nsTs = B.sb("nsTs", [128, 2, 8, 8], BF16)
    As = B.sb("As", [8, 1024])
    Abs_ = B.sb("Abs_", [8, 1024], BF16)
    ots = B.sb("ots", [8, 16, 64])
    si_ = [0]

    def s_scores(keysT_fn, nkt, kt0, kvh, b, nk, selmask):
        rows = slice(kvh * 64, (kvh + 1) * 64)
        bk = si_[0] % 2
        for q_ in range(nkt):
            kt = kt0 + q_
            pe(lambda e, q_=q_, kt=kt, bk=bk: e.matmul(
                psb[bk][0:nk, q_ * 64:(q_ + 1) * 64], lhsT=keysT_fn(rows, kt), rhs=qTs2[rows, b, :],
                start=True, stop=not (selmask and kt < 64 and "s_mask" not in DEV_SKIP)), r=["keys_any", "qTs2"], w=[f"ps{bk}"])
            if selmask and kt < 64 and "s_mask" not in DEV_SKIP:
                lt, rt = EXW[rows, kt % 32, :], nsTs[rows, kt // 32, :, :]
                pe(lambda e, q_=q_, bk=bk, lt=lt, rt=rt: e.matmul(
                    psb[bk][0:nk, q_ * 64:(q_ + 1) * 64], lhsT=lt, rhs=rt, start=False, stop=True),
                    r=["EXW", "nsTs"], w=[f"ps{bk}"])
        return bk

    def s_pv(bk_unused, px, nkt, kt0, kvh, nk, Vfn, nv, acc_banks, hpb, first_kt):
        for q_ in ([] if "s_pvsel" in DEV_SKIP else range(nkt)):
            kt = kt0 + q_
            for g in range(8):
                hh = kvh * 8 + g if len(acc_banks) * hpb >= 16 else g
                bk = acc_banks[hh // hpb]
                pe(lambda e, q_=q_, kt=kt, g=g, bk=bk, hh=hh: e.matmul(
                    psb[bk][0:8, (hh % hpb) * nv:(hh % hpb + 1) * nv], lhsT=px[0:nk, q_ * 64 + g * 8:q_ * 64 + g * 8 + 8],
                    rhs=Vfn(kt, kvh), start=(kt == first_kt and hh % hpb == 0), stop=False, skip_group_check=True),
                    r=[f"pxS{si_[0] % 2}", "V_any"], w=[f"ps{bk}"])

    for b in ([] if "nsas" in DEV_SKIP else range(DEV_NSEQ)):
        S.barrier()
        for pg in ([] if "s_gather" in DEV_SKIP else range(64)):
            fb = pg % 2
            S.dma("pool", lambda e, b=b, pg=pg, fb=fb: e.indirect_dma_start(
                out=pgf[fb][:], out_offset=None, in_=ckv_in[:, :],
                in_offset=bass.IndirectOffsetOnAxis(ap=idx[:, b * 64 + pg:b * 64 + pg + 1], axis=0)),
                reads=["pti"], writes=[f"pgf{fb}"])
            cb = pg % 2
            act(lambda e, fb=fb, cb=cb: e.activation(out=pgb[cb][:], in_=pgf[fb][:], func=AF.Copy),
                r=[f"pgf{fb}"], w=[f"pgb{cb}"])
            dve(lambda e, pg=pg, cb=cb: e.tensor_copy(out=svAs[:, pg, :, 0:64],
                                                       in_=pgb[cb][:, 384:512].rearrange("p (k d) -> p k d", k=2)),
                 r=[f"pgb{cb}"], w=["svAs"])
            tb = 2 + pg % 2
            pt_ = bank_bf(tb)
            for n_ in range(3):
                pe(lambda e, n_=n_, cb=cb, pt_=pt_: e.transpose(out=pt_[:, n_ * 128:(n_ + 1) * 128],
                                                               in_=pgb[cb][:, n_ * 128:(n_ + 1) * 128], identity=ident[:]),
                   r=[f"pgb{cb}", "ident"], w=[f"ps{tb}"])
            dve(lambda e, pg=pg, pt_=pt_: e.tensor_copy(out=kT3[:, :, pg * 128:(pg + 1) * 128],
                                                        in_=pt_[:, 0:384].rearrange("p (n c) -> p n c", n=3)),
                r=[f"ps{tb}"], w=["kT3"])
        dve(lambda e, b=b: e.tensor_copy(out=kT3[:, :, 8192:8200], in_=newT[:, 0:3, b * 8:(b + 1) * 8]),
            r=["newT"], w=["kT3"])
        sp(lambda e, b=b: e.dma_start(out=st8[:, 0:768], in_=kvn[TP + 8 * b:TP + 8 * b + 8, :]), w=["st8"])
        sp(lambda e, b=b: e.dma_start(out=st8[:, 768:816], in_=proj[TP + 8 * b:TP + 8 * b + 8, C_AG:C_AG + 48]), w=["st8"])
        dve(lambda e: e.tensor_copy(out=svAs[0:8, 64, :, 0:64], in_=st8[:, 384:512].rearrange("p (k d) -> p k d", k=2)),
            r=["st8"], w=["svAs"])
        dve(lambda e: e.tensor_copy(out=wvAs[0:8, 4, :, 0:64], in_=st8[:, 640:768].rearrange("p (k d) -> p k d", k=2)),
            r=["st8"], w=["wvAs"])
        act(lambda e: e.activation(out=gts[:], in_=st8[:, 768:816], func=AF.Sigmoid), r=["st8"], w=["gts"])
        sp(lambda e, b=b: e.dma_start(out=cwf[:], in_=cwin[b].rearrange("(n p) c -> p n c", p=128)), w=["cwf"])
        act(lambda e: e.activation(out=cwb[:], in_=cwf[:], func=AF.Copy), r=["cwf"], w=["cwb"])
        dve(lambda e: e.tensor_copy(out=wvAs[:, 0:4, :, 0:64], in_=cwb[:, :, 128:256].rearrange("p n (k d) -> p n k d", k=2)),
            r=["cwb"], w=["wvAs"])
        pw = bank_bf(2)
        for n_ in range(4):
            pe(lambda e, n_=n_, pw=pw: e.transpose(out=pw[:, n_ * 128:(n_ + 1) * 128], in_=cwb[:, n_, 0:128],
                                                   identity=ident[:]), r=["cwb", "ident"], w=["ps2"])
        dve(lambda e, pw=pw: e.tensor_copy(out=wkTs[:, 0:512], in_=pw[:, 0:512]), r=["ps2"], w=["wkTs"])
        dve(lambda e, b=b: e.tensor_copy(out=wkTs[:, 512:520], in_=newT[:, 3, b * 8:(b + 1) * 8]), r=["newT"], w=["wkTs"])
        S.barrier()
        for kv in ([] if "s_cmp" in DEV_SKIP else range(2)):
            sv_ = kT3[:, kv, :].rearrange("p (n s) -> p n s", s=16)
            for kvh in range(2):
                grp = kv * 2 + kvh
                rows = slice(kvh * 64, (kvh + 1) * 64)
                bk = 2 + grp % 2
                for j in range(32):
                    pe(lambda e, kv=kv, j=j, rows=rows, sv_=sv_, bk=bk: e.matmul(
                        psb[bk][:, 0:511], lhsT=W1s[rows, kv, j, :], rhs=sv_[rows, (j // 16):(j // 16) + 511, j % 16],
                        start=(j == 0), stop=(j == 31)), r=[f"W1s{kv}", "kT3"], w=[f"ps{bk}"])
                act(lambda e, grp=grp, kv=kv, bk=bk: e.activation(out=xbs[:, grp, 0:511], in_=psb[bk][:, 0:511],
                                                                  func=AF.Identity, bias=pebs[:, kv:kv + 1]),
                    r=[f"ps{bk}", "pebs"], w=["xbs"])
        dve(lambda e: e.tensor_tensor(out=x3s[:], in0=xbs[:], in1=xbs[:], op=ALU.mult), r=["xbs"], w=["x3s"])
        pool(lambda e: e.tensor_tensor(out=x3s[:], in0=x3s[:], in1=xbs[:], op=ALU.mult), r=["x3s", "xbs"], w=["x3s"])
        dve(lambda e: e.scalar_tensor_tensor(out=x3s[:].rearrange("p g n -> p (g n)"),
                                             in0=x3s[:].rearrange("p g n -> p (g n)"), scalar=0.044715,
                                             in1=xbs[:].rearrange("p g n -> p (g n)"), op0=ALU.mult, op1=ALU.add),
            r=["x3s", "xbs"], w=["x3s"])
        act(lambda e: e.activation(out=x3s[:], in_=x3s[:], func=AF.Sigmoid, scale=1.5957691216057308),
            r=["x3s"], w=["x3s"])
        dve(lambda e: e.tensor_tensor(out=gTs[:], in0=xbs[:], in1=x3s[:], op=ALU.mult), r=["x3s", "xbs"], w=["gTs"])
        for nt in range(4):
            bk = 2 + nt // 2
            for kv in range(2):
                for kvh in range(2):
                    grp = kv * 2 + kvh
                    c0 = (nt % 2) * 256 + grp * 64
                    mm1(psb[bk][:, c0:c0 + 64], gTs[:, grp, nt * 128:(nt + 1) * 128], W2s[:, kv, :], ["gTs", "W2s"],
                        [f"ps{bk}"])
        for nt in range(4):
            bk = 2 + nt // 2
            c0 = (nt % 2) * 256
            dve(lambda e, nt=nt, bk=bk, c0=c0: e.tensor_copy(out=kcfs[:, nt, :], in_=psb[bk][:, c0:c0 + 128]),
                r=[f"ps{bk}"], w=["kcfs"])
            act(lambda e, nt=nt, bk=bk, c0=c0: e.activation(
                out=vcAs[:, nt, :, 0:64], in_=psb[bk][:, c0 + 128:c0 + 256].rearrange("p (k d) -> p k d", k=2),
                func=AF.Copy), r=[f"ps{bk}"], w=["vcAs"])
        rms_heads(kcfs[:].rearrange("p n c -> p (n c)"), S_GK0, 8, 1.0, ["kcfs", "stab"], "kcfs", s_sq, s_ss, s_tmp)
        act(lambda e: e.activation(out=kcbs[:], in_=kcfs[:], func=AF.Copy), r=["kcfs"], w=["kcbs"])
        pc_ = bank_bf(2)
        for nt in range(4):
            pe(lambda e, nt=nt, pc_=pc_: e.transpose(out=pc_[:, nt * 128:(nt + 1) * 128], in_=kcbs[:, nt, :],
                                                     identity=ident[:]), r=["kcbs", "ident"], w=["ps2"])
        dve(lambda e, pc_=pc_: e.tensor_copy(out=kcTs[:], in_=pc_[:, 0:512]), r=["ps2"], w=["kcTs"])
        S.barrier()
        for kvh in ([] if "s_attn" in DEV_SKIP else range(2)):
            si_[0] += 1
            bk = s_scores(lambda rows, kt: kcTs[rows, kt * 128:(kt + 1) * 128], 4, 0, kvh, b, 128, False)
            tm, px = tmpS[si_[0] % 2], pS[si_[0] % 2]
            dve(lambda e, bk=bk, tm=tm, kvh=kvh: e.tensor_tensor(
                out=tm[:, 0:256].rearrange("p (n c) -> p n c", n=4), in0=psb[bk][:, 0:256].rearrange("p (n c) -> p n c", n=4),
                in1=S_BIASC.rearrange("p (n k c) -> p n k c", n=4, k=2)[:, :, kvh, :], op=ALU.add),
                r=[f"ps{bk}", "stab"], w=[f"tmS{si_[0] % 2}"])
            act(lambda e, tm=tm, px=px: e.activation(out=px[:, 0:256], in_=tm[:, 0:256], func=AF.Exp), r=[f"tmS{si_[0] % 2}"], w=[f"pxS{si_[0] % 2}"])
            for nt in range(4):
                for g in range(8):
                    bk2 = 4 + g // 2
                    pe(lambda e, nt=nt, g=g, bk2=bk2, px=px, kvh=kvh: e.matmul(
                        psb[bk2][0:8, (g % 2) * 194:(g % 2 + 1) * 194], lhsT=px[:, nt * 64 + g * 8:nt * 64 + g * 8 + 8],
                        rhs=vcAs[:, nt, kvh, :], start=(nt == 0 and g % 2 == 0), stop=False, skip_group_check=True),
                        r=[f"pxS{si_[0] % 2}", "vcAs"], w=[f"ps{bk2}"])
            for j in range(4):
                ov = psb[4 + j][0:8, 0:388].rearrange("p (g c) -> p g c", g=2)
                dve(lambda e, j=j, ov=ov: e.tensor_scalar(out=rds[:, 2 * j:2 * j + 2], in0=ov[:, :, 64], scalar1=1e-30,
                                                          scalar2=None, op0=ALU.max), r=[f"ps{4 + j}"], w=["rds"])
            dve(lambda e: e.reciprocal(out=rds[:, 0:8], in_=rds[:, 0:8]), r=["rds"], w=["rds"])
            gv = gts[:, kvh * 24:(kvh + 1) * 24].rearrange("p (g x) -> p g x", x=3)
            dve(lambda e, gv=gv: e.tensor_tensor(out=gss[:, 0:8], in0=rds[:, 0:8], in1=gv[:, :, 0], op=ALU.mult),
                r=["rds", "gts"], w=["gss"])
            for j in range(4):
                ov = psb[4 + j][0:8, 0:388].rearrange("p (g c) -> p g c", g=2)
                dve(lambda e, j=j, ov=ov: e.tensor_tensor(
                    out=imps[:, 2 * j:2 * j + 2, :], in0=ov[:, :, 65:194],
                    in1=rds[:, 2 * j:2 * j + 2].unsqueeze(2).to_broadcast([8, 2, 129]), op=ALU.mult),
                    r=[f"ps{4 + j}", "rds"], w=["imps"])
                dve(lambda e, j=j, ov=ov, kvh=kvh: e.tensor_tensor(
                    out=As[:, kvh * 512 + j * 128:kvh * 512 + (j + 1) * 128].rearrange("p (g d) -> p g d", g=2),
                    in0=ov[:, :, 0:64], in1=gss[:, 2 * j:2 * j + 2].unsqueeze(2).to_broadcast([8, 2, 64]), op=ALU.mult),
                    r=[f"ps{4 + j}", "gss"], w=["As"])
            dve(lambda e: e.tensor_reduce(out=impv[:], in_=imps[:].rearrange("p g j -> p j g"), axis=AX.X, op=ALU.add),
                r=["imps"], w=["impv"])
            dve(lambda e: e.tensor_tensor(out=impv[:], in0=impv[:], in1=S_FS[0:8, :], op=ALU.max), r=["impv", "stab"], w=["impv"])
            for c3 in range(3):
                dve(lambda e, c3=c3: e.tensor_tensor(
                    out=cmps[:], in0=impv[:].unsqueeze(1).to_broadcast([8, 43, 129]),
                    in1=impv[:, c3 * 43:(c3 + 1) * 43].unsqueeze(2).to_broadcast([8, 43, 129]), op=ALU.is_gt),
                    r=["impv"], w=["cmps"])
                dve(lambda e, c3=c3: e.tensor_reduce(out=cnts[:, c3 * 43:(c3 + 1) * 43], in_=cmps[:], axis=AX.X, op=ALU.add),
                    r=["cmps"], w=["cnts"])
            dve(lambda e: e.memset(nsels[:], 0.0), w=["nsels"])
            dve(lambda e: e.tensor_scalar(out=nsels[:, 0:129], in0=cnts[:], scalar1=15.5, scalar2=-30000.0, op0=ALU.is_gt,
                                          op1=ALU.mult), r=["cnts"], w=["nsels"])
            dve(lambda e: e.tensor_copy(out=nselW[:], in_=nsels[:, 0:128].rearrange("p (w j) -> p w j", w=2)
                                        .unsqueeze(2).to_broadcast([8, 2, 2, 64])), r=["nsels"], w=["nselW"])
            for w2 in range(2):
                pe(lambda e, w2=w2: e.transpose(out=bank_bf(3)[:, 8 * w2:8 * w2 + 8],
                                                in_=nselW[:, w2, :, :].rearrange("p a j -> p (a j)"),
                                                identity=ident[0:8, 0:8]), r=["nselW", "ident"], w=["ps3"])
            dve(lambda e: e.tensor_copy(out=nsTs[:], in_=bank_bf(3)[:, 0:16].rearrange("p (w t) -> p w t", w=2)
                                        .unsqueeze(2).to_broadcast([128, 2, 8, 8])), r=["ps3"], w=["nsTs"])
            acc = (4, 5, 6, 7)
            for bt in ([] if "s_sel" in DEV_SKIP else range(9)):
                kt0 = bt * 8
                nkt = 8 if bt < 8 else 1
                nk = 128 if bt < 8 else 8
                si_[0] += 1
                bk = s_scores(lambda rows, kt: kT3[rows, 2, kt * 128:kt * 128 + (128 if kt < 64 else 8)], nkt, kt0, kvh, b,
                              nk, True)
                tm, px = tmpS[si_[0] % 2], pS[si_[0] % 2]
                w_ = nkt * 64
                if bt == 8:
                    dve(lambda e, bk=bk, tm=tm, kvh=kvh: e.tensor_tensor(out=tm[0:8, 0:64], in0=psb[bk][0:8, 0:64],
                                                                         in1=S_BIASW[0:8, 4, kvh, :], op=ALU.add),
                        r=[f"ps{bk}", "stab"], w=[f"tmS{si_[0] % 2}"])
                else:
                    dve(lambda e, bk=bk, tm=tm, kvh=kvh, nk=nk, nkt=nkt, w_=w_: e.tensor_tensor(
                        out=tm[0:nk, 0:w_].rearrange("p (n c) -> p n c", n=nkt),
                        in0=psb[bk][0:nk, 0:w_].rearrange("p (n c) -> p n c", n=nkt),
                        in1=S_BASE2[0:nk, kvh, :].unsqueeze(1).to_broadcast([nk, nkt, 64]), op=ALU.add),
                        r=[f"ps{bk}", "stab"], w=[f"tmS{si_[0] % 2}"])
                if bt < 8:
                  dve(lambda e, tm=tm, kvh=kvh, nk=nk, nkt=nkt, w_=w_, kt0=kt0: e.tensor_tensor(
                    out=tm[0:nk, 0:w_].rearrange("p (n g t) -> p n g t", n=nkt, g=8),
                    in0=tm[0:nk, 0:w_].rearrange("p (n g t) -> p n g t", n=nkt, g=8),
                    in1=S_CVEC2[0:nk, kt0:kt0 + nkt, kvh, :].unsqueeze(3).to_broadcast([nk, nkt, 8, 8]), op=ALU.add),
                    r=[f"tmS{si_[0] % 2}", "stab"], w=[f"tmS{si_[0] % 2}"])
                act(lambda e, tm=tm, px=px, nk=nk, w_=w_: e.activation(out=px[0:nk, 0:w_], in_=tm[0:nk, 0:w_], func=AF.Exp),
                    r=[f"tmS{si_[0] % 2}"], w=[f"pxS{si_[0] % 2}"])
                s_pv(bk, px, nkt, kt0, kvh, nk, lambda kt, kvh_: svAs[0:(128 if kt < 64 else 8), kt, kvh_, :], 65,
                     acc, 2, 0)
            for j in range(4):
                ov = psb[4 + j][0:8, 0:130].rearrange("p (g c) -> p g c", g=2)
                dve(lambda e, j=j, ov=ov: e.tensor_scalar(out=rds[:, 2 * j:2 * j + 2], in0=ov[:, :, 64], scalar1=1e-30,
                                                          scalar2=None, op0=ALU.max), r=[f"ps{4 + j}"], w=["rds"])
            dve(lambda e: e.reciprocal(out=rds[:, 0:8], in_=rds[:, 0:8]), r=["rds"], w=["rds"])
            dve(lambda e, gv=gv: e.tensor_tensor(out=gss[:, 0:8], in0=rds[:, 0:8], in1=gv[:, :, 1], op=ALU.mult),
                r=["rds", "gts"], w=["gss"])
            for j in range(4):
                ov = psb[4 + j][0:8, 0:130].rearrange("p (g c) -> p g c", g=2)
                dve(lambda e, j=j, ov=ov: e.tensor_tensor(
                    out=ots[:, 2 * j:2 * j + 2, :], in0=ov[:, :, 0:64],
                    in1=gss[:, 2 * j:2 * j + 2].unsqueeze(2).to_broadcast([8, 2, 64]), op=ALU.mult),
                    r=[f"ps{4 + j}", "gss"], w=["ots"])
            dve(lambda e, kvh=kvh: e.tensor_tensor(out=As[:, kvh * 512:(kvh + 1) * 512], in0=As[:, kvh * 512:(kvh + 1) * 512],
                                                   in1=ots[:, 0:8, :].rearrange("p g d -> p (g d)"), op=ALU.add),
                r=["As", "ots"], w=["As"])
            si_[0] += 1
            bk = s_scores(lambda rows, kt: wkTs[rows, kt * 128:(kt + 1) * 128], 4, 0, kvh, b, 128, False)
            pe(lambda e, bk=bk, kvh=kvh, b=b: e.matmul(psb[bk][0:8, 256:320], lhsT=wkTs[kvh * 64:(kvh + 1) * 64, 512:520],
                                                       rhs=qTs2[kvh * 64:(kvh + 1) * 64, b, :], start=True, stop=True),
               r=["wkTs", "qTs2"], w=[f"ps{bk}"])
            tm, px = tmpS[si_[0] % 2], pS[si_[0] % 2]
            dve(lambda e, bk=bk, tm=tm, kvh=kvh: e.tensor_tensor(
                out=tm[:, 0:256].rearrange("p (n c) -> p n c", n=4), in0=psb[bk][:, 0:256].rearrange("p (n c) -> p n c", n=4),
                in1=S_BIASW[:, 0:4, kvh, :], op=ALU.add), r=[f"ps{bk}", "stab"], w=[f"tmS{si_[0] % 2}"])
            dve(lambda e, bk=bk, tm=tm, kvh=kvh: e.tensor_tensor(out=tm[0:8, 256:320], in0=psb[bk][0:8, 256:320],
                                                                 in1=S_BIASW[0:8, 4, kvh, :], op=ALU.add),
                r=[f"ps{bk}", "stab"], w=[f"tmS{si_[0] % 2}"])
            act(lambda e, tm=tm, px=px: e.activation(out=px[:, 0:256], in_=tm[:, 0:256], func=AF.Exp), r=[f"tmS{si_[0] % 2}"], w=[f"pxS{si_[0] % 2}"])
            act(lambda e, tm=tm, px=px: e.activation(out=px[0:8, 256:320], in_=tm[0:8, 256:320], func=AF.Exp),
                r=[f"tmS{si_[0] % 2}"], w=[f"pxS{si_[0] % 2}"])
            for kt in range(5):
                nk = 128 if kt < 4 else 8
                for g in range(8):
                    bk2 = 4 + g // 2
                    pe(lambda e, kt=kt, g=g, bk2=bk2, px=px, nk=nk, kvh=kvh: e.matmul(
                        psb[bk2][0:8, (g % 2) * 65:(g % 2 + 1) * 65], lhsT=px[0:nk, kt * 64 + g * 8:kt * 64 + g * 8 + 8],
                        rhs=wvAs[0:nk, kt, kvh, :], start=(kt == 0 and g % 2 == 0), stop=False, skip_group_check=True),
                        r=[f"pxS{si_[0] % 2}", "wvAs"], w=[f"ps{bk2}"])
            for j in range(4):
                ov = psb[4 + j][0:8, 0:130].rearrange("p (g c) -> p g c", g=2)
                dve(lambda e, j=j, ov=ov: e.tensor_scalar(out=rds[:, 2 * j:2 * j + 2], in0=ov[:, :, 64], scalar1=1e-30,
                                                          scalar2=None, op0=ALU.max), r=[f"ps{4 + j}"], w=["rds"])
            dve(lambda e: e.reciprocal(out=rds[:, 0:8], in_=rds[:, 0:8]), r=["rds"], w=["rds"])
            dve(lambda e, gv=gv: e.tensor_tensor(out=gss[:, 0:8], in0=rds[:, 0:8], in1=gv[:, :, 2], op=ALU.mult),
                r=["rds", "gts"], w=["gss"])
            for j in range(4):
                ov = psb[4 + j][0:8, 0:130].rearrange("p (g c) -> p g c", g=2)
                dve(lambda e, j=j, ov=ov: e.tensor_tensor(
                    out=ots[:, 2 * j:2 * j + 2, :], in0=ov[:, :, 0:64],
                    in1=gss[:, 2 * j:2 * j + 2].unsqueeze(2).to_broadcast([8, 2, 64]), op=ALU.mult),
                    r=[f"ps{4 + j}", "gss"], w=["ots"])
            dve(lambda e, kvh=kvh: e.tensor_tensor(out=As[:, kvh * 512:(kvh + 1) * 512], in0=As[:, kvh * 512:(kvh + 1) * 512],
                                                   in1=ots[:, 0:8, :].rearrange("p g d -> p (g d)"), op=ALU.add),
                r=["As", "ots"], w=["As"])
        act(lambda e: e.activation(out=Abs_[:], in_=As[:], func=AF.Copy), r=["As"], w=["Abs_"])
        sp(lambda e, b=b: e.dma_start(out=Ascr[TP + 8 * b:TP + 8 * b + 8, :], in_=Abs_[:]), r=["Abs_"], w=[("Ascr_s", b)])
    B.release(m4s)
    yacc = B.sb("yacc", [128, NT, 1024])
    hnT = B.sb("hnT", [128, 8, TT], BF16)
    comb = B.sb("comb", [128, NT, 32])
    m5 = B.mark()
    Wa = B.sb("Wa", [128, 8, 1024], BF16)
    Wm = B.sb("Wm", [128, 8, 1024], BF16)
    Wo = B.sb("Wo", [128, 8, 1024], BF16)
    for nm, dst, src in (("Wa", Wa, wa_in), ("Wm", Wm, wm_in), ("Wo", Wo, wo_in)):
        sv = src.rearrange("(kc p) n -> p kc n", p=128)
        for hh in range(2):
            S.dma("pool", lambda e, dst=dst, sv=sv, hh=hh: e.dma_start(out=dst[:, 4 * hh:4 * hh + 4, :],
                                                                        in_=sv[:, 4 * hh:4 * hh + 4, :]),
                  writes=[nm + str(hh)])
    wr = B.sb("wr", [128, 8, 36])
    rbb = B.sb("rbb", [128, 36])
    g2t = B.sb("g2t", [128, 1024])
    sp(lambda e: e.dma_start(out=wr[:], in_=wr_in), w=["wr"])
    sp(lambda e: e.dma_start(out=rbb[:], in_=rb_in), w=["rbb"])
    sp(lambda e: e.dma_start(out=g2t[:], in_=g2_in), w=["g2t"])
    Ab5 = B.sb("Ab5", [128, 1024], BF16)
    Mb5 = B.sb("Mb5", [128, 1024], BF16)
    AT5 = B.sb("AT5", [128, 8, 128], BF16)
    MT5 = B.sb("MT5", [128, 8, 128], BF16)
    mgt = B.sb("mgt", [128, 2048])
    mix = B.sb("mix", [128, 1024])
    mixb = B.sb("mixb", [128, 1024], BF16)
    xT5 = AT5
    x5 = B.sb("x5", [128, 1024])
    hn5 = B.sb("hn5", [128, 1024])
    hnb = B.sb("hnb", [128, 1024], BF16)
    hnTf = B.sb("hnTf", [128, 8, 128])
    s5 = B.sb("s5", [128, 8])
    lg = B.sb("lg", [128, 36])
    r5 = B.sb("r5", [128, 64])
    r5b = B.sb("r5b", [128, 4, 8])
    idf5 = B.sb("idf5", [128, 128])
    sp(lambda e: e.dma_start(out=idf5[:], in_=mtab_in[:, 768:896]), w=["idf5"])

    def tr8(src_bf, dstT, bank, rk, wk):
        pv = bank_bf(bank)
        for kc in range(8):
            pe(lambda e, kc=kc, pv=pv: e.transpose(out=pv[:, kc * 128:(kc + 1) * 128],
                                                   in_=src_bf[:, kc * 128:(kc + 1) * 128], identity=ident[:]),
               r=[rk, "ident"], w=[f"ps{bank}"])
        dve(lambda e, pv=pv: e.tensor_copy(out=dstT, in_=pv.rearrange("p (k c) -> p k c", k=8)),
            r=[f"ps{bank}"], w=[wk])

    def proj1024(lT, W, wname, banks, rk):
        for hh in range(2):
            for kc in range(8):
                pe(lambda e, hh=hh, kc=kc: e.matmul(psb[banks[hh]][:, :], lhsT=lT[:, kc, :],
                                                    rhs=W[:, kc, hh * 512:(hh + 1) * 512],
                                                    start=(kc == 0), stop=(kc == 7)),
                   r=[rk, wname + "0", wname + "1"], w=[f"ps{banks[hh]}"])

    for t in ([] if "moe" in DEV_SKIP else range(NT)):
        r0 = t * 128
        sp(lambda e, r0=r0: e.dma_start(out=Ab5[:], in_=Ascr[r0:r0 + 128, :]), w=["Ab5"])
        sp(lambda e, r0=r0: e.dma_start(out=Mb5[:], in_=Mscr[r0:r0 + 128, :]), w=["Mb5"])
        sp(lambda e, r0=r0: e.dma_start(out=mgt[:], in_=proj[r0:r0 + 128, C_MG:C_MG + 2048]), w=["mgt"])
        sp(lambda e, r0=r0: e.dma_start(out=x5[:], in_=x[r0:r0 + 128, :]), w=["x5"])
        act(lambda e: e.activation(out=mgt[:], in_=mgt[:], func=AF.Sigmoid), r=["mgt"], w=["mgt"])
        tr8(Ab5, AT5[:], 0, "Ab5", "AT5")
        tr8(Mb5, MT5[:], 1, "Mb5", "MT5")
        proj1024(AT5, Wa, "Wa", (2, 3), "AT5")
        proj1024(MT5, Wm, "Wm", (4, 5), "MT5")
        dve(lambda e: e.tensor_tensor(out=mix[:], in0=psall[:, 1024:2048], in1=mgt[:, 0:1024], op=ALU.mult),
            r=["ps2", "ps3", "mgt"], w=["mix"])
        dve(lambda e: e.tensor_tensor(out=hn5[:], in0=psall[:, 2048:3072], in1=mgt[:, 1024:2048], op=ALU.mult),
            r=["ps4", "ps5", "mgt"], w=["hn5"])
        dve(lambda e: e.tensor_tensor(out=mixb[:], in0=mix[:], in1=hn5[:], op=ALU.add), r=["mix", "hn5"], w=["mixb"])
        tr8(mixb, xT5[:], 6, "mixb", "AT5")
        proj1024(xT5, Wo, "Wo", (2, 3), "AT5")
        dve(lambda e, t=t: e.tensor_tensor(out=yacc[:, t, :], in0=psall[:, 1024:2048], in1=x5[:], op=ALU.add),
            r=["ps2", "ps3", "x5"], w=[("yacc", t)])
        act(lambda e, t=t: e.activation(out=mix[:], in_=yacc[:, t, :], func=AF.Square, accum_out=s5[:, 0:1]),
            r=[("yacc", t)], w=["mix", "s5"])
        dve(lambda e: e.tensor_scalar(out=s5[:, 0:1], in0=s5[:, 0:1], scalar1=1.0 / D, scalar2=EPS, op0=ALU.mult,
                                      op1=ALU.add), r=["s5"], w=["s5"])
        act(lambda e: e.activation(out=s5[:, 0:1], in_=s5[:, 0:1], func=AF.Sqrt), r=["s5"], w=["s5"])
        dve(lambda e: e.reciprocal(out=s5[:, 0:1], in_=s5[:, 0:1]), r=["s5"], w=["s5"])
        dve(lambda e, t=t: e.scalar_tensor_tensor(out=hn5[:], in0=yacc[:, t, :], scalar=s5[:, 0:1], in1=g2t[:],
                                                  op0=ALU.mult, op1=ALU.mult), r=[("yacc", t), "s5", "g2t"], w=["hn5"])
        act(lambda e: e.activation(out=hnb[:], in_=hn5[:], func=AF.Copy), r=["hn5"], w=["hnb"])
        tr8(hnb, hnT[:, :, r0:r0 + 128], 7, "hnb", ("hnT", t))
        for kc in range(8):
            bk = 4 + kc // 4
            pe(lambda e, kc=kc, bk=bk: e.transpose(out=psb[bk][:, (kc % 4) * 128:(kc % 4 + 1) * 128],
                                                   in_=hn5[:, kc * 128:(kc + 1) * 128], identity=idf5[:]),
               r=["hn5", "idf5"], w=[f"ps{bk}"])
        dve(lambda e: e.tensor_copy(out=hnTf[:], in_=psall[:, 2048:3072].rearrange("p (k c) -> p k c", k=8)),
            r=["ps4", "ps5"], w=["hnTf"])
        for kc in range(8):
            pe(lambda e, kc=kc: e.matmul(psb[6][:, 0:36], lhsT=hnTf[:, kc, :], rhs=wr[:, kc, :], start=(kc == 0),
                                         stop=(kc == 7)), r=["hnTf", "wr"], w=["ps6"])
        dve(lambda e: e.tensor_tensor(out=lg[:], in0=psb[6][:, 0:36], in1=rbb[:], op=ALU.add), r=["ps6", "rbb"], w=["lg"])
        R_ = lambda a, b_=None: r5[:, a:(a + 1 if b_ is None else b_)]
        dve(lambda e: e.tensor_reduce(out=R_(0), in_=lg[:, 0:4], axis=AX.X, op=ALU.max), r=["lg"], w=["r5"])
        dve(lambda e: e.tensor_scalar(out=R_(1), in0=R_(0), scalar1=-1.0, scalar2=None, op0=ALU.mult), r=["r5"], w=["r5"])
        dve(lambda e: e.tensor_scalar(out=R_(16, 20), in0=lg[:, 0:4], scalar1=R_(0), scalar2=None, op0=ALU.is_ge),
            r=["lg", "r5"], w=["r5"])
        act(lambda e: e.activation(out=R_(20, 24), in_=lg[:, 0:4], func=AF.Exp, bias=R_(1), accum_out=R_(2)),
            r=["lg", "r5"], w=["r5"])
        dve(lambda e: e.reciprocal(out=R_(3), in_=R_(2)), r=["r5"], w=["r5"])
        dve(lambda e: e.tensor_tensor(out=r5b[:], in0=lg[:, 4:36].rearrange("p (g x) -> p g x", g=4),
                                      in1=R_(16, 20).unsqueeze(2).to_broadcast([128, 4, 8]), op=ALU.mult),
            r=["lg", "r5"], w=["r5b"])
        dve(lambda e: e.tensor_reduce(out=R_(24, 32), in_=r5b[:].rearrange("p g x -> p x g"), axis=AX.X, op=ALU.add),
            r=["r5b"], w=["r5"])
        dve(lambda e: e.tensor_reduce(out=R_(4), in_=R_(24, 32), axis=AX.X, op=ALU.max), r=["r5"], w=["r5"])
        dve(lambda e: e.tensor_scalar(out=R_(32, 40), in0=R_(24, 32), scalar1=R_(4), scalar2=None, op0=ALU.is_ge),
            r=["r5"], w=["r5"])
        dve(lambda e: e.scalar_tensor_tensor(out=R_(40, 48), in0=R_(32, 40), scalar=-1e30, in1=R_(24, 32),
                                             op0=ALU.mult, op1=ALU.add), r=["r5"], w=["r5"])
        dve(lambda e: e.tensor_reduce(out=R_(5), in_=R_(40, 48), axis=AX.X, op=ALU.max), r=["r5"], w=["r5"])
        dve(lambda e: e.tensor_scalar(out=R_(48, 56), in0=R_(40, 48), scalar1=R_(5), scalar2=None, op0=ALU.is_ge),
            r=["r5"], w=["r5"])
        dve(lambda e: e.tensor_tensor(out=R_(6), in0=R_(5), in1=R_(4), op=ALU.subtract), r=["r5"], w=["r5"])
        act(lambda e: e.activation(out=R_(6), in_=R_(6), func=AF.Exp), r=["r5"], w=["r5"])
        dve(lambda e: e.tensor_scalar(out=R_(7), in0=R_(6), scalar1=1.0, scalar2=None, op0=ALU.add), r=["r5"], w=["r5"])
        dve(lambda e: e.reciprocal(out=R_(7), in_=R_(7)), r=["r5"], w=["r5"])
        dve(lambda e: e.tensor_tensor(out=R_(8), in0=R_(6), in1=R_(7), op=ALU.mult), r=["r5"], w=["r5"])
        dve(lambda e: e.tensor_scalar(out=R_(7, 9), in0=R_(7, 9), scalar1=R_(3), scalar2=None, op0=ALU.mult),
            r=["r5"], w=["r5"])
        dve(lambda e: e.tensor_scalar(out=R_(56, 64), in0=R_(32, 40), scalar1=R_(7), scalar2=None, op0=ALU.mult),
            r=["r5"], w=["r5"])
        dve(lambda e: e.scalar_tensor_tensor(out=R_(56, 64), in0=R_(48, 56), scalar=R_(8), in1=R_(56, 64),
                                             op0=ALU.mult, op1=ALU.add), r=["r5"], w=["r5"])
        dve(lambda e, t=t: e.tensor_tensor(out=comb[:, t, :].rearrange("p (g x) -> p g x", g=4),
                                           in0=R_(16, 20).unsqueeze(2).to_broadcast([128, 4, 8]),
                                           in1=R_(56, 64).unsqueeze(1).to_broadcast([128, 4, 8]), op=ALU.mult),
            r=["r5"], w=[("comb", t)])
    B.release(m5)

    wg = [B.sb(f"wg{i}", [128, 8, 512], BF16) for i in range(2)]
    wu = [B.sb(f"wu{i}", [128, 8, 512], BF16) for i in range(2)]
    wd = [B.sb(f"wd{i}", [128, 4, 1024], BF16) for i in range(2)]
    sg6 = [B.sb(f"sg6{i}", [128, 512]) for i in range(2)]
    hm6 = [B.sb(f"hm6{i}", [128, 4, 512], BF16) for i in range(2)]
    blocks = [(0, 4), (4, 4), (8, 4), (12, 4), (16, 1)]
    hi_ = 0
    for ex in ([] if "moe" in DEV_SKIP else range(32)):
        wb = ex % 2
        S.dma("pool", lambda e, ex=ex, wb=wb: e.dma_start(out=wg[wb][:], in_=weg_in[ex].rearrange("(kc p) n -> p kc n", p=128)),
              writes=[f"wg{wb}"])
        S.dma("pool", lambda e, ex=ex, wb=wb: e.dma_start(out=wu[wb][:], in_=weu_in[ex].rearrange("(kc p) n -> p kc n", p=128)),
              writes=[f"wu{wb}"])
        S.dma("pool", lambda e, ex=ex, wb=wb: e.dma_start(out=wd[wb][:], in_=wed_in[ex].rearrange("(kc p) n -> p kc n", p=128)),
              writes=[f"wd{wb}"])
        for (t0, ntl) in blocks:
            ncol = ntl * 128
            hb_ = hi_ % 2
            hi_ += 1
            for fc in range(4):
                gb, ub_ = (0, 1) if fc % 2 == 0 else (2, 3)
                for kc in range(8):
                    pe(lambda e, kc=kc, fc=fc, gb=gb, t0=t0, ncol=ncol, wb=wb: e.matmul(
                        psb[gb][:, 0:ncol], lhsT=wg[wb][:, kc, fc * 128:(fc + 1) * 128],
                        rhs=hnT[:, kc, t0 * 128:t0 * 128 + ncol], start=(kc == 0), stop=(kc == 7)),
                        r=[f"wg{wb}"] + [("hnT", t0 + q_) for q_ in range(ntl)], w=[f"ps{gb}"])
                for kc in range(8):
                    pe(lambda e, kc=kc, fc=fc, ub_=ub_, t0=t0, ncol=ncol, wb=wb: e.matmul(
                        psb[ub_][:, 0:ncol], lhsT=wu[wb][:, kc, fc * 128:(fc + 1) * 128],
                        rhs=hnT[:, kc, t0 * 128:t0 * 128 + ncol], start=(kc == 0), stop=(kc == 7)),
                        r=[f"wu{wb}"] + [("hnT", t0 + q_) for q_ in range(ntl)], w=[f"ps{ub_}"])
                sgb = fc % 2
                act(lambda e, gb=gb, sgb=sgb, ncol=ncol: e.activation(out=sg6[sgb][:, 0:ncol], in_=psb[gb][:, 0:ncol],
                                                                      func=AF.Silu), r=[f"ps{gb}"], w=[f"sg6{sgb}"])
                dve(lambda e, ub_=ub_, sgb=sgb, fc=fc, hb_=hb_, ncol=ncol: e.tensor_tensor(
                    out=hm6[hb_][:, fc, 0:ncol], in0=sg6[sgb][:, 0:ncol], in1=psb[ub_][:, 0:ncol], op=ALU.mult),
                    r=[f"sg6{sgb}", f"ps{ub_}"], w=[f"hm6{hb_}"])
            for q_ in range(ntl):
                t = t0 + q_
                ob = (4, 5) if q_ % 2 == 0 else (6, 7)
                for hh in range(2):
                    for fc in range(4):
                        pe(lambda e, hh=hh, fc=fc, q_=q_, ob=ob, hb_=hb_, wb=wb: e.matmul(
                            psb[ob[hh]][:, :], lhsT=hm6[hb_][:, fc, q_ * 128:(q_ + 1) * 128],
                            rhs=wd[wb][:, fc, hh * 512:(hh + 1) * 512], start=(fc == 0), stop=(fc == 3)),
                            r=[f"hm6{hb_}", f"wd{wb}"], w=[f"ps{ob[hh]}"])
                dve(lambda e, t=t, ob=ob, ex=ex: e.scalar_tensor_tensor(
                    out=yacc[:, t, :], in0=psall[:, ob[0] * 512:ob[0] * 512 + 1024], scalar=comb[:, t, ex:ex + 1],
                    in1=yacc[:, t, :], op0=ALU.mult, op1=ALU.add),
                    r=[f"ps{ob[0]}", f"ps{ob[1]}", ("comb", t), ("yacc", t)], w=[("yacc", t)])
    for t in range(NT):
        sp(lambda e, t=t: e.dma_start(out=y_out[t * 128:(t + 1) * 128, :], in_=yacc[:, t, :]),
           r=[("yacc", t)], w=[("y_out", t)])
    S.finish("sp")
    S.emit(nc, B.es)
    B.es.close()
    return nc


_PROGRAM = None


def _bf16(a):
    return np.asarray(a).astype(ml_dtypes.bfloat16)


def _mamba_tables():
    i = np.arange(128)
    same = (i[:, None] // 8) == (i[None, :] // 8)
    le = i[:, None] <= i[None, :]
    tabs = [le, le & same, np.ones((128, 128), bool), same]
    out = [t.astype(np.float32) for t in tabs]
    out.append(np.where(le, 0.0, -1e4).astype(np.float32))
    out.append(np.where(le & same, 0.0, -1e4).astype(np.float32))
    out.append(np.eye(128, dtype=np.float32))
    rowm = (i[:, None] // 8 == np.arange(16)[None, :]).astype(np.float32)
    out.append(rowm)
    out.append(np.broadcast_to(rowm[:, :, None], (128, 16, 128)).reshape(128, 2048))
    return np.ascontiguousarray(np.concatenate(out, axis=1).astype(np.float32))


def _slopes():
    h = np.arange(1, 17, dtype=np.float64)
    return np.exp2(-8.0 * h / 16).astype(np.float32).astype(np.float64)


def _attn_tables(q_norm, k_norm0, cmp_pe):
    sl = _slopes().reshape(2, 8)
    p = np.arange(128, dtype=np.float64)[:, None]
    f = np.arange(128, dtype=np.float64)[None, :]
    cols = []
    cols.append(np.broadcast_to(np.tile(q_norm, 16)[None, :], (128, 1024)))
    cols.append(np.broadcast_to(np.tile(k_norm0, 2)[None, :], (128, 128)))
    n = np.arange(128)[:, None]
    j = np.arange(32)[None, :]
    cover = ((16 * n < 64 * (j + 1)) & (16 * n + 31 >= 64 * j)).astype(np.float64)
    cover[127] = 0
    cols.append(cover)
    basec = -sl[None, :, :, None] * (f[:, None, None, :] - 16 * p[:, :, None, None] - 31)
    cols.append(basec.reshape(128, 2048))
    cols.append(16 * p + 31 - f)
    base = -sl[None, :, :, None] * (f[:, None, None, :] - p[:, :, None, None])
    cols.append(base.reshape(128, 2048))
    cols.append((base + np.where(p > f, -30000.0, 0.0)[:, None, None, :]).reshape(128, 2048))
    cols.append((base + np.where(p <= f, -30000.0, 0.0)[:, None, None, :]).reshape(128, 2048))
    dl = np.arange(65, dtype=np.float64)
    cvec = -(dl[:, None] * 128.0) * _slopes()[None, :]
    cols.append(np.broadcast_to(cvec.reshape(1, 65 * 16), (128, 65 * 16)))
    c = np.arange(62)[None, :] - 30
    hi = (np.arange(128)[:, None] >= 64).astype(np.int64)
    back = hi - c
    cols.append(np.where((back >= 0) & (back <= 1), 1e30, 0.0))
    cols.append(np.where(back < 0, -1.0, 3e38))
    pet = np.transpose(cmp_pe, (2, 0, 1)).reshape(64, 64)
    cols.append(np.concatenate([pet, pet], axis=0))
    out = np.concatenate([np.asarray(c_, np.float64) for c_ in cols], axis=1)
    assert out.shape == (128, AT_W), out.shape
    return np.ascontiguousarray(out.astype(np.float32))


def _sample_tables(q_norm, k_norm0, cmp_pe):
    sl = _slopes().reshape(2, 8)
    p = np.arange(128, dtype=np.float64)
    tq = np.arange(8, dtype=np.float64)
    cols = []
    cols.append(np.broadcast_to(np.tile(q_norm, 16)[None, :], (128, 1024)))
    cols.append(np.broadcast_to(np.tile(k_norm0, 8)[None, :], (128, 512)))
    n = 128 * np.arange(4)[None, :] + p[:, None]
    dist = 8192 + tq[None, None, None, None, :] - (16 * n[:, :, None, None, None] + 31)
    bc = -sl[None, None, :, :, None] * dist
    bc = np.where((n >= 511)[:, :, None, None, None], -30000.0, bc)
    cols.append(bc.reshape(128, 512))
    j = np.arange(129)[None, None, :]
    cov = ((16 * n[:, :, None] < 64 * (j + 1)) & (16 * n[:, :, None] + 31 >= 64 * j) & (n[:, :, None] < 511))
    cols.append(cov.astype(np.float64).reshape(128, 516))
    base2 = -sl[None, :, :, None] * (tq[None, None, None, :] - p[:, None, None, None])
    cols.append(base2.reshape(128, 128))
    kt = np.arange(65, dtype=np.float64)
    cv2 = -sl[None, :, :] * (8192 - 128 * kt)[:, None, None]
    cols.append(np.broadcast_to(cv2.reshape(1, 1040), (128, 1040)))
    w = 128 * np.arange(5)[None, :] + p[:, None]
    dw = 512 + tq[None, None, None, None, :] - w[:, :, None, None, None]
    bw = -sl[None, None, :, :, None] * dw
    bw = np.where((dw >= 0) & (dw < 512) & (w[:, :, None, None, None] < 520), bw, -30000.0)
    cols.append(bw.reshape(128, 640))
    fs = np.zeros((128, 129)); fs[:, [0, 127, 128]] = 1e30
    cols.append(fs)
    pet = np.transpose(cmp_pe, (2, 0, 1)).reshape(64, 64)
    cols.append(np.concatenate([pet, pet], axis=0))
    cols.append(p[:, None])
    out = np.concatenate([np.asarray(c_, np.float64) for c_ in cols], axis=1)
    pad = ST_W - out.shape[1]
    assert pad >= 0, out.shape
    out = np.concatenate([out, np.zeros((128, pad))], axis=1)
    return np.ascontiguousarray(out.astype(np.float32))


def _exw_table():
    r = np.arange(128)[:, None, None] % 64
    kk = np.arange(32)[None, :, None]
    m = np.arange(128)[None, None, :]
    return np.ascontiguousarray((r == 2 * kk + m // 64).astype(np.float32).astype(ml_dtypes.bfloat16))


def _ex_table():
    jj = np.arange(32)[:, None, None]
    kt = np.arange(16)[None, :, None]
    m = np.arange(128)[None, None, :]
    return np.ascontiguousarray((jj == 2 * kt + m // 64).astype(np.float32).astype(ml_dtypes.bfloat16))


def kernel(**inputs):
    global _PROGRAM
    f = lambda k: np.ascontiguousarray(np.asarray(inputs[k]))
    x_prompt, x_sample = f("x_prompt"), f("x_sample")
    k_norm = f("k_norm")[0]
    if _PROGRAM is None:
        _PROGRAM = build_program()
    nc = _PROGRAM
    shared = {
        "w_in": f("w_in")[0],
        "g1_bc": np.ascontiguousarray(np.broadcast_to(f("norm1")[0][None, :], (128, D))),
        "gk1_bc": np.ascontiguousarray(np.broadcast_to(np.tile(k_norm[1], 2)[None, :], (128, 128))),
        "gk2_bc": np.ascontiguousarray(np.broadcast_to(np.tile(k_norm[2], 2)[None, :], (128, 128))),
        "ident_bf": np.eye(128, dtype=np.float32).astype(ml_dtypes.bfloat16),
        "conv_wb": np.ascontiguousarray(np.broadcast_to(
            np.concatenate([f("conv_w")[0], f("conv_b")], axis=0)[None], (128, 5, 2048))),
        "mvec": np.ascontiguousarray(np.broadcast_to(
            np.concatenate([f("dt_bias")[0], f("a_log")[0], f("d_skip")[0]])[None, :], (128, 48))),
        "gssm_bc": np.ascontiguousarray(np.broadcast_to(f("ssm_norm")[0][None, :], (128, 1024))),
        "mtab": _mamba_tables(),
        "atab": _attn_tables(f("q_norm")[0], k_norm[0], f("cmp_pe")[0]),
        "ex_bf": _ex_table(),
        "exw_bf": _exw_table(),
        "stab": _sample_tables(f("q_norm")[0], k_norm[0], f("cmp_pe")[0]),
        "cache_kv": f("cache_kv")[0].reshape(10240 * 128, 512),
        "w_branch_attn": f("w_branch_attn")[0],
        "w_branch_ssm": f("w_branch_ssm")[0],
        "w_out": f("w_out")[0],
        "wr": np.ascontiguousarray(np.transpose(
            np.concatenate([f("w_router_group")[0], f("w_router_expert")[0]], axis=1).reshape(8, 128, 36), (1, 0, 2))),
        "rb_bc": np.ascontiguousarray(np.broadcast_to(
            np.concatenate([f("b_router_group")[0], f("b_router_expert")[0]])[None, :], (128, 36))),
        "g2_bc": np.ascontiguousarray(np.broadcast_to(f("norm2")[0][None, :], (128, D))),
        "w_exp_gate": f("w_exp_gate")[0][:(1 if "moe" in DEV_SKIP else 32)],
        "w_exp_up": f("w_exp_up")[0][:(1 if "moe" in DEV_SKIP else 32)],
        "w_exp_down": f("w_exp_down")[0][:(1 if "moe" in DEV_SKIP else 32)],
        "w1h": np.ascontiguousarray(np.tile(
            np.transpose(f("cmp_w1")[0].reshape(2, 32, 64, 128), (2, 0, 1, 3)), (2, 1, 1, 1))),
        "w2h": np.ascontiguousarray(np.transpose(f("cmp_w2")[0], (1, 0, 2))),
    }
    st_conv = f("state_conv")[0]
    page_table = f("page_table")
    st_ssm = f("state_ssm")[0].reshape(128, 1024, 128)
    cwin = f("cache_win_kv")[0].reshape(128, 512, 256)
    in_maps = []
    for c in range(NCORES):
        m = dict(shared)
        m["x"] = np.ascontiguousarray(np.concatenate(
            [x_prompt[c], x_sample[16 * c:16 * c + 16].reshape(TS, D)], axis=0))
        m["cache_win"] = np.ascontiguousarray(cwin[16 * c:16 * c + 16])
        m["state_conv"] = np.ascontiguousarray(st_conv[16 * c:16 * c + 16])
        m["pt_bc"] = np.ascontiguousarray(np.broadcast_to(
            page_table[16 * c:16 * c + 16].reshape(1, 1024), (128, 1024)).astype(np.int32))
        m["state_ssm"] = np.ascontiguousarray(st_ssm[16 * c:16 * c + 16])
        in_maps.append(m)
    res = run_bass_kernel_spmd(nc, in_maps, core_ids=list(range(NCORES)))
    R = res.results

    def cat(name, sl=None):
        return np.stack([np.asarray(r[name]) if sl is None else np.asarray(r[name])[sl] for r in R], axis=0)

    kv = cat("kv_out")
    kv_prompt = kv[:, :TP].reshape(1, 8, 2048, 4, 2, 64)
    kv_sample = kv[:, TP:].reshape(1, 128, 8, 4, 2, 64)
    win_prompt = cat("winp_out").reshape(1, 8, 512, 2, 2, 64)
    win_sample = cat("wins_out").reshape(1, 128, 512, 2, 2, 64)
    conv_prompt = cat("convp_out").reshape(1, 8, 3, 2048)
    conv_sample = cat("convs_out").reshape(1, 128, 3, 2048)
    yy = cat("y_out")
    y_prompt = np.ascontiguousarray(yy[:, :TP])
    y_sample = np.ascontiguousarray(yy[:, TP:].reshape(128, 8, 1024))
    ssm_prompt = cat("ssmp_out").reshape(1, 8, 16, 64, 128)
    ssm_sample = cat("ssms_out").reshape(1, 128, 16, 64, 128)
    global DEBUG_OUT
    DEBUG_OUT = {"Ascr": np.asarray(R[0]["Ascr"]).astype(np.float32)}
    return (y_prompt, y_sample, kv_prompt, win_prompt, conv_prompt, ssm_prompt,
            kv_sample, win_sample, conv_sample, ssm_sample)
```
